# Optimizing a Trainium2 kernel written in Bass

```python
import jax
import jax.numpy as jnp
from jax import lax
import numpy as np

D_MODEL = 1024
BATCH = 2
SEQ = 16384
DEPTH = 2

N_A_LAYERS = DEPTH // 2
N_B_LAYERS = DEPTH - N_A_LAYERS
N_DENSE = (DEPTH + 1) // 2
N_MOE = DEPTH // 2

RWKV_HEAD = 64
RWKV_HEADS = D_MODEL // RWKV_HEAD
DECAY_LORA = 64
AAA_LORA = 64
GATE_LORA = 160
LNX_EPS = 64e-5

ATTN_HEAD = 64
GROUP_HEADS = 8
DILATED_GROUPS = ((128, 1), (512, 4), (2048, 16))
N_GROUPS = len(DILATED_GROUPS)
Q_WIDTH = N_GROUPS * GROUP_HEADS * ATTN_HEAD
ATTN_OUT = GROUP_HEADS * ATTN_HEAD
Q_BLOCK = 128
ROPE_THETA = 10000.0

D_FF = 2816
N_EXPERTS = 8
TOP_K = 2
D_FF_EXPERT = 3584
MOE_BLOCK = 256

DEEPNORM_ALPHA = (2 * DEPTH) ** 0.25
DEEPNORM_BETA = (8 * DEPTH) ** -0.25
LN_EPS = 1e-5
SUB_MOD = DEPTH * 2 * 3 * D_MODEL
ADA_WIDTH = SUB_MOD + 2 * D_MODEL

kernel_name = 'yoco_rwkv7_dilated_attn_moe'


def layer_norm(x, g, b):
    xf = x.astype(jnp.float32)
    mu = xf.mean(-1, keepdims=True)
    var = jnp.square(xf - mu).mean(-1, keepdims=True)
    return ((xf - mu) * lax.rsqrt(var + LN_EPS) * g + b).astype(x.dtype)


def rope(t):
    s_len, dh = t.shape[1], t.shape[-1]
    half = dh // 2
    inv_freq = ROPE_THETA ** (-jnp.arange(half, dtype=jnp.float32) / half)
    ang = jnp.arange(s_len, dtype=jnp.float32)[:, None] * inv_freq[None, :]
    cos = jnp.cos(ang)[None, :, None, :]
    sin = jnp.sin(ang)[None, :, None, :]
    tf = t.astype(jnp.float32)
    t1, t2 = tf[..., :half], tf[..., half:]
    return jnp.concatenate([t1 * cos - t2 * sin, t2 * cos + t1 * sin], axis=-1).astype(t.dtype)


def rwkv7_time_mix(h, mu, w_rkv, w0, w1, w2, a0, a1, a2, g1, g2, k_k, k_a, r_k, lnx_g, lnx_b, w_out):
    B, S, D = h.shape
    H, N = RWKV_HEADS, RWKV_HEAD
    xx = jnp.pad(h, ((0, 0), (1, 0), (0, 0)))[:, :-1] - h
    w_lerp = (w_rkv.reshape(D, 3, D) * mu[:3].T[:, :, None]).reshape(D, 3 * D)
    r, k, v = jnp.split(h @ w_rkv + xx @ w_lerp, 3, axis=-1)
    xw = h + xx * mu[3]
    xa = h + xx * mu[4]
    xg = h + xx * mu[5]
    w = -jax.nn.softplus(-(w0 + jnp.tanh(xw @ w1) @ w2)) - 0.5
    a = jax.nn.sigmoid(a0 + (xa @ a1) @ a2)
    g = jax.nn.sigmoid(xg @ g1) @ g2

    def heads(t):
        return t.reshape(B, S, H, N).astype(jnp.float32)

    kk = heads(k * k_k)
    kk = kk / jnp.maximum(jnp.linalg.norm(kk, axis=-1, keepdims=True), 1e-12)
    k = k * (1.0 + (a - 1.0) * k_a)
    rh, kh, vh, ah = heads(r), heads(k), heads(v), heads(a)
    decay = jnp.exp(-jnp.exp(heads(w)))
    seq_in = tuple(jnp.moveaxis(t, 1, 0) for t in (rh, decay, kh, vh, -kk, kk * ah))

    def step(state, inp):
        r_t, w_t, k_t, v_t, a_t, b_t = inp
        sa = jnp.einsum('bhvk,bhk->bhv', state, a_t)
        state = state * w_t[:, :, None, :] + sa[..., None] * b_t[:, :, None, :] + v_t[..., None] * k_t[:, :, None, :]
        return state, jnp.einsum('bhvk,bhk->bhv', state, r_t)

    s0 = jnp.zeros((B, H, N, N), jnp.float32)
    _, y = lax.scan(step, s0, seq_in)
    y = jnp.moveaxis(y, 0, 1)
    mu_y = y.mean(-1, keepdims=True)
    var_y = jnp.square(y - mu_y).mean(-1, keepdims=True)
    y = ((y - mu_y) * lax.rsqrt(var_y + LNX_EPS)).reshape(B, S, D) * lnx_g + lnx_b
    bonus = (rh * kh * r_k).sum(-1, keepdims=True) * vh
    y = y + bonus.reshape(B, S, D)
    return (y * g).astype(h.dtype) @ w_out


def shared_kv(h, w_kv):
    B, S, _ = h.shape
    kv = (h @ w_kv).reshape(B, S, 2, N_GROUPS * GROUP_HEADS, ATTN_HEAD)
    k = rope(kv[:, :, 0]).reshape(B, S, N_GROUPS, GROUP_HEADS, ATTN_HEAD)
    v = kv[:, :, 1].reshape(B, S, N_GROUPS, GROUP_HEADS, ATTN_HEAD)
    return k, v


def dilated_branch(q, k, v, window, dilation):
    B, S, H, Dh = q.shape
    steps = window // dilation
    L = S // dilation
    nb = -(-L // Q_BLOCK)
    Lp = nb * Q_BLOCK

    def by_phase(t):
        t = t.reshape(B, L, dilation, H, Dh).transpose(0, 2, 1, 3, 4)
        t = jnp.pad(t, ((0, 0), (0, 0), (0, Lp - L), (0, 0), (0, 0)))
        return t.reshape(B, dilation, nb, Q_BLOCK, H, Dh)

    def with_prev(t):
        prev = jnp.pad(t, ((0, 0), (0, 0), (1, 0), (0, 0), (0, 0), (0, 0)))[:, :, :nb]
        return jnp.concatenate([prev, t], axis=3)

    qb = by_phase(q)
    kc = with_prev(by_phase(k))
    vc = with_prev(by_phase(v)).astype(jnp.float32)
    s = jnp.einsum('bpnqhd,bpnkhd->bpnhqk', qb, kc).astype(jnp.float32) * (Dh ** -0.5)
    qi = jnp.arange(Q_BLOCK)[:, None] + Q_BLOCK
    ki = jnp.arange(2 * Q_BLOCK)[None, :]
    dist = qi - ki
    band = (dist >= 0) & (dist <= steps)
    valid = (jnp.arange(nb)[:, None] > 0) | (ki >= Q_BLOCK)
    mask = band[None] & valid[:, None, :]
    s = jnp.where(mask[None, None, :, None], s, -jnp.inf)
    m = s.max(-1, keepdims=True)
    e = jnp.exp(s - m)
    l = e.sum(-1, keepdims=True)
    o = jnp.einsum('bpnhqk,bpnkhd->bpnqhd', e, vc) / jnp.swapaxes(l[..., 0], -1, -2)[..., None]
    lse = jnp.swapaxes((m + jnp.log(l))[..., 0], -1, -2)

    def from_phase(t):
        t = t.reshape((B, dilation, Lp) + t.shape[4:])[:, :, :L]
        return jnp.moveaxis(t, 1, 2).reshape((B, S) + t.shape[3:])

    return from_phase(o), from_phase(lse)


def dilated_attention(h, k_sh, v_sh, w_q, w_o):
    B, S, _ = h.shape
    q = rope((h @ w_q).reshape(B, S, N_GROUPS * GROUP_HEADS, ATTN_HEAD))
    q = q.reshape(B, S, N_GROUPS, GROUP_HEADS, ATTN_HEAD)
    outs, lses = [], []
    for gi, (win, dil) in enumerate(DILATED_GROUPS):
        o, lse = dilated_branch(q[:, :, gi], k_sh[:, :, gi], v_sh[:, :, gi], win, dil)
        outs.append(o)
        lses.append(lse)
    wts = jax.nn.softmax(jnp.stack(lses), axis=0)
    o = jnp.sum(wts[..., None] * jnp.stack(outs), axis=0)
    return o.reshape(B, S, ATTN_OUT).astype(h.dtype) @ w_o


def swiglu(h, w_gu, w_down):
    gt, up = jnp.split(h @ w_gu, 2, axis=-1)
    return (jax.nn.silu(gt) * up) @ w_down


def moe_swiglu(h, router, w_gu, w_down):
    B, S, D = h.shape
    T = B * S
    A = T * TOP_K
    ht = h.reshape(T, D)
    logits = (ht @ router).astype(jnp.float32)
    top_logit, top_idx = lax.top_k(logits, TOP_K)
    top_w = jax.nn.softmax(top_logit, axis=-1)
    e_flat = top_idx.reshape(-1)
    t_flat = jnp.repeat(jnp.arange(T, dtype=jnp.int32), TOP_K)
    w_flat = top_w.reshape(-1)
    order = jnp.argsort(e_flat)
    e_sorted = e_flat[order]
    counts = jnp.bincount(e_flat, length=N_EXPERTS)
    padded = (counts + MOE_BLOCK - 1) // MOE_BLOCK * MOE_BLOCK
    start = jnp.cumsum(counts) - counts
    pad_end = jnp.cumsum(padded)
    pad_start = pad_end - padded
    dest = pad_start[e_sorted] + jnp.arange(A) - start[e_sorted]
    n_blocks = -(-A // MOE_BLOCK) + N_EXPERTS
    R = n_blocks * MOE_BLOCK
    row_tok = jnp.full((R,), T, jnp.int32).at[dest].set(t_flat[order])
    row_w = jnp.zeros((R,), jnp.float32).at[dest].set(w_flat[order])
    block_exp = jnp.minimum(jnp.searchsorted(pad_end, jnp.arange(n_blocks) * MOE_BLOCK, side='right'), N_EXPERTS - 1)
    h_pad = jnp.concatenate([ht, jnp.zeros((1, D), ht.dtype)], axis=0)
    xb = h_pad[row_tok].reshape(n_blocks, MOE_BLOCK, D)

    def expert_block(args):
        xe, e = args
        gt, up = jnp.split(xe @ w_gu[e], 2, axis=-1)
        return (jax.nn.silu(gt) * up) @ w_down[e]

    yb = lax.map(expert_block, (xb, block_exp))
    y = yb.reshape(R, D) * row_w[:, None].astype(yb.dtype)
    out = jnp.zeros((T + 1, D), y.dtype).at[row_tok].add(y)[:T]
    return out.reshape(B, S, D)


def setup_inputs(seed: int = 0) -> dict:
    key = jax.random.key(seed)
    k = jax.random.split(key, 30)
    D = D_MODEL
    NA, NB = N_A_LAYERS, N_B_LAYERS

    def nrm(i, shape, scale):
        return jax.random.normal(k[i], shape, jnp.float32) * scale

    def uni(i, shape, lo, hi):
        return jax.random.uniform(k[i], shape, jnp.float32, lo, hi)

    return {
        'x': nrm(0, (BATCH, SEQ, D), 1.0),
        'c': nrm(1, (BATCH, D), 1.0),
        'ada_w': nrm(2, (D, ADA_WIDTH), 0.3 * D ** -0.5),
        'ada_b': nrm(3, (ADA_WIDTH,), 0.01),
        'ln_g': 1.0 + nrm(4, (DEPTH, 2, D), 0.02),
        'ln_b': nrm(5, (DEPTH, 2, D), 0.02),
        'rw_mu': uni(6, (NA, 6, D), 0.0, 1.0),
        'rw_w_rkv': nrm(7, (NA, D, 3 * D), D ** -0.5),
        'rw_w0': uni(8, (NA, D), -6.0, 0.0),
        'rw_w1': nrm(9, (NA, D, DECAY_LORA), D ** -0.5),
        'rw_w2': nrm(10, (NA, DECAY_LORA, D), 0.5 * DECAY_LORA ** -0.5),
        'rw_a0': nrm(11, (NA, D), 0.1),
        'rw_a1': nrm(12, (NA, D, AAA_LORA), D ** -0.5),
        'rw_a2': nrm(13, (NA, AAA_LORA, D), AAA_LORA ** -0.5),
        'rw_g1': nrm(14, (NA, D, GATE_LORA), D ** -0.5),
        'rw_g2': nrm(15, (NA, GATE_LORA, D), GATE_LORA ** -0.5),
        'rw_k_k': 0.85 + nrm(16, (NA, D), 0.05),
        'rw_k_a': 1.0 + nrm(17, (NA, D), 0.05),
        'rw_r_k': nrm(18, (NA, RWKV_HEADS, RWKV_HEAD), 0.1),
        'rw_lnx_g': 1.0 + nrm(19, (NA, D), 0.02),
        'rw_lnx_b': nrm(20, (NA, D), 0.02),
        'rw_w_out': nrm(21, (NA, D, D), DEEPNORM_BETA * D ** -0.5),
        'kv_w': nrm(22, (D, 2 * Q_WIDTH), D ** -0.5),
        'attn_w_q': nrm(23, (NB, D, Q_WIDTH), D ** -0.5),
        'attn_w_o': nrm(24, (NB, ATTN_OUT, D), DEEPNORM_BETA * ATTN_OUT ** -0.5),
        'ffn_w_gu': nrm(25, (N_DENSE, D, 2 * D_FF), D ** -0.5),
        'ffn_w_down': nrm(26, (N_DENSE, D_FF, D), DEEPNORM_BETA * D_FF ** -0.5),
        'moe_router': nrm(27, (N_MOE, D, N_EXPERTS), D ** -0.5),
        'moe_w_gu': nrm(28, (N_MOE, N_EXPERTS, D, 2 * D_FF_EXPERT), D ** -0.5),
        'moe_w_down': nrm(29, (N_MOE, N_EXPERTS, D_FF_EXPERT, D), DEEPNORM_BETA * D_FF_EXPERT ** -0.5),
    }


def reference(x, c, ada_w, ada_b, ln_g, ln_b, rw_mu, rw_w_rkv, rw_w0, rw_w1, rw_w2, rw_a0, rw_a1, rw_a2,
              rw_g1, rw_g2, rw_k_k, rw_k_a, rw_r_k, rw_lnx_g, rw_lnx_b, rw_w_out, kv_w, attn_w_q, attn_w_o,
              ffn_w_gu, ffn_w_down, moe_router, moe_w_gu, moe_w_down):
    b = x.shape[0]
    mod = jax.nn.silu(c) @ ada_w + ada_b
    sub_mod = mod[:, :SUB_MOD].reshape(b, DEPTH, 2, 3, D_MODEL)
    kv_mod = mod[:, SUB_MOD:].reshape(b, 2, D_MODEL)

    def modulate(t, shift, scale):
        return t * (1.0 + scale[:, None, :]) + shift[:, None, :]

    def post_norm(t, y, l, j):
        gate = sub_mod[:, l, j, 2][:, None, :]
        return layer_norm(DEEPNORM_ALPHA * t + (1.0 + gate) * y, ln_g[l, j], ln_b[l, j])

    k_sh, v_sh = None, None
    for l in range(DEPTH):
        h = modulate(x, sub_mod[:, l, 0, 0], sub_mod[:, l, 0, 1])
        if l < N_A_LAYERS:
            i = l
            y = rwkv7_time_mix(h, rw_mu[i], rw_w_rkv[i], rw_w0[i], rw_w1[i], rw_w2[i], rw_a0[i], rw_a1[i],
                               rw_a2[i], rw_g1[i], rw_g2[i], rw_k_k[i], rw_k_a[i], rw_r_k[i], rw_lnx_g[i],
                               rw_lnx_b[i], rw_w_out[i])
        else:
            i = l - N_A_LAYERS
            y = dilated_attention(h, k_sh, v_sh, attn_w_q[i], attn_w_o[i])
        x = post_norm(x, y, l, 0)
        h = modulate(x, sub_mod[:, l, 1, 0], sub_mod[:, l, 1, 1])
        if l % 2 == 0:
            y = swiglu(h, ffn_w_gu[l // 2], ffn_w_down[l // 2])
        else:
            y = moe_swiglu(h, moe_router[l // 2], moe_w_gu[l // 2], moe_w_down[l // 2])
        x = post_norm(x, y, l, 1)
        if l == N_A_LAYERS - 1:
            k_sh, v_sh = shared_kv(modulate(x, kv_mod[:, 0], kv_mod[:, 1]), kv_w)
    return x
```

```python
import math
import numpy as np
from contextlib import ExitStack
import concourse.bass as bass
import concourse.mybir as mybir
from concourse.bass_utils import run_bass_kernel_spmd
import ml_dtypes

F32 = mybir.dt.float32
BF16 = mybir.dt.bfloat16
I32 = mybir.dt.int32
AF = mybir.ActivationFunctionType
ALU = mybir.AluOpType
AX = mybir.AxisListType

D = 1024
SEQ = 16384
NCORE = 8
TPC = 4096
ALPHA = 4.0 ** 0.25
LN_EPS = 1e-5
D_FF = 2816
NEXP = 8
D_FFE = 3584
TWO_PI = 2.0 * math.pi


SAME_ENGINE_FIFO = False
EMBED_WAIT = True


class Prog:
    CE = ('pe', 'act', 'dve', 'pool')

    def __init__(self, nc, stack, ndma=12):
        self.nc = nc
        self.q = {e: [] for e in ('pe', 'act', 'dve', 'pool', 'sp')}
        self.esem = {e: stack.enter_context(nc.semaphore('s_' + e)) for e in self.CE}
        self.ecnt = {e: 0 for e in self.CE}
        self.dpool = {e: [stack.enter_context(nc.semaphore('d_%s%d' % (e, i))) for i in range(ndma)]
                      for e in ('sp', 'pool', 'act')}
        self.dnext = {e: 0 for e in self.dpool}
        self.dval = {}
        self.lastw = {}
        self.readers = {}
        self.seen = {e: {} for e in self.q}
        self.semobj = {}

    def _deps(self, eng, reads, writes):
        evs = []
        for k in reads:
            ev = self.lastw.get(k)
            if ev is not None:
                evs.append(ev)
        for k in writes:
            ev = self.lastw.get(k)
            if ev is not None:
                evs.append(ev)
            evs.extend(self.readers.get(k, ()))
        need = {}
        for (sem, val, src) in evs:
            if src == eng and (eng == 'pe' or (SAME_ENGINE_FIFO and eng in ('act', 'dve'))):
                continue
            if self.seen[eng].get(sem.num, 0) >= val:
                continue
            if need.get(sem.num, 0) < val:
                need[sem.num] = val
                self.semobj[sem.num] = sem
        for num, val in need.items():
            self.q[eng].append(('wait', self.semobj[num], val))
            self.seen[eng][num] = val

    def _commit(self, ev, reads, writes):
        ws = set(writes)
        for k in writes:
            self.lastw[k] = ev
            self.readers[k] = []
        for k in reads:
            if k in ws:
                continue
            lst = self.readers.setdefault(k, [])
            lst[:] = [e for e in lst if e[0].num != ev[0].num]
            lst.append(ev)

    def op(self, eng, fn, reads=(), writes=()):
        self._deps(eng, reads, writes)
        self.ecnt[eng] += 1
        ev = (self.esem[eng], self.ecnt[eng], eng)
        self.q[eng].append(('op', fn, ev))
        self._commit(ev, reads, writes)

    def dma(self, eng, out, in_, reads=(), writes=(), **kw):
        self._deps(eng, reads, writes)
        pool = self.dpool[eng]
        sem = pool[self.dnext[eng] % len(pool)]
        self.dnext[eng] += 1
        prev = self.dval.get(sem.num, 0)
        if prev > 0 and self.seen[eng].get(sem.num, 0) < prev:
            self.q[eng].append(('wait', sem, prev))
            self.seen[eng][sem.num] = prev
        self.dval[sem.num] = prev + 16
        ev = (sem, prev + 16, 'dma')
        self.q[eng].append(('dma', out, in_, ev, kw))
        self._commit(ev, reads, writes)

    def barrier(self):
        for eng in self.q:
            for src in self.CE:
                if self.ecnt[src] > 0 and self.seen[eng].get(self.esem[src].num, 0) < self.ecnt[src]:
                    self.q[eng].append(('wait', self.esem[src], self.ecnt[src]))
                    self.seen[eng][self.esem[src].num] = self.ecnt[src]
            for e2 in self.dpool:
                for sem in self.dpool[e2]:
                    v = self.dval.get(sem.num, 0)
                    if v > 0 and self.seen[eng].get(sem.num, 0) < v:
                        self.q[eng].append(('wait', sem, v))
                        self.seen[eng][sem.num] = v

    def finish(self):
        for e in self.dpool:
            for sem in self.dpool[e]:
                v = self.dval.get(sem.num, 0)
                if v > 0:
                    self.q['sp'].append(('wait', sem, v))
        for e in self.CE:
            if self.ecnt[e] > 0:
                self.q['sp'].append(('wait', self.esem[e], self.ecnt[e]))

    def emit(self):
        nc = self.nc
        with nc.Block() as block:
            def run(engname):
                def body(e):
                    pend = []
                    for it in self.q[engname]:
                        if it[0] == 'wait':
                            pend.append(it)
                            continue
                        emb = None
                        if EMBED_WAIT and pend:
                            emb = pend.pop()
                        for w in pend:
                            e.wait_ge(w[1], w[2])
                        pend = []
                        if it[0] == 'op':
                            ins = it[1](e)
                            if emb is not None:
                                ins = ins._wait_ge(emb[1], emb[2])
                            ins.then_inc(it[2][0], 1)
                        else:
                            ins = e.dma_start(out=it[1], in_=it[2], **it[4])
                            if emb is not None:
                                ins = ins._wait_ge(emb[1], emb[2])
                            ins.then_inc(it[3][0], 16)
                    for w in pend:
                        e.wait_ge(w[1], w[2])
                return body
            block.sync(run('sp'))
            block.tensor(run('pe'))
            block.scalar(run('act'))
            block.vector(run('dve'))
            block.gpsimd(run('pool'))


class Ctx:
    def __init__(self, nc, st):
        self.nc = nc
        self.st = st
        self.p = Prog(nc, st)
        self.ps = [st.enter_context(nc.psum_tensor("ps%d" % i, [128, 512], F32)) for i in range(5)]
        self.pt = [st.enter_context(nc.psum_tensor("pt%d" % i, [128, 2, 512], BF16)) for i in range(3)]
        self.psn = 0
        self.ptn = 0
        self.evn = 0
        self.ident = self.sb("ident", [128, 128], BF16)
        p = self.p
        p.op('pool', lambda e: e.memset(self.ident[:], 0.0), writes=['ident'])
        p.op('pool', lambda e: e.affine_select(out=self.ident[:], in_=self.ident[:], pattern=[[-1, 128]],
                                               compare_op=ALU.not_equal, fill=1.0, base=0, channel_multiplier=1),
             reads=['ident'], writes=['ident'])

    def sb(self, name, shape, dt):
        return self.st.enter_context(self.nc.sbuf_tensor(name, shape, dt))

    def psum(self):
        i = self.psn % len(self.ps)
        self.psn += 1
        return self.ps[i], ('ps', i)

    def ptbank(self):
        i = self.ptn % len(self.pt)
        self.ptn += 1
        return self.pt[i], ('pt', i)

    def evac_eng(self):
        self.evn += 1
        return 'dve' if self.evn % 2 else 'act'

    def copy(self, eng, out, in_, reads, writes):
        if eng == 'act':
            self.p.op('act', lambda e: e.copy(out=out, in_=in_), reads=reads, writes=writes)
        else:
            self.p.op(eng, lambda e: e.tensor_copy(out=out, in_=in_), reads=reads, writes=writes)

    def to_featmajor(self, src, srckeys, dst, dstkey, ntile, nchunk):
        p = self.p
        for c2 in range(0, nchunk, 2):
            bank, bk = self.ptbank()
            ncc = min(2, nchunk - c2)
            for cc in range(ncc):
                c = c2 + cc
                for j in range(ntile):
                    p.op('pe', lambda e, c=c, cc=cc, j=j, bank=bank: e.transpose(out=bank[:, cc, j * 128:(j + 1) * 128],
                                                                                 in_=src[:, j, c * 128:(c + 1) * 128],
                                                                                 identity=self.ident[:]),
                         reads=[srckeys[j], 'ident'], writes=[bk])
            self.copy(self.evac_eng(), dst[:, c2:c2 + ncc, 0:ntile * 128], bank[:, 0:ncc, 0:ntile * 128], [bk],
                      [(dstkey, c2 + cc) for cc in range(ncc)])

    def layer_norm_tile(self, z, zkey, tag):
        p = self.p
        if not hasattr(self, 'ln_tmp'):
            self.ln_stats = self.sb("ln_stats", [128, 2, 6], F32)
            self.ln_mv = self.sb("ln_mv", [128, 2], F32)
            self.ln_rs = self.sb("ln_rs", [128, 1], F32)
            self.ln_nm = self.sb("ln_nm", [128, 1], F32)
            self.ln_tmp = True
        st_, mv, rs, nm = self.ln_stats, self.ln_mv, self.ln_rs, self.ln_nm
        for h in range(2):
            p.op('dve', lambda e, h=h: e.bn_stats(out=st_[:, h, :], in_=z[:, h * 512:(h + 1) * 512]),
                 reads=[zkey], writes=[('lnst', h)])
        p.op('dve', lambda e: e.bn_aggr(out=mv[:], in_=st_[:].rearrange("p a b -> p (a b)")),
             reads=[('lnst', 0), ('lnst', 1)], writes=['lnmv'])
        p.op('act', lambda e: e.activation(out=rs[:], in_=mv[:, 1:2], func=AF.Sqrt, bias=LN_EPS, scale=1.0),
             reads=['lnmv'], writes=['lnrs'])
        p.op('dve', lambda e: e.reciprocal(out=rs[:], in_=rs[:]), reads=['lnrs'], writes=['lnrs'])
        p.op('dve', lambda e: e.tensor_scalar(out=nm[:], in0=mv[:, 0:1], scalar1=rs[:], scalar2=-1.0,
                                              op0=ALU.mult, op1=ALU.mult),
             reads=['lnmv', 'lnrs'], writes=['lnnm'])
        p.op('act', lambda e: e.activation(out=z, in_=z, func=AF.Identity, bias=nm[:], scale=rs[:]),
             reads=[zkey, 'lnrs', 'lnnm'], writes=[zkey])

    def bcast_row(self, dst, dstkey, row_ap, eng='sp'):
        self.p.dma(eng, dst, row_ap.partition_broadcast(128), writes=[dstkey])


def _silu_c_bcast(cx, cT_ap):
    p = cx.p
    ct = cx.sb("ct", [128, 8], F32)
    zer = cx.sb("zer", [128, 128], F32)
    scB = cx.sb("scB", [128, 8, 128], BF16)
    p.dma('sp', ct[:], cT_ap, writes=['ct'])
    p.op('act', lambda e: e.activation(out=ct[:], in_=ct[:], func=AF.Silu), reads=['ct'], writes=['ct'])
    p.op('pool', lambda e: e.memset(zer[:], 0.0), writes=['zer'])
    for c in range(8):
        p.op('act', lambda e, c=c: e.activation(out=scB[:, c, :], in_=zer[:], func=AF.Identity,
                                                bias=ct[:, c:c + 1], scale=1.0),
             reads=['ct', 'zer'], writes=['scB'])
    return scB


def _mod_tile(cx, scB, dst, dstkey, adaw_blk, adab_row, wbuf, wkey, tmp, tmpkey, plus_one):
    p = cx.p
    cx.bcast_row(tmp, tmpkey, adab_row)
    wv = adaw_blk.rearrange("(c p) n -> p c n", p=128)
    for half in range(2):
        p.dma('pool', wbuf[:, :, :], wv[:, :, half * 512:(half + 1) * 512], reads=[], writes=[wkey])
        ps, pk = cx.psum()
        for c in range(8):
            p.op('pe', lambda e, c=c, ps=ps: e.matmul(ps[:], lhsT=scB[:, c, :], rhs=wbuf[:, c, :],
                                                      start=(c == 0), stop=(c == 7)),
                 reads=['scB', wkey], writes=[pk])
        sl = slice(half * 512, (half + 1) * 512)
        p.op('dve', lambda e, ps=ps, sl=sl: e.scalar_tensor_tensor(out=dst[:, sl], in0=ps[:], scalar=(1.0 if plus_one else 0.0),
                                                                    in1=tmp[:, sl], op0=ALU.add, op1=ALU.add),
             reads=[pk, tmpkey], writes=[dstkey])


def _rope_tables(cx, posb_ap, ntile, ang, angkey, kk, kkkey):
    p = cx.p
    nc = cx.nc
    posi = cx.sb("posi", [128, ntile], I32)
    pos = cx.sb("pos", [128, ntile], F32)
    pb = cx.sb("pb", [128, 1], F32)
    cos = cx.sb("cos", [128, ntile, 32], F32)
    sin = cx.sb("sin", [128, ntile, 32], F32)
    p.dma('sp', pb[:], posb_ap, writes=['pb'])
    p.op('pool', lambda e: e.iota(posi[:], pattern=[[128, ntile]], base=0, channel_multiplier=1), writes=['posi'])
    p.op('dve', lambda e: e.tensor_copy(out=pos[:], in_=posi[:]), reads=['posi'], writes=['pos'])
    p.op('dve', lambda e: e.tensor_scalar(out=pos[:], in0=pos[:], scalar1=pb[:], scalar2=None, op0=ALU.add),
         reads=['pos', 'pb'], writes=['pos'])
    for i in range(32):
        invf = float(np.float32(10000.0) ** np.float32(-i / 32.0))
        p.op('dve', lambda e, i=i, invf=invf: e.tensor_scalar(out=ang[:, :, i], in0=pos[:], scalar1=invf, scalar2=None,
                                                              op0=ALU.mult),
             reads=['pos'], writes=[angkey])
    MAGIC = 12582912.0
    C1 = 6.28125
    C2 = TWO_PI - C1
    p.op('dve', lambda e: e.tensor_scalar(out=kk, in0=ang, scalar1=1.0 / TWO_PI, scalar2=MAGIC,
                                          op0=ALU.mult, op1=ALU.add), reads=[angkey], writes=[kkkey])
    p.op('dve', lambda e: e.tensor_scalar(out=kk, in0=kk, scalar1=-MAGIC, scalar2=None, op0=ALU.add),
         reads=[kkkey], writes=[kkkey])
    p.op('dve', lambda e: e.scalar_tensor_tensor(out=ang, in0=kk, scalar=-C1, in1=ang, op0=ALU.mult, op1=ALU.add),
         reads=[kkkey, angkey], writes=[angkey])
    p.op('dve', lambda e: e.scalar_tensor_tensor(out=ang, in0=kk, scalar=-C2, in1=ang, op0=ALU.mult, op1=ALU.add),
         reads=[kkkey, angkey], writes=[angkey])
    p.op('dve', lambda e: e.tensor_scalar(out=ang, in0=ang, scalar1=math.pi, scalar2=-math.pi, op0=ALU.min, op1=ALU.max),
         reads=[angkey], writes=[angkey])
    p.op('act', lambda e: e.activation(out=sin[:], in_=ang, func=AF.Sin), reads=[angkey], writes=['sin'])
    p.op('act', lambda e: e.activation(out=kk, in_=ang, func=AF.Abs), reads=[angkey], writes=[kkkey])
    p.op('act', lambda e: e.activation(out=cos[:], in_=kk, func=AF.Sin, bias=cx.halfpi[:], scale=-1.0),
         reads=[kkkey, 'halfpi'], writes=['cos'])
    return cos, sin


def _rope_apply(cx, ps, pk, out, outkey, cos_t, sin_t, nh, tmp):
    p = cx.p
    pv = ps.rearrange("p (h t d) -> p h t d", h=nh, t=2)
    ov = out.rearrange("p (h t d) -> p h t d", h=nh, t=2)
    cb = cos_t.unsqueeze(1).to_broadcast([128, nh, 32])
    sbb = sin_t.unsqueeze(1).to_broadcast([128, nh, 32])
    ta = tmp[:, 0, :].rearrange("p (h d) -> p h d", h=nh)
    tb = tmp[:, 1, :].rearrange("p (h d) -> p h d", h=nh)
    tk = ('ropetmp',)
    p.op('dve', lambda e: e.tensor_tensor(out=ta, in0=pv[:, :, 0, :], in1=cb, op=ALU.mult), reads=[pk, 'cos'], writes=[tk])
    p.op('dve', lambda e: e.tensor_tensor(out=tb, in0=pv[:, :, 1, :], in1=sbb, op=ALU.mult), reads=[pk, 'sin'], writes=[tk])
    p.op('dve', lambda e: e.tensor_tensor(out=ov[:, :, 0, :], in0=ta, in1=tb, op=ALU.subtract), reads=[tk], writes=[outkey])
    p.op('dve', lambda e: e.tensor_tensor(out=ta, in0=pv[:, :, 1, :], in1=cb, op=ALU.mult), reads=[pk, 'cos'], writes=[tk])
    p.op('dve', lambda e: e.tensor_tensor(out=tb, in0=pv[:, :, 0, :], in1=sbb, op=ALU.mult), reads=[pk, 'sin'], writes=[tk])
    p.op('dve', lambda e: e.tensor_tensor(out=ov[:, :, 1, :], in0=ta, in1=tb, op=ALU.add), reads=[tk], writes=[outkey])


def build_B(nblk=TPC // 512):
    nc = bass.Bass("TRN2", target_bir_lowering=False)
    dt = lambda name, shape, dtype, kind: nc.dram_tensor(name, shape, dtype, kind=kind).ap()
    x_d = dt("x", [TPC, D], F32, "ExternalInput")
    yg_d = dt("yg", [TPC, D], F32, "ExternalInput")
    cT_d = dt("cT", [128, 8], F32, "ExternalInput")
    adaw_d = dt("adaw", [8, D, D], F32, "ExternalInput")
    adab_d = dt("adab", [8, D], F32, "ExternalInput")
    lng_d = dt("lng", [2, D], F32, "ExternalInput")
    lnb_d = dt("lnb", [2, D], F32, "ExternalInput")
    wout_d = dt("w_out", [D, D], F32, "ExternalInput")
    wgu_d = dt("w_gu", [D, 2 * D_FF], F32, "ExternalInput")
    wdn_d = dt("w_down", [D_FF, D], F32, "ExternalInput")
    wkv_d = dt("w_kv", [D, 3072], F32, "ExternalInput")
    wq_d = dt("w_q", [D, 1536], F32, "ExternalInput")
    posb_d = dt("posb", [128, 1], F32, "ExternalInput")
    x2_d = dt("x2", [TPC, D], F32, "ExternalOutput")
    k_d = dt("k", [TPC, 1536], BF16, "ExternalOutput")
    v_d = dt("v", [TPC, 1536], BF16, "ExternalOutput")
    q_d = dt("q", [TPC, 1536], BF16, "ExternalOutput")

    with ExitStack() as st:
        cx = Ctx(nc, st)
        p = cx.p
        sb = cx.sb
        XA = sb("XA", [128, 4, D], F32)
        YB = sb("YB", [128, 4, D], F32)
        TB = sb("TB", [128, 4, D], BF16)
        HT = [sb("HT%d" % i, [128, 8, 512], BF16) for i in range(2)]
        HID = sb("HID", [128, 22, 512], BF16)
        SG = [sb("SG%d" % i, [128, 512], F32) for i in range(2)]
        W8 = [sb("W8_%d" % i, [128, 8, 512], BF16) for i in range(2)]
        WD = [sb("WD_%d" % i, [128, 22, 256], BF16) for i in range(2)]
        OUTB = sb("OUTB", [128, 4, 1536], BF16)
        TMP = sb("TMP", [128, D], F32)
        RT = sb("RT", [128, 2, 256], F32)
        cx.halfpi = sb("halfpi", [128, 1], F32)
        p.op('pool', lambda e: e.memset(cx.halfpi[:], math.pi / 2), writes=['halfpi'])
        names = ["G1", "Gb0", "Bb0", "G2", "B2", "G3g", "Gb1", "Bb1", "G4", "B4", "G5", "B5"]
        T = {n: sb(n, [128, D], F32) for n in names}

        scB = _silu_c_bcast(cx, cT_d)
        tm = {"sh1": XA[:, 0, :], "sc1": XA[:, 1, :], "shkv": XA[:, 2, :], "sckv": XA[:, 3, :],
              "shq": YB[:, 0, :], "scq": YB[:, 1, :], "bias": YB[:, 2, :]}
        tk = {'sh1': ('XA', 0), 'sc1': ('XA', 1), 'shkv': ('XA', 2), 'sckv': ('XA', 3), 'shq': ('YB', 0), 'scq': ('YB', 1), 'bias': ('YB', 2)}

        def mod(dst, dkey, blk, plus_one):
            _mod_tile(cx, scB, dst, dkey, adaw_d[blk], adab_d[blk:blk + 1, :], W8[0], ('W8', 0), tm["bias"], tk["bias"], plus_one)

        mod(T["G1"][:], "G1", 0, True)
        mod(tm["sh1"], tk["sh1"], 1, False)
        mod(tm["sc1"], tk["sc1"], 2, True)
        mod(T["G3g"][:], "G3g", 3, True)
        mod(tm["shkv"], tk["shkv"], 4, False)
        mod(tm["sckv"], tk["sckv"], 5, True)
        mod(tm["shq"], tk["shq"], 6, False)
        mod(tm["scq"], tk["scq"], 7, True)
        cx.bcast_row(T["Gb0"][:], "Gb0", lng_d[0:1, :])
        cx.bcast_row(T["Bb0"][:], "Bb0", lnb_d[0:1, :])
        cx.bcast_row(T["Gb1"][:], "Gb1", lng_d[1:2, :])
        cx.bcast_row(T["Bb1"][:], "Bb1", lnb_d[1:2, :])

        def fold(G, B, Gb, Bb, sc, sh):
            p.op('dve', lambda e: e.tensor_tensor(out=T[G][:], in0=T[Gb][:], in1=tm[sc], op=ALU.mult), reads=[Gb, tk[sc]], writes=[G])
            p.op('dve', lambda e: e.tensor_tensor(out=T[B][:], in0=T[Bb][:], in1=tm[sc], op=ALU.mult), reads=[Bb, tk[sc]], writes=[B])
            p.op('dve', lambda e: e.tensor_tensor(out=T[B][:], in0=T[B][:], in1=tm[sh], op=ALU.add), reads=[B, tk[sh]], writes=[B])

        fold("G2", "B2", "Gb0", "Bb0", "sc1", "sh1")
        fold("G4", "B4", "Gb1", "Bb1", "sckv", "shkv")
        fold("G5", "B5", "Gb1", "Bb1", "scq", "shq")
        cos, sin = _rope_tables(cx, posb_d, 32, TMP[:].rearrange("p (a b) -> p a b", b=32), 'TMP',
                                YB[:, 3, :].rearrange("p (a b) -> p a b", b=32), ('YB', 3))

        w8_list = []
        wd_list = []

        def w8_src(blk_i):
            res = []
            res.append((wout_d, [(0, 512, 0)]))
            res.append((wout_d, [(512, 512, 0)]))
            for s in range(11):
                res.append((wgu_d, [(256 * s, 256, 0), (D_FF + 256 * s, 256, 256)]))
            for s in range(6):
                res.append((wkv_d, [(512 * s, 512, 0)]))
            for s in range(3):
                res.append((wq_d, [(512 * s, 512, 0)]))
            return res

        NBLK = nblk
        for b in range(NBLK):
            w8_list.extend(w8_src(b))
            for s in range(4):
                wd_list.append(s)
        w8_state = {'issued': 0}
        wd_state = {'issued': 0}

        def w8_issue(upto):
            while w8_state['issued'] <= upto and w8_state['issued'] < len(w8_list):
                i = w8_state['issued']
                src, parts = w8_list[i]
                buf = W8[i % 2]
                sv = src.rearrange("(c p) n -> p c n", p=128)
                for (lo, n, dlo) in parts:
                    p.dma('pool', buf[:, :, dlo:dlo + n], sv[:, :, lo:lo + n], writes=[('W8', i % 2)])
                w8_state['issued'] += 1

        def wd_issue(upto):
            while wd_state['issued'] <= upto and wd_state['issued'] < len(wd_list):
                i = wd_state['issued']
                s = wd_list[i]
                buf = WD[i % 2]
                sv = wdn_d.rearrange("(c p) n -> p c n", p=128)
                p.dma('pool', buf[:, :, :], sv[:, :, s * 256:(s + 1) * 256], writes=[('WD', i % 2)])
                wd_state['issued'] += 1

        w8_i = 0
        wd_i = 0

        def post_norm(blk_tiles_z, Gb, Bb, hs):
            pass

        for blk in range(NBLK):
            r0 = blk * 512
            xv = x_d[r0:r0 + 512, :].rearrange("(j p) d -> p j d", p=128)
            ygv = yg_d[r0:r0 + 512, :].rearrange("(j p) d -> p j d", p=128)
            for j in range(4):
                p.dma('sp', XA[:, j, :], xv[:, j, :], writes=[('XA', j)])
                p.dma('sp', YB[:, j, :], ygv[:, j, :], writes=[('YB', j)])
            for j in range(4):
                cx.copy('act' if j % 2 else 'dve', TB[:, j, :], YB[:, j, :], [('YB', j)], [('TB', j)])
            cx.to_featmajor(TB, [('TB', j) for j in range(4)], HT[0], 'HT0', 4, 8)
            for n in range(2):
                w8_issue(w8_i + 1)
                wb, wk = W8[w8_i % 2], ('W8', w8_i % 2)
                for j in range(4):
                    ps, pk = cx.psum()
                    for c in range(8):
                        p.op('pe', lambda e, c=c, j=j, ps=ps, wb=wb: e.matmul(ps[:], lhsT=HT[0][:, c, j * 128:(j + 1) * 128], rhs=wb[:, c, :],
                                                                             start=(c == 0), stop=(c == 7)),
                             reads=[('HT0', c), wk], writes=[pk])
                    sl = slice(n * 512, (n + 1) * 512)
                    p.op('dve', lambda e, j=j, ps=ps, sl=sl: e.tensor_tensor(out=YB[:, j, sl], in0=ps[:], in1=T["G1"][:, sl], op=ALU.mult),
                         reads=[pk, "G1"], writes=[('YB', j)])
                w8_i += 1
            for j in range(4):
                p.op('dve', lambda e, j=j: e.scalar_tensor_tensor(out=XA[:, j, :], in0=XA[:, j, :], scalar=ALPHA, in1=YB[:, j, :],
                                                                  op0=ALU.mult, op1=ALU.add),
                     reads=[('XA', j), ('YB', j)], writes=[('XA', j)])
                cx.layer_norm_tile(XA[:, j, :], ('XA', j), 'a')
                p.op('dve', lambda e, j=j: e.tensor_tensor(out=TMP[:], in0=XA[:, j, :], in1=T["G2"][:], op=ALU.mult),
                     reads=[('XA', j), "G2"], writes=['TMP'])
                p.op('dve', lambda e, j=j: e.tensor_tensor(out=TB[:, j, :], in0=TMP[:], in1=T["B2"][:], op=ALU.add),
                     reads=['TMP', "B2"], writes=[('TB', j)])
                p.op('dve', lambda e, j=j: e.tensor_tensor(out=XA[:, j, :], in0=XA[:, j, :], in1=T["Gb0"][:], op=ALU.mult),
                     reads=[('XA', j), "Gb0"], writes=[('XA', j)])
                p.op('dve', lambda e, j=j: e.tensor_tensor(out=XA[:, j, :], in0=XA[:, j, :], in1=T["Bb0"][:], op=ALU.add),
                     reads=[('XA', j), "Bb0"], writes=[('XA', j)])
            cx.to_featmajor(TB, [('TB', j) for j in range(4)], HT[1], 'HT1', 4, 8)
            for s in range(11):
                w8_issue(w8_i + 1)
                wb, wk = W8[w8_i % 2], ('W8', w8_i % 2)
                for fc in range(2):
                    psg, pkg = cx.psum()
                    for c in range(8):
                        p.op('pe', lambda e, c=c, fc=fc, psg=psg, wb=wb: e.matmul(psg[:], lhsT=wb[:, c, fc * 128:(fc + 1) * 128], rhs=HT[1][:, c, :],
                                                                                 start=(c == 0), stop=(c == 7)),
                             reads=[('HT1', c), wk], writes=[pkg])
                    psu, pku = cx.psum()
                    for c in range(8):
                        p.op('pe', lambda e, c=c, fc=fc, psu=psu, wb=wb: e.matmul(psu[:], lhsT=wb[:, c, 256 + fc * 128:256 + (fc + 1) * 128], rhs=HT[1][:, c, :],
                                                                                 start=(c == 0), stop=(c == 7)),
                             reads=[('HT1', c), wk], writes=[pku])
                    f = s * 2 + fc
                    sg, sgk = SG[f % 2], ('SG', f % 2)
                    p.op('act', lambda e, psg=psg, sg=sg: e.activation(out=sg[:], in_=psg[:], func=AF.Silu), reads=[pkg], writes=[sgk])
                    p.op('dve', lambda e, psu=psu, sg=sg, f=f: e.tensor_tensor(out=HID[:, f, :], in0=psu[:], in1=sg[:], op=ALU.mult),
                         reads=[pku, sgk], writes=[('HID', f)])
                w8_i += 1
            for n in range(4):
                wd_issue(wd_i + 1)
                wb, wk = WD[wd_i % 2], ('WD', wd_i % 2)
                for j in range(4):
                    ps, pk = cx.psum()
                    for f in range(22):
                        p.op('pe', lambda e, f=f, j=j, ps=ps, wb=wb: e.matmul(ps[:, 0:256], lhsT=HID[:, f, j * 128:(j + 1) * 128], rhs=wb[:, f, :],
                                                                             start=(f == 0), stop=(f == 21)),
                             reads=[('HID', f), wk], writes=[pk])
                    sl = slice(n * 256, (n + 1) * 256)
                    p.op('dve', lambda e, j=j, ps=ps, sl=sl: e.tensor_tensor(out=YB[:, j, sl], in0=ps[:, 0:256], in1=T["G3g"][:, sl], op=ALU.mult),
                         reads=[pk, "G3g"], writes=[('YB', j)])
                wd_i += 1
            x2v = x2_d[r0:r0 + 512, :].rearrange("(j p) d -> p j d", p=128)
            for which in range(2):
                G, B = ("G4", "B4") if which == 0 else ("G5", "B5")
                for j in range(4):
                    if which == 0:
                        p.op('dve', lambda e, j=j: e.scalar_tensor_tensor(out=XA[:, j, :], in0=XA[:, j, :], scalar=ALPHA, in1=YB[:, j, :],
                                                                          op0=ALU.mult, op1=ALU.add),
                             reads=[('XA', j), ('YB', j)], writes=[('XA', j)])
                        cx.layer_norm_tile(XA[:, j, :], ('XA', j), 'b')
                    p.op('dve', lambda e, j=j, G=G: e.tensor_tensor(out=TMP[:], in0=XA[:, j, :], in1=T[G][:], op=ALU.mult),
                         reads=[('XA', j), G], writes=['TMP'])
                    p.op('dve', lambda e, j=j, B=B: e.tensor_tensor(out=TB[:, j, :], in0=TMP[:], in1=T[B][:], op=ALU.add),
                         reads=['TMP', B], writes=[('TB', j)])
                    if which == 1:
                        p.op('dve', lambda e, j=j: e.tensor_tensor(out=YB[:, j, :], in0=XA[:, j, :], in1=T["Gb1"][:], op=ALU.mult),
                             reads=[('XA', j), "Gb1"], writes=[('YB', j)])
                        p.op('dve', lambda e, j=j: e.tensor_tensor(out=YB[:, j, :], in0=YB[:, j, :], in1=T["Bb1"][:], op=ALU.add),
                             reads=[('YB', j), "Bb1"], writes=[('YB', j)])
                        p.dma('sp', x2v[:, j, :], YB[:, j, :], reads=[('YB', j)])
                cx.to_featmajor(TB, [('TB', j) for j in range(4)], HT[which], 'HT%d' % which, 4, 8)
                nslab = 6 if which == 0 else 3
                for s in range(nslab):
                    w8_issue(w8_i + 1)
                    wb, wk = W8[w8_i % 2], ('W8', w8_i % 2)
                    if which == 0 and s < 3:
                        dst_d, ds, rope = k_d, s, True
                    elif which == 0:
                        dst_d, ds, rope = v_d, s - 3, False
                    else:
                        dst_d, ds, rope = q_d, s, True
                    for j in range(4):
                        ps, pk = cx.psum()
                        for c in range(8):
                            p.op('pe', lambda e, c=c, j=j, ps=ps, wb=wb, which=which: e.matmul(ps[:], lhsT=HT[which][:, c, j * 128:(j + 1) * 128], rhs=wb[:, c, :],
                                                                                              start=(c == 0), stop=(c == 7)),
                                 reads=[('HT%d' % which, c), wk], writes=[pk])
                        oap = OUTB[:, j, ds * 512:(ds + 1) * 512]
                        ok = ('OUTB', j, ds)
                        if rope:
                            tile = blk * 4 + j
                            _rope_apply(cx, ps[:], pk, oap, ok, cos[:, tile, :], sin[:, tile, :], 8, RT)
                        else:
                            cx.copy('act', oap, ps[:], [pk], [ok])
                    w8_i += 1
                    if ds == 2:
                        dv = dst_d[r0:r0 + 512, :].rearrange("(j p) d -> p j d", p=128)
                        for j in range(4):
                            p.dma('sp', dv[:, j, :], OUTB[:, j, :], reads=[('OUTB', j, 0), ('OUTB', j, 1), ('OUTB', j, 2)])
        p.finish()
        p.emit()
    return nc


def _cT(c_row):
    return np.ascontiguousarray(c_row.reshape(8, 128).T)


def _ada_blocks(ada_w, ada_b, idxs):
    w = np.ascontiguousarray(np.stack([ada_w[:, i * D:(i + 1) * D] for i in idxs]))
    b = np.ascontiguousarray(np.stack([ada_b[i * D:(i + 1) * D] for i in idxs]))
    return w, b


def maps_B(inp, yg):
    idxs = [2, 3, 4, 5, 12, 13, 6, 7]
    aw, ab = _ada_blocks(inp['ada_w'], inp['ada_b'], idxs)
    maps = []
    for c in range(NCORE):
        b, s0 = c // 4, (c % 4) * TPC
        maps.append({
            "x": np.ascontiguousarray(inp['x'][b, s0:s0 + TPC]),
            "yg": np.ascontiguousarray(yg[b, s0:s0 + TPC]),
            "cT": _cT(inp['c'][b]),
            "adaw": aw, "adab": ab,
            "lng": np.ascontiguousarray(inp['ln_g'][0]), "lnb": np.ascontiguousarray(inp['ln_b'][0]),
            "w_out": inp['rw_w_out'][0], "w_gu": inp['ffn_w_gu'][0], "w_down": inp['ffn_w_down'][0],
            "w_kv": inp['kv_w'], "w_q": inp['attn_w_q'][0],
            "posb": np.full((128, 1), float(s0), np.float32),
        })
    return maps


CH = 128
DECAY_SCALE = -math.exp(-0.5)
LNX_EPS = 64e-5


def build_A(nblk=SEQ // 512):
    nc = bass.Bass("TRN2", target_bir_lowering=False)
    dt = lambda name, shape, dtype, kind: nc.dram_tensor(name, shape, dtype, kind=kind).ap()
    xp_d = dt("xpad", [SEQ + 1, D], F32, "ExternalInput")
    cT_d = dt("cT", [128, 8], F32, "ExternalInput")
    adaw_d = dt("adaw", [2, D, D], F32, "ExternalInput")
    adab_d = dt("adab", [2, D], F32, "ExternalInput")
    muT_d = dt("muT", [128, 6, 8], F32, "ExternalInput")
    wrkv_d = dt("w_rkv", [D, 768], F32, "ExternalInput")
    wl1_d = dt("w_l1", [D, 288], F32, "ExternalInput")
    w2a_d = dt("w2a", [128, 256], F32, "ExternalInput")
    g2_d = dt("g2", [160, 256], F32, "ExternalInput")
    chv_d = dt("chv", [128, 2, 6], F32, "ExternalInput")
    lnx_d = dt("lnx", [2, 256], F32, "ExternalInput")
    yg_d = dt("yg", [SEQ, 256], F32, "ExternalOutput")

    with ExitStack() as st:
        cx = Ctx(nc, st)
        p = cx.p
        sb = cx.sb
        ident = cx.ident
        X = sb("X", [128, 4, D], F32)
        XP = sb("XP", [128, 4, D], F32)
        HB = sb("HB", [128, 4, D], BF16)
        XXB = sb("XXB", [128, 4, D], BF16)
        hT = sb("hT", [128, 8, 512], BF16)
        xxT = sb("xxT", [128, 8, 512], BF16)
        SC1 = sb("SC1", [128, D], F32)
        SH = sb("SH", [128, D], F32)
        W = sb("W", [128, 8, 1056], BF16)
        WM = sb("WM", [128, 8, 1056], BF16)
        muT = sb("muT_s", [128, 6, 8], F32)
        W2A = sb("W2A", [128, 256], BF16)
        G2a = sb("G2a", [128, 256], BF16)
        G2b = sb("G2b", [32, 256], BF16)
        chv = sb("chv_s", [128, 2, 6], F32)
        LNXG = sb("LNXG", [128, 256], F32)
        LNXB = sb("LNXB", [128, 256], F32)
        identf = sb("identf", [128, 128], F32)
        M1 = sb("M1", [128, 256], BF16)
        ML = sb("ML", [128, 128], BF16)
        RMASK = sb("RMASK", [128, 512], BF16)
        HSEL = sb("HSEL", [128, 2], F32)
        BONES = sb("BONES", [128, 128], F32)
        S = sb("S", [64, 4, 64], F32)
        Wbuf = hT
        WST = XP[:].rearrange("p a b -> p (a b)")[:, 0:1056]
        p.op('dve', lambda e: e.tensor_copy(out=identf[:], in_=ident[:]), reads=['ident'], writes=['identf'])
        p.op('pool', lambda e: e.memset(M1[:], 1.0), writes=['M1'])
        p.op('pool', lambda e: e.affine_select(out=M1[:, 0:128], in_=M1[:, 0:128], pattern=[[1, 128]], compare_op=ALU.is_gt, fill=0.0,
                                               base=0, channel_multiplier=-1), reads=['M1'], writes=['M1'])
        p.op('pool', lambda e: e.affine_select(out=M1[:, 128:256], in_=M1[:, 128:256], pattern=[[1, 128]], compare_op=ALU.is_ge, fill=0.0,
                                               base=0, channel_multiplier=-1), reads=['M1'], writes=['M1'])
        p.op('pool', lambda e: e.memset(ML[:], 1.0), writes=['ML'])
        p.op('pool', lambda e: e.affine_select(out=ML[:], in_=ML[:], pattern=[[-1, 128]], compare_op=ALU.is_gt, fill=0.0,
                                               base=0, channel_multiplier=1), reads=['ML'], writes=['ML'])
        p.op('pool', lambda e: e.memset(RMASK[:], 1.0), writes=['RMASK'])
        for q in range(4):
            p.op('pool', lambda e, q=q: e.memset(RMASK[:, q * 128:q * 128 + 1], 0.0), reads=['RMASK'], writes=['RMASK'])
        p.op('pool', lambda e: e.memset(HSEL[:], 0.0), writes=['HSEL'])
        p.op('pool', lambda e: e.memset(HSEL[0:64, 0:1], 1.0), reads=['HSEL'], writes=['HSEL'])
        p.op('pool', lambda e: e.memset(HSEL[64:128, 1:2], 1.0), reads=['HSEL'], writes=['HSEL'])
        p.op('pool', lambda e: e.memset(BONES[:], 0.0), writes=['BONES'])
        p.op('pool', lambda e: e.memset(BONES[0:64, 0:64], 1.0), reads=['BONES'], writes=['BONES'])
        p.op('pool', lambda e: e.memset(BONES[64:128, 64:128], 1.0), reads=['BONES'], writes=['BONES'])
        p.op('pool', lambda e: e.memset(S[:], 0.0), writes=[('S', h) for h in range(4)])
        p.dma('sp', muT[:], muT_d, writes=['muT'])
        p.dma('sp', chv[:], chv_d, writes=['chv'])
        p.op('dve', lambda e: e.tensor_scalar(out=chv[:, :, 5], in0=chv[:, :, 3], scalar1=-1.0, scalar2=1.0, op0=ALU.mult, op1=ALU.add),
             reads=['chv'], writes=['chv'])
        cx.bcast_row(LNXG[:], 'LNXG', lnx_d[0:1, :])
        cx.bcast_row(LNXB[:], 'LNXB', lnx_d[1:2, :])
        p.dma('pool', W2A[:], w2a_d, writes=['W2A'])
        p.dma('pool', G2a[:], g2_d[0:128, :], writes=['G2a'])
        p.dma('pool', G2b[:], g2_d[128:160, :], writes=['G2b'])
        scB = _silu_c_bcast(cx, cT_d)
        _mod_tile(cx, scB, SH[:], 'SH', adaw_d[0], adab_d[0:1, :], Wbuf, 'Wbuf', X[:, 0, :], ('X', 0), False)
        _mod_tile(cx, scB, SC1[:], 'SC1', adaw_d[1], adab_d[1:2, :], Wbuf, 'Wbuf', X[:, 1, :], ('X', 1), True)
        wr = wrkv_d.rearrange("(c p) n -> p c n", p=128)
        wl = wl1_d.rearrange("(c p) n -> p c n", p=128)
        groups = [(0, 256, 0), (256, 512, 1), (512, 768, 2), (768, 832, 3), (832, 896, 4), (896, 1056, 5)]
        for c in range(8):
            p.dma('sp', WST[:, 0:768], wr[:, c, :], writes=['WST'])
            p.dma('sp', WST[:, 768:1056], wl[:, c, :], writes=['WST'])
            p.op('act', lambda e, c=c: e.copy(out=W[:, c, :], in_=WST), reads=['WST'], writes=['W'])
            for (lo, hi, mi) in groups:
                p.op('dve', lambda e, c=c, lo=lo, hi=hi, mi=mi: e.tensor_scalar(out=WM[:, c, lo:hi], in0=WST[:, lo:hi], scalar1=muT[:, mi, c:c + 1],
                                                                                scalar2=None, op0=ALU.mult),
                     reads=['WST', 'muT'], writes=['WM'])

        p.barrier()
        fnames = ["Rf", "Kf", "Af", "LW", "CS", "E1", "E2", "E3", "E4", "KK", "KM", "Bf", "T1"]
        Fm = {n: sb("F_" + n, [128, 512], F32) for n in fnames}
        PROD = [sb("PROD%d" % m, [128, 512], F32) for m in range(2)]
        bnames = ["RT", "KT", "BT", "AT", "KH", "BH", "VT"]
        Bm = [{n: sb("B_%s%d" % (n, m), [128, 512], BF16) for n in bnames} for m in range(2)]
        LA = sb("LA", [128, 512], BF16)
        GS = sb("GS", [128, 2, 512], BF16)
        Gtok = sb("Gtok", [128, 4, 256], F32)
        TMs2 = [[sb("TMs%d_%d" % (m, i), [128, 512], BF16) for i in range(2)] for m in range(2)]
        G1s_ = [sb("G1s%d" % h, [128, 256], BF16) for h in range(8)]
        G2s_ = [sb("G2s%d" % h, [128, 256], BF16) for h in range(8)]
        Lb_ = [[sb("Lb%d_%d" % (h, i), [128, 128], BF16) for i in range(2)] for h in range(8)]
        Gb_ = [[sb("Gb%d_%d" % (h, i), [128, 128], BF16) for i in range(2)] for h in range(8)]
        TTb_ = [[sb("TTb%d_%d" % (h, i), [128, 128], BF16) for i in range(2)] for h in range(8)]
        Xs_ = [sb("Xs%d" % h, [128, 64], BF16) for h in range(8)]
        WU_ = [sb("WU%d" % h, [128, 128], BF16) for h in range(8)]
        DP_ = [sb("DP%d" % h, [128, 64], F32) for h in range(8)]
        MT_ = [sb("MT%d" % h, [64, 64], F32) for h in range(8)]
        Ns_ = [sb("Ns%d" % h, [64, 64], F32) for h in range(8)]
        QT_ = [sb("QT%d" % h, [64, 128], F32) for h in range(8)]
        gst_ = [sb("gst%d" % h, [128, 6], F32) for h in range(8)]
        gmv_ = [sb("gmv%d" % h, [128, 2], F32) for h in range(8)]
        grs_ = [sb("grs%d" % h, [128, 1], F32) for h in range(8)]
        gnm_ = [sb("gnm%d" % h, [128, 1], F32) for h in range(8)]
        YO2 = [sb("YO%d" % i, [128, 256], F32) for i in range(2)]
        SBON2 = [sb("SBON%d" % i, [128, 4], F32) for i in range(2)]
        PCs = [sb("PCs%d" % m, [128, 4], F32) for m in range(2)]
        cx.lnxeps = sb("lnxeps", [128, 1], F32)
        p.op('pool', lambda e: e.memset(cx.lnxeps[:], LNX_EPS), writes=['lnxeps'])

        def proj_fm(ps, pk, lo, n):
            for c in range(8):
                p.op('pe', lambda e, c=c: e.matmul(ps[0:n, :], lhsT=W[:, c, lo:lo + n], rhs=hT[:, c, :], start=(c == 0), stop=False),
                     reads=[('hT', c), 'W'], writes=[pk])
            for c in range(8):
                p.op('pe', lambda e, c=c: e.matmul(ps[0:n, :], lhsT=WM[:, c, lo:lo + n], rhs=xxT[:, c, :], start=False, stop=(c == 7)),
                     reads=[('xxT', c), 'WM'], writes=[pk])


        def head_chain(m, hl, q, cs_):
            B_ = Bm[m]
            bk = lambda n: ('B', n, m)
            hh = 2 * m + hl
            P = slice(64 * hl, 64 * hl + 64)
            TMq = TMs2[m][q % 2]
            YO = YO2[q % 2]
            SBON = SBON2[q % 2]
            At_h = TMq[:, 64 * hl:64 * hl + 64]
            Bh_h = TMq[:, 128 + 64 * hl:128 + 64 * hl + 64]
            Kh_h = TMq[:, 256 + 64 * hl:256 + 64 * hl + 64]
            V_h = TMq[:, 384 + 64 * hl:384 + 64 * hl + 64]
            tmk = ('TMs', m, q % 2)
            hb = hh + 4 * (q % 2)
            G1s, G2s, Lb, Gb, TTb, Xs, WU, DP, MT, Ns, QT = G1s_[hb], G2s_[hb], Lb_[hb], Gb_[hb], TTb_[hb], Xs_[hb], WU_[hb], DP_[hb], MT_[hb], Ns_[hb], QT_[hb]
            gst, gmv, grs, gnm = gst_[hb], gmv_[hb], grs_[hb], gnm_[hb]
            K_ = lambda n: (n, hb)
            ps, pk = cx.psum()
            p.op('pe', lambda e, ps=ps: e.matmul(ps[:, 0:128], lhsT=B_["BT"][P, cs_], rhs=B_["AT"][P, cs_], start=True, stop=True), reads=[bk("BT"), bk("AT")], writes=[pk])
            p.op('pe', lambda e, ps=ps: e.matmul(ps[:, 128:256], lhsT=B_["BT"][P, cs_], rhs=B_["RT"][P, cs_], start=True, stop=True), reads=[bk("BT"), bk("RT")], writes=[pk])
            p.op('dve', lambda e, ps=ps: e.tensor_tensor(out=G1s[:], in0=ps[:, 0:256], in1=M1[:], op=ALU.mult), reads=[pk, 'M1'], writes=[K_('G1s')])
            ps, pk = cx.psum()
            p.op('pe', lambda e, ps=ps: e.matmul(ps[:, 0:128], lhsT=B_["KT"][P, cs_], rhs=B_["AT"][P, cs_], start=True, stop=True), reads=[bk("KT"), bk("AT")], writes=[pk])
            p.op('pe', lambda e, ps=ps: e.matmul(ps[:, 128:256], lhsT=B_["KT"][P, cs_], rhs=B_["RT"][P, cs_], start=True, stop=True), reads=[bk("KT"), bk("RT")], writes=[pk])
            p.op('dve', lambda e, ps=ps: e.tensor_tensor(out=G2s[:], in0=ps[:, 0:256], in1=M1[:], op=ALU.mult), reads=[pk, 'M1'], writes=[K_('G2s')])
            ps, pk = cx.psum()
            p.op('pe', lambda e, ps=ps: e.matmul(ps[:, 0:128], lhsT=B_["AT"][P, cs_], rhs=B_["BT"][P, cs_], start=True, stop=True), reads=[bk("BT"), bk("AT")], writes=[pk])
            p.op('dve', lambda e, ps=ps: e.tensor_tensor(out=Lb[0][:], in0=ps[:, 0:128], in1=ML[:], op=ALU.mult), reads=[pk, 'ML'], writes=[K_('Lb0')])
            yield
            p.op('act', lambda e: e.copy(out=Gb[0][:], in_=G1s[:, 0:128]), reads=[K_('G1s')], writes=[K_('Gb0')])
            p.op('dve', lambda e: e.tensor_tensor(out=TTb[0][:], in0=G1s[:, 0:128], in1=ident[:], op=ALU.add), reads=[K_('G1s'), 'ident'], writes=[K_('TTb0')])
            ps, pk = cx.psum()
            p.op('pe', lambda e, ps=ps: e.matmul(ps[:, 0:64], lhsT=G2s[:, 0:128], rhs=V_h, start=True, stop=True), reads=[K_('G2s'), tmk], writes=[pk])
            p.op('act', lambda e, ps=ps: e.copy(out=Xs[:], in_=ps[:, 0:64]), reads=[pk], writes=[K_('Xs')])
            yield
            cur = 0
            for lvl in range(1, 7):
                nxt = 1 - cur
                ps, pk = cx.psum()
                p.op('pe', lambda e, ps=ps, cur=cur: e.matmul(ps[:, 0:128], lhsT=Gb[cur][:], rhs=Lb[cur][:], start=True, stop=True), reads=[K_('Gb%d' % cur), K_('Lb%d' % cur)], writes=[pk])
                p.op('act', lambda e, ps=ps, nxt=nxt: e.copy(out=Lb[nxt][:], in_=ps[:, 0:128]), reads=[pk], writes=[K_('Lb%d' % nxt)])
                if lvl < 6:
                    ps2, pk2 = cx.psum()
                    p.op('pe', lambda e, ps2=ps2, cur=cur: e.matmul(ps2[:, 0:128], lhsT=Lb[cur][:], rhs=Gb[cur][:], start=True, stop=True), reads=[K_('Gb%d' % cur), K_('Lb%d' % cur)], writes=[pk2])
                    p.op('dve', lambda e, ps2=ps2, nxt=nxt: e.tensor_copy(out=Gb[nxt][:], in_=ps2[:, 0:128]), reads=[pk2], writes=[K_('Gb%d' % nxt)])
                yield
                ps3, pk3 = cx.psum()
                p.op('pe', lambda e, ps3=ps3, cur=cur, nxt=nxt: e.matmul(ps3[:, 0:128], lhsT=Lb[nxt][:], rhs=TTb[cur][:], start=True, stop=True), reads=[K_('Lb%d' % nxt), K_('TTb%d' % cur)], writes=[pk3])
                p.op('dve', lambda e, ps3=ps3, cur=cur, nxt=nxt: e.tensor_tensor(out=TTb[nxt][:], in0=ps3[:, 0:128], in1=TTb[cur][:], op=ALU.add), reads=[pk3, K_('TTb%d' % cur)], writes=[K_('TTb%d' % nxt)])
                cur = nxt
                yield
            TT = TTb[cur]
            ttk = K_('TTb%d' % cur)
            ps, pk = cx.psum()
            p.op('pe', lambda e, ps=ps: e.matmul(ps[:, 0:64], lhsT=TT[:], rhs=At_h, start=True, stop=True), reads=[ttk, tmk], writes=[pk])
            p.op('pe', lambda e, ps=ps: e.matmul(ps[:, 64:128], lhsT=TT[:], rhs=Xs[:], start=True, stop=True), reads=[ttk, K_('Xs')], writes=[pk])
            p.op('dve', lambda e, ps=ps: e.tensor_copy(out=WU[:], in_=ps[:, 0:128]), reads=[pk], writes=[K_('WU')])
            p.op('dve', lambda e: e.tensor_scalar(out=DP[P, :], in0=identf[P, 64 * hl:64 * hl + 64], scalar1=PCs[m][P, q:q + 1], scalar2=None, op0=ALU.mult),
                 reads=['identf', ('PC', m)], writes=[K_('DP')])
            yield
            ps, pk = cx.psum()
            p.op('pe', lambda e, ps=ps: e.matmul(ps[0:64, 0:64], lhsT=WU[:, 0:64], rhs=Bh_h, start=True, stop=True), reads=[K_('WU'), tmk], writes=[pk])
            p.op('pe', lambda e, ps=ps: e.matmul(ps[0:64, 64:128], lhsT=identf[P, 64 * hl:64 * hl + 64], rhs=DP[P, :], start=True, stop=True), reads=['identf', K_('DP')], writes=[pk])
            p.op('act', lambda e, ps=ps: e.copy(out=MT[:], in_=ps[0:64, 0:64]), reads=[pk], writes=[K_('MT')])
            p.op('dve', lambda e, ps=ps: e.tensor_tensor(out=MT[:], in0=ps[0:64, 64:128], in1=MT[:], op=ALU.add), reads=[pk, K_('MT')], writes=[K_('MT')])
            ps, pk = cx.psum()
            p.op('pe', lambda e, ps=ps: e.matmul(ps[0:64, 0:64], lhsT=Bh_h, rhs=WU[:, 64:128], start=True, stop=False), reads=[K_('WU'), tmk], writes=[pk])
            p.op('pe', lambda e, ps=ps: e.matmul(ps[0:64, 0:64], lhsT=Kh_h, rhs=V_h, start=False, stop=True), reads=[tmk], writes=[pk])
            p.op('act', lambda e, ps=ps: e.copy(out=Ns[:], in_=ps[0:64, 0:64]), reads=[pk], writes=[K_('Ns')])
            ps, pk = cx.psum()
            p.op('pe', lambda e, ps=ps: e.matmul(ps[0:64, 0:128], lhsT=ident[:, 64 * hl:64 * hl + 64], rhs=B_["RT"][:, cs_], start=True, stop=False), reads=['ident', bk("RT")], writes=[pk])
            p.op('pe', lambda e, ps=ps: e.matmul(ps[0:64, 0:128], lhsT=WU[:, 0:64], rhs=G1s[:, 128:256], start=False, stop=True), reads=[K_('WU'), K_('G1s')], writes=[pk])
            p.op('act', lambda e, ps=ps: e.copy(out=QT[:], in_=ps[0:64, 0:128]), reads=[pk], writes=[K_('QT')])
            yield
            ps, pk = cx.psum()
            p.op('pe', lambda e, ps=ps: e.matmul(ps[:, 0:64], lhsT=G1s[:, 128:256], rhs=WU[:, 64:128], start=True, stop=False), reads=[K_('G1s'), K_('WU')], writes=[pk])
            p.op('pe', lambda e, ps=ps: e.matmul(ps[:, 0:64], lhsT=G2s[:, 128:256], rhs=V_h, start=False, stop=False), reads=[K_('G2s'), tmk], writes=[pk])
            p.op('pe', lambda e, ps=ps: e.matmul(ps[:, 0:64], lhsT=QT[:], rhs=S[:, hh, :], start=False, stop=True), reads=[K_('QT'), ('S', hh)], writes=[pk])
            yo = YO[:, hh * 64:(hh + 1) * 64]
            yk = ('YO', hh, q % 2)
            p.op('act', lambda e, ps=ps: e.copy(out=yo, in_=ps[:, 0:64]), reads=[pk], writes=[yk])
            ps, pk = cx.psum()
            p.op('pe', lambda e, ps=ps: e.matmul(ps[0:64, 0:64], lhsT=MT[:], rhs=S[:, hh, :], start=True, stop=True), reads=[K_('MT'), ('S', hh)], writes=[pk])
            p.op('dve', lambda e, ps=ps: e.tensor_tensor(out=S[:, hh, :], in0=ps[0:64, 0:64], in1=Ns[:], op=ALU.add), reads=[pk, K_('Ns')], writes=[('S', hh)])
            yield
            p.op('dve', lambda e: e.bn_stats(out=gst[:], in_=yo), reads=[yk], writes=[K_('gst')])
            p.op('dve', lambda e: e.bn_aggr(out=gmv[:], in_=gst[:]), reads=[K_('gst')], writes=[K_('gmv')])
            p.op('act', lambda e: e.activation(out=grs[:], in_=gmv[:, 1:2], func=AF.Sqrt, bias=cx.lnxeps[:], scale=1.0), reads=[K_('gmv'), 'lnxeps'], writes=[K_('grs')])
            yield
            p.op('dve', lambda e: e.reciprocal(out=grs[:], in_=grs[:]), reads=[K_('grs')], writes=[K_('grs')])
            p.op('dve', lambda e: e.tensor_scalar(out=gnm[:], in0=gmv[:, 0:1], scalar1=grs[:], scalar2=-1.0, op0=ALU.mult, op1=ALU.mult), reads=[K_('gmv'), K_('grs')], writes=[K_('gnm')])
            p.op('act', lambda e: e.activation(out=yo, in_=yo, func=AF.Identity, bias=gnm[:], scale=grs[:]), reads=[yk, K_('grs'), K_('gnm')], writes=[yk])
            yield
            sl = slice(hh * 64, (hh + 1) * 64)
            p.op('dve', lambda e: e.tensor_tensor(out=yo, in0=yo, in1=LNXG[:, sl], op=ALU.mult), reads=[yk, 'LNXG'], writes=[yk])
            p.op('dve', lambda e: e.tensor_tensor(out=yo, in0=yo, in1=LNXB[:, sl], op=ALU.add), reads=[yk, 'LNXB'], writes=[yk])
            p.op('dve', lambda e: e.scalar_tensor_tensor(out=yo, in0=V_h, scalar=SBON[:, hh:hh + 1], in1=yo, op0=ALU.mult, op1=ALU.add),
                 reads=[yk, tmk, ('SBON', m, q % 2)], writes=[yk])

        for blk in range(nblk):
            r0 = blk * 512
            xv = xp_d[r0 + 1:r0 + 513, :].rearrange("(j p) d -> p j d", p=128)
            xpv = xp_d[r0:r0 + 512, :].rearrange("(j p) d -> p j d", p=128)
            for j in range(4):
                p.dma('sp', X[:, j, :], xv[:, j, :], writes=[('X', j)])
                p.dma('sp', XP[:, j, :], xpv[:, j, :], writes=[('XP', j)])
            for j in range(4):
                p.op('dve', lambda e, j=j: e.tensor_tensor(out=X[:, j, :], in0=X[:, j, :], in1=SC1[:], op=ALU.mult), reads=[('X', j), 'SC1'], writes=[('X', j)])
                p.op('pool', lambda e, j=j: e.tensor_tensor(out=XP[:, j, :], in0=XP[:, j, :], in1=SC1[:], op=ALU.mult), reads=[('XP', j), 'SC1'], writes=[('XP', j)])
                p.op('dve', lambda e, j=j: e.tensor_tensor(out=HB[:, j, :], in0=X[:, j, :], in1=SH[:], op=ALU.add), reads=[('X', j), 'SH'], writes=[('HB', j)])
                p.op('pool', lambda e, j=j: e.tensor_tensor(out=XXB[:, j, :], in0=XP[:, j, :], in1=X[:, j, :], op=ALU.subtract), reads=[('XP', j), ('X', j)], writes=[('XXB', j)])
            if blk == 0:
                p.op('dve', lambda e: e.tensor_scalar(out=XXB[0:1, 0, :], in0=HB[0:1, 0, :], scalar1=-1.0, scalar2=None, op0=ALU.mult),
                     reads=[('HB', 0), ('XXB', 0)], writes=[('XXB', 0)])
            cx.to_featmajor(HB, [('HB', j) for j in range(4)], hT, 'hT', 4, 8)
            cx.to_featmajor(XXB, [('XXB', j) for j in range(4)], xxT, 'xxT', 4, 8)
            ps, pk = cx.psum()
            proj_fm(ps, pk, 768, 128)
            p.op('act', lambda e, ps=ps: e.activation(out=LA[0:64, :], in_=ps[0:64, :], func=AF.Tanh), reads=[pk], writes=['LA'])
            p.op('act', lambda e, ps=ps: e.copy(out=LA[64:128, :], in_=ps[64:128, :]), reads=[pk], writes=['LA'])
            ps, pk = cx.psum()
            proj_fm(ps, pk, 896, 128)
            p.op('act', lambda e, ps=ps: e.activation(out=GS[:, 0, :], in_=ps[:], func=AF.Sigmoid), reads=[pk], writes=['GS'])
            ps, pk = cx.psum()
            proj_fm(ps, pk, 1024, 32)
            p.op('act', lambda e, ps=ps: e.activation(out=GS[0:32, 1, :], in_=ps[0:32, :], func=AF.Sigmoid), reads=[pk], writes=['GS'])
            for j in range(4):
                ps, pk = cx.psum()
                p.op('pe', lambda e, j=j, ps=ps: e.matmul(ps[:, 0:256], lhsT=GS[:, 0, j * 128:(j + 1) * 128], rhs=G2a[:], start=True, stop=False),
                     reads=['GS', 'G2a'], writes=[pk])
                p.op('pe', lambda e, j=j, ps=ps: e.matmul(ps[:, 0:256], lhsT=GS[0:32, 1, j * 128:(j + 1) * 128], rhs=G2b[:], start=False, stop=True),
                     reads=['GS', 'G2b'], writes=[pk])
                p.op('act', lambda e, j=j, ps=ps: e.copy(out=Gtok[:, j, :], in_=ps[:, 0:256]), reads=[pk], writes=[('Gtok', j)])
            for m in range(2):
                B_ = Bm[m]
                bk = lambda n, m=m: ('B', n, m)
                ps, pk = cx.psum()
                proj_fm(ps, pk, m * 128, 128)
                p.op('act', lambda e, ps=ps: e.copy(out=Fm["Rf"][:], in_=ps[:]), reads=[pk], writes=['Rf'])
                ps, pk = cx.psum()
                proj_fm(ps, pk, 256 + m * 128, 128)
                p.op('act', lambda e, ps=ps: e.copy(out=Fm["Kf"][:], in_=ps[:]), reads=[pk], writes=['Kf'])
                ps, pk = cx.psum()
                proj_fm(ps, pk, 512 + m * 128, 128)
                p.op('act', lambda e, ps=ps, B_=B_: e.copy(out=B_["VT"][:], in_=ps[:]), reads=[pk], writes=[bk("VT")])
                ps, pk = cx.psum()
                p.op('pe', lambda e, ps=ps, m=m: e.matmul(ps[:], lhsT=W2A[0:64, m * 128:(m + 1) * 128], rhs=LA[0:64, :], start=True, stop=True),
                     reads=['W2A', 'LA'], writes=[pk])
                p.op('act', lambda e, ps=ps, m=m: e.activation(out=Fm["LW"][:], in_=ps[:], func=AF.Sigmoid, bias=chv[:, m, 0:1], scale=1.0),
                     reads=[pk, 'chv'], writes=['LW'])
                ps, pk = cx.psum()
                p.op('pe', lambda e, ps=ps, m=m: e.matmul(ps[:], lhsT=W2A[64:128, m * 128:(m + 1) * 128], rhs=LA[64:128, :], start=True, stop=True),
                     reads=['W2A', 'LA'], writes=[pk])
                p.op('act', lambda e, ps=ps, m=m: e.activation(out=Fm["Af"][:], in_=ps[:], func=AF.Sigmoid, bias=chv[:, m, 1:2], scale=1.0),
                     reads=[pk, 'chv'], writes=['Af'])
                F_ = Fm
                p.op('dve', lambda e: e.tensor_scalar(out=F_["LW"][:], in0=F_["LW"][:], scalar1=DECAY_SCALE, scalar2=None, op0=ALU.mult), reads=['LW'], writes=['LW'])
                p.op('dve', lambda e: e.tensor_tensor_scan(out=F_["CS"][:], data0=RMASK[:], data1=F_["LW"][:], initial=0.0, op0=ALU.mult, op1=ALU.add),
                     reads=['LW', 'RMASK'], writes=['CS'])
                p.op('act', lambda e: e.activation(out=F_["E1"][:], in_=F_["CS"][:], func=AF.Exp), reads=['CS'], writes=['E1'])
                p.op('act', lambda e: e.activation(out=F_["E2"][:], in_=F_["CS"][:], func=AF.Exp, scale=-1.0), reads=['CS'], writes=['E2'])
                p.op('pool', lambda e: e.tensor_tensor(out=F_["E3"][:], in0=F_["CS"][:], in1=F_["LW"][:], op=ALU.subtract), reads=['CS', 'LW'], writes=['E3'])
                p.op('act', lambda e: e.activation(out=F_["E3"][:], in_=F_["E3"][:], func=AF.Exp), reads=['E3'], writes=['E3'])
                for q in range(4):
                    p.op('dve', lambda e, q=q: e.tensor_scalar(out=F_["E4"][:, q * 128:(q + 1) * 128], in0=F_["E2"][:, q * 128:(q + 1) * 128],
                                                               scalar1=F_["E1"][:, q * 128 + 127:q * 128 + 128], scalar2=None, op0=ALU.mult),
                         reads=['E1', 'E2'], writes=['E4'])
                p.op('dve', lambda e, m=m: e.tensor_scalar(out=F_["KK"][:], in0=F_["Kf"][:], scalar1=chv[:, m, 2:3], scalar2=None, op0=ALU.mult), reads=['Kf', 'chv'], writes=['KK'])
                p.op('pool', lambda e: e.tensor_tensor(out=F_["T1"][:], in0=F_["KK"][:], in1=F_["KK"][:], op=ALU.mult), reads=['KK'], writes=['T1'])
                ps, pk = cx.psum()
                p.op('pe', lambda e, ps=ps: e.matmul(ps[:], lhsT=BONES[:], rhs=F_["T1"][:], start=True, stop=True), reads=['BONES', 'T1'], writes=[pk])
                p.op('act', lambda e, ps=ps: e.activation(out=F_["T1"][:], in_=ps[:], func=AF.Sqrt), reads=[pk], writes=['T1'])
                p.op('dve', lambda e: e.tensor_scalar(out=F_["T1"][:], in0=F_["T1"][:], scalar1=1e-12, scalar2=None, op0=ALU.max), reads=['T1'], writes=['T1'])
                p.op('dve', lambda e: e.reciprocal(out=F_["T1"][:], in_=F_["T1"][:]), reads=['T1'], writes=['T1'])
                p.op('dve', lambda e: e.tensor_tensor(out=F_["KK"][:], in0=F_["KK"][:], in1=F_["T1"][:], op=ALU.mult), reads=['KK', 'T1'], writes=['KK'])
                p.op('dve', lambda e, m=m: e.tensor_scalar(out=F_["T1"][:], in0=F_["Af"][:], scalar1=chv[:, m, 3:4], scalar2=chv[:, m, 5:6], op0=ALU.mult, op1=ALU.add),
                     reads=['Af', 'chv'], writes=['T1'])
                p.op('pool', lambda e: e.tensor_tensor(out=F_["KM"][:], in0=F_["Kf"][:], in1=F_["T1"][:], op=ALU.mult), reads=['Kf', 'T1'], writes=['KM'])
                p.op('pool', lambda e: e.tensor_tensor(out=F_["Bf"][:], in0=F_["KK"][:], in1=F_["Af"][:], op=ALU.mult), reads=['KK', 'Af'], writes=['Bf'])
                p.op('dve', lambda e, B_=B_: e.tensor_tensor(out=B_["RT"][:], in0=F_["Rf"][:], in1=F_["E1"][:], op=ALU.mult), reads=['Rf', 'E1'], writes=[bk("RT")])
                p.op('pool', lambda e, B_=B_: e.tensor_tensor(out=B_["KT"][:], in0=F_["KM"][:], in1=F_["E2"][:], op=ALU.mult), reads=['KM', 'E2'], writes=[bk("KT")])
                p.op('dve', lambda e, B_=B_: e.tensor_tensor(out=B_["BT"][:], in0=F_["Bf"][:], in1=F_["E2"][:], op=ALU.mult), reads=['Bf', 'E2'], writes=[bk("BT")])
                p.op('dve', lambda e, B_=B_: e.scalar_tensor_tensor(out=B_["AT"][:], in0=F_["KK"][:], scalar=-1.0, in1=F_["E3"][:], op0=ALU.mult, op1=ALU.mult),
                     reads=['KK', 'E3'], writes=[bk("AT")])
                p.op('pool', lambda e, B_=B_: e.tensor_tensor(out=B_["KH"][:], in0=F_["KM"][:], in1=F_["E4"][:], op=ALU.mult), reads=['KM', 'E4'], writes=[bk("KH")])
                p.op('dve', lambda e, B_=B_: e.tensor_tensor(out=B_["BH"][:], in0=F_["Bf"][:], in1=F_["E4"][:], op=ALU.mult), reads=['Bf', 'E4'], writes=[bk("BH")])
                p.op('dve', lambda e, m=m: e.scalar_tensor_tensor(out=PROD[m][:], in0=F_["Rf"][:], scalar=chv[:, m, 4:5], in1=F_["KM"][:], op0=ALU.mult, op1=ALU.mult),
                     reads=['Rf', 'KM', 'chv'], writes=[('PROD', m)])
                for q in range(4):
                    pass
                p.op('act', lambda e, m=m: e.copy(out=PCs[m][:], in_=F_["E1"][:].rearrange("p (q t) -> p q t", t=128)[:, :, 127]), reads=['E1'], writes=[('PC', m)])

            for q0 in (0, 2):
                for q in (q0, q0 + 1):
                    cs_ = slice(q * 128, (q + 1) * 128)
                    for m in range(2):
                        B_ = Bm[m]
                        bk = lambda n, m=m: ('B', n, m)
                        bank, bkk = cx.ptbank()
                        bv = bank[:].rearrange("p a b -> p (a b)")
                        for i, n in enumerate(["AT", "BH", "KH", "VT"]):
                            p.op('pe', lambda e, cs_=cs_, q=q, i=i, n=n, B_=B_, bv=bv: e.transpose(out=bv[:, i * 128:(i + 1) * 128], in_=B_[n][:, cs_], identity=ident[:]),
                                 reads=[bk(n), 'ident'], writes=[bkk])
                        cx.copy('act', TMs2[m][q % 2][:], bv[:, 0:512], [bkk], [('TMs', m, q % 2)])
                        ps, pk = cx.psum()
                        p.op('pe', lambda e, cs_=cs_, q=q, ps=ps, m=m: e.matmul(ps[:, 0:2], lhsT=PROD[m][:, cs_], rhs=HSEL[:], start=True, stop=True), reads=[('PROD', m), 'HSEL'], writes=[pk])
                        p.op('dve', lambda e, cs_=cs_, q=q, ps=ps, m=m: e.tensor_copy(out=SBON2[q % 2][:, 2 * m:2 * m + 2], in_=ps[:, 0:2]), reads=[pk], writes=[('SBON', m, q % 2)])

                alive = [head_chain(m, hl, q, slice(q * 128, (q + 1) * 128)) for q in (q0, q0 + 1) for m in range(2) for hl in range(2)]
                while alive:
                    nxt_alive = []
                    for g_ in alive:
                        try:
                            next(g_)
                            nxt_alive.append(g_)
                        except StopIteration:
                            pass
                    alive = nxt_alive
                for q in (q0, q0 + 1):
                    cs_ = slice(q * 128, (q + 1) * 128)
                    yks = [('YO', h, q % 2) for h in range(4)]
                    p.op('dve', lambda e, cs_=cs_, q=q: e.tensor_tensor(out=YO2[q % 2][:], in0=YO2[q % 2][:], in1=Gtok[:, q, :], op=ALU.mult), reads=yks + [('Gtok', q)], writes=yks)
                    p.dma('sp', yg_d[r0 + q * 128:r0 + (q + 1) * 128, :], YO2[q % 2][:], reads=yks)
        p.finish()
        p.emit()
    return nc


def maps_A(inp):
    aw, ab = _ada_blocks(inp['ada_w'], inp['ada_b'], [0, 1])
    muT = np.ascontiguousarray(inp['rw_mu'][0].reshape(6, 8, 128).transpose(2, 0, 1))
    wl1 = np.ascontiguousarray(np.concatenate([inp['rw_w1'][0], inp['rw_a1'][0], inp['rw_g1'][0]], axis=1))
    maps = []
    for c in range(NCORE):
        b, hg = c // 4, c % 4
        cols = slice(256 * hg, 256 * hg + 256)
        wr = inp['rw_w_rkv'][0]
        wrkv = np.ascontiguousarray(np.concatenate([wr[:, 0 * D:1 * D][:, cols], wr[:, 1 * D:2 * D][:, cols], wr[:, 2 * D:3 * D][:, cols]], axis=1))
        w2a = np.ascontiguousarray(np.concatenate([inp['rw_w2'][0][:, cols], inp['rw_a2'][0][:, cols]], axis=0))
        vec = lambda v: v.reshape(-1)[cols].reshape(2, 128).T
        chv = np.zeros((128, 2, 6), np.float32)
        for i, v in enumerate([inp['rw_w0'][0], inp['rw_a0'][0], inp['rw_k_k'][0], inp['rw_k_a'][0], inp['rw_r_k'][0]]):
            chv[:, :, i] = vec(v)
        xpad = np.concatenate([np.zeros((1, D), np.float32), inp['x'][b]], axis=0)
        maps.append({
            "xpad": xpad, "cT": _cT(inp['c'][b]), "adaw": aw, "adab": ab, "muT": muT,
            "w_rkv": wrkv, "w_l1": wl1, "w2a": w2a, "g2": np.ascontiguousarray(inp['rw_g2'][0][:, cols]),
            "chv": chv, "lnx": np.ascontiguousarray(np.stack([inp['rw_lnx_g'][0][cols], inp['rw_lnx_b'][0][cols]])),
        })
    return maps


HALO = 2048
NEG = -30000.0
REC = 66
PASS_T = 1024


def build_C(do_attn=True, npass=TPC // PASS_T, nexp=NEXP, ntile_attn=32):
    nc = bass.Bass("TRN2", target_bir_lowering=False)
    dt = lambda name, shape, dtype, kind: nc.dram_tensor(name, shape, dtype, kind=kind).ap()
    x2_d = dt("x2", [TPC, D], F32, "ExternalInput")
    q_d = dt("q", [TPC, 1536], BF16, "ExternalInput")
    kh_d = dt("kh", [TPC + HALO, 1536], BF16, "ExternalInput")
    vh_d = dt("vh", [TPC + HALO, 1536], BF16, "ExternalInput")
    hb_d = dt("halo_bias", [128, 1], F32, "ExternalInput")
    cT_d = dt("cT", [128, 8], F32, "ExternalInput")
    adaw_d = dt("adaw", [4, D, D], F32, "ExternalInput")
    adab_d = dt("adab", [4, D], F32, "ExternalInput")
    lng_d = dt("lng", [2, D], F32, "ExternalInput")
    lnb_d = dt("lnb", [2, D], F32, "ExternalInput")
    wo_d = dt("w_o", [512, D], F32, "ExternalInput")
    rt_d = dt("router", [8, D], F32, "ExternalInput")
    wgu_d = dt("moe_gu", [NEXP, D, 2 * D_FFE], F32, "ExternalInput")
    wdn_d = dt("moe_dn", [NEXP, D_FFE, D], F32, "ExternalInput")
    out_d = dt("out", [TPC, D], F32, "ExternalOutput")
    scr_d = dt("scr_att", [3, TPC, 8 * REC], F32, "Internal")
    x3_d = dt("scr_x3", [TPC, D], F32, "Internal")

    with ExitStack() as st:
        cx = Ctx(nc, st)
        p = cx.p
        sb = cx.sb
        ident = cx.ident
        names = ["G1", "Gb0", "Bb0", "G2", "B2", "G3g", "Gb1", "Bb1"]
        T = {n: sb(n, [128, D], F32) for n in names}
        RB = sb("RB", [128, 8, D], F32)
        XA = sb("XA", [128, D], F32)
        YB = sb("YB", [128, D], F32)
        TMP = sb("TMP", [128, D], F32)
        W8 = [sb("W8_%d" % i, [128, 8, 512], BF16) for i in range(2)]
        scB = _silu_c_bcast(cx, cT_d)

        def mod(dst, dkey, blk, plus_one):
            _mod_tile(cx, scB, dst, dkey, adaw_d[blk], adab_d[blk:blk + 1, :], W8[0], ('W8', 0), TMP[:], 'TMP', plus_one)

        mod(T["G1"][:], "G1", 0, True)
        mod(XA[:], 'XA', 1, False)
        mod(YB[:], 'YB', 2, True)
        mod(T["G3g"][:], "G3g", 3, True)
        cx.bcast_row(T["Gb0"][:], "Gb0", lng_d[0:1, :])
        cx.bcast_row(T["Bb0"][:], "Bb0", lnb_d[0:1, :])
        cx.bcast_row(T["Gb1"][:], "Gb1", lng_d[1:2, :])
        cx.bcast_row(T["Bb1"][:], "Bb1", lnb_d[1:2, :])
        p.op('dve', lambda e: e.tensor_tensor(out=T["G2"][:], in0=T["Gb0"][:], in1=YB[:], op=ALU.mult), reads=["Gb0", 'YB'], writes=["G2"])
        p.op('dve', lambda e: e.tensor_tensor(out=T["B2"][:], in0=T["Bb0"][:], in1=YB[:], op=ALU.mult), reads=["Bb0", 'YB'], writes=["B2"])
        p.op('dve', lambda e: e.tensor_tensor(out=T["B2"][:], in0=T["B2"][:], in1=XA[:], op=ALU.add), reads=["B2", 'XA'], writes=["B2"])
        for e_ in range(8):
            cx.bcast_row(RB[:, e_, :], ('RB', e_), rt_d[e_:e_ + 1, :])
        WO = sb("WO", [128, 4, D], BF16)
        p.dma('pool', WO[:, :, 0:512], wo_d.rearrange("(c p) n -> p c n", p=128)[:, :, 0:512], writes=['WO'])
        p.dma('pool', WO[:, :, 512:1024], wo_d.rearrange("(c p) n -> p c n", p=128)[:, :, 512:1024], writes=['WO'])

        if do_attn:
          with ExitStack() as st2:
            sb2 = lambda name, shape, dt_: st2.enter_context(nc.sbuf_tensor(name, shape, dt_))
            MASK = sb2("MASK", [128, 256], F32)
            MASKF = sb2("MASKF", [128, 256], F32)
            hbias = sb2("hbias", [128, 1], F32)
            p.dma('sp', hbias[:], hb_d, writes=['hbias'])
            p.op('pool', lambda e: e.memset(MASK[:], 0.0), writes=['MASK'])
            p.op('pool', lambda e: e.affine_select(out=MASK[:], in_=MASK[:], pattern=[[1, 256]], compare_op=ALU.is_ge, fill=NEG, base=0, channel_multiplier=-1),
                 reads=['MASK'], writes=['MASK'])
            p.op('pool', lambda e: e.affine_select(out=MASK[:], in_=MASK[:], pattern=[[-1, 256]], compare_op=ALU.is_ge, fill=NEG, base=128, channel_multiplier=1),
                 reads=['MASK'], writes=['MASK'])
            p.op('dve', lambda e: e.tensor_copy(out=MASKF[:], in_=MASK[:]), reads=['MASK'], writes=['MASKF'])
            p.op('dve', lambda e: e.tensor_scalar(out=MASKF[:, 0:128], in0=MASKF[:, 0:128], scalar1=hbias[:], scalar2=None, op0=ALU.add),
                 reads=['MASKF', 'hbias'], writes=['MASKF'])
            QB = [sb2("QB%d" % i, [128, 512], BF16) for i in range(2)]
            KB = [sb2("KB%d" % i, [128, 2, 512], BF16) for i in range(2)]
            VB = [sb2("VB%d" % i, [128, 2, 512], BF16) for i in range(2)]
            QTs = [sb2("QT%d" % i, [128, 4, 128], BF16) for i in range(2)]
            KTs = [sb2("KT%d" % i, [128, 4, 256], BF16) for i in range(2)]
            SMs = [sb2("SM%d" % h, [128, 256], F32) for h in range(8)]
            PBs = [sb2("PB%d" % h, [128, 256], BF16) for h in range(8)]
            PTs = [sb2("PT%d" % h, [128, 2, 128], BF16) for h in range(8)]
            OUTA = [sb2("OUTA%d" % i, [128, 8, REC], F32) for i in range(2)]
            mxs = [sb2("mx%d" % h, [128, 1], F32) for h in range(8)]
            nmxs = [sb2("nmx%d" % h, [128, 1], F32) for h in range(8)]

            def attn_head(h, bi, mk, mkk, oa):
                hp, hl = h // 2, h % 2
                P = slice(64 * hl, 64 * hl + 64)
                QT, KT, SM, PB, PT, mx, nmx = QTs[bi], KTs[bi], SMs[h], PBs[h], PTs[h], mxs[h], nmxs[h]
                qk, kk_, oak = ('QT', bi), ('KT', bi), ('OUTA', bi, h)
                K_ = lambda n: (n, h)
                ps, pk = cx.psum()
                p.op('pe', lambda e, ps=ps: e.matmul(ps[:, 0:256], lhsT=QT[P, hp, :], rhs=KT[P, hp, :], start=True, stop=True), reads=[qk, kk_], writes=[pk])
                p.op('dve', lambda e, ps=ps: e.tensor_tensor(out=SM[:], in0=ps[:, 0:256], in1=mk[:], op=ALU.add), reads=[pk, mkk], writes=[K_('SM')])
                yield
                p.op('dve', lambda e: e.reduce_max(out=mx[:], in_=SM[:], axis=AX.X), reads=[K_('SM')], writes=[K_('mx')])
                p.op('dve', lambda e: e.tensor_scalar(out=nmx[:], in0=mx[:], scalar1=-0.125, scalar2=None, op0=ALU.mult), reads=[K_('mx')], writes=[K_('nmx')])
                yield
                p.op('act', lambda e: e.activation(out=PB[:], in_=SM[:], func=AF.Exp, bias=nmx[:], scale=0.125, accum_out=oa[:, h, 64:65]),
                     reads=[K_('SM'), K_('nmx')], writes=[K_('PB'), oak])
                p.op('dve', lambda e: e.tensor_scalar(out=oa[:, h, 65:66], in0=mx[:], scalar1=0.125, scalar2=None, op0=ALU.mult), reads=[K_('mx'), oak], writes=[oak])
                yield
                bank, bkk = cx.ptbank()
                for a in range(2):
                    p.op('pe', lambda e, a=a, bank=bank: e.transpose(out=bank[:, 0, a * 128:(a + 1) * 128], in_=PB[:, a * 128:(a + 1) * 128], identity=ident[:]),
                         reads=[K_('PB'), 'ident'], writes=[bkk])
                cx.copy('act', PT[:].rearrange("p a b -> p (a b)"), bank[:, 0, 0:256], [bkk], [K_('PT')])
                yield
                ps2, pk2 = cx.psum()
                for a in range(2):
                    p.op('pe', lambda e, a=a, ps2=ps2: e.matmul(ps2[:, 0:64], lhsT=PT[:, a, :], rhs=VB[bi][:, a, h * 64:(h + 1) * 64], start=(a == 0), stop=(a == 1)),
                         reads=[K_('PT'), ('VB', bi)], writes=[pk2])
                p.op('dve', lambda e, ps2=ps2: e.tensor_copy(out=oa[:, h, 0:64], in_=ps2[:, 0:64]), reads=[pk2, oak], writes=[oak])

            nb_i = 0
            for g, d in enumerate((1, 4, 16)):
                nblocks = TPC // (128 * d)
                for n in range(nblocks):
                    if n * d * 128 >= ntile_attn * 128:
                        continue
                    for ph in range(d):
                        bi = nb_i % 2
                        nb_i += 1
                        t0 = 128 * d * n + ph
                        qv = q_d[t0:t0 + 127 * d + 1:d, g * 512:(g + 1) * 512]
                        r0 = HALO + 128 * d * (n - 1) + ph
                        kv_ = kh_d[r0:r0 + 255 * d + 1:d, g * 512:(g + 1) * 512].rearrange("(a p) c -> p a c", a=2)
                        vv_ = vh_d[r0:r0 + 255 * d + 1:d, g * 512:(g + 1) * 512].rearrange("(a p) c -> p a c", a=2)
                        p.dma('sp', QB[bi][:], qv, writes=[('QB', bi)])
                        p.dma('sp', KB[bi][:], kv_, writes=[('KB', bi)])
                        p.dma('sp', VB[bi][:], vv_, writes=[('VB', bi)])
                        bank, bkk = cx.ptbank()
                        bv = bank[:].rearrange("p a b -> p (a b)")
                        for hp in range(4):
                            p.op('pe', lambda e, hp=hp, bv=bv, bi=bi: e.transpose(out=bv[:, hp * 128:(hp + 1) * 128], in_=QB[bi][:, hp * 128:(hp + 1) * 128], identity=ident[:]),
                                 reads=[('QB', bi), 'ident'], writes=[bkk])
                        cx.copy('act', QTs[bi][:].rearrange("p a b -> p (a b)"), bv[:, 0:512], [bkk], [('QT', bi)])
                        bank, bkk = cx.ptbank()
                        bv = bank[:].rearrange("p a b -> p (a b)")
                        for hp in range(4):
                            for a in range(2):
                                p.op('pe', lambda e, hp=hp, a=a, bv=bv, bi=bi: e.transpose(out=bv[:, hp * 256 + a * 128:hp * 256 + (a + 1) * 128],
                                                                                         in_=KB[bi][:, a, hp * 128:(hp + 1) * 128], identity=ident[:]),
                                     reads=[('KB', bi), 'ident'], writes=[bkk])
                        cx.copy('dve', KTs[bi][:].rearrange("p a b -> p (a b)"), bv[:, 0:1024], [bkk], [('KT', bi)])
                        first = (n == 0)
                        mk, mkk = (MASKF, 'MASKF') if first else (MASK, 'MASK')
                        oa = OUTA[bi]
                        alive = [attn_head(h, bi, mk, mkk, oa) for h in range(8)]
                        while alive:
                            nxt_alive = []
                            for g_ in alive:
                                try:
                                    next(g_)
                                    nxt_alive.append(g_)
                                except StopIteration:
                                    pass
                            alive = nxt_alive
                        sv = scr_d[g, t0:t0 + 127 * d + 1:d, :]
                        p.dma('sp', sv, oa[:].rearrange("p a b -> p (a b)"), reads=[('OUTA', bi, h) for h in range(8)],
                              writes=[('scr', g, (t0 + i * d) // 128) for i in range(0, 128, max(1, 128 // d))] if d > 1 else [('scr', g, t0 // 128)])
            p.barrier()

        NT = PASS_T // 128
        HTm2 = [sb("HTm%d" % i, [128, 8, PASS_T], BF16) for i in range(2)]
        ACC = sb("ACC", [128, NT, D], F32)
        GT2 = [sb("GT%d" % i, [128, NT, 8], F32) for i in range(2)]
        RECS = sb("RECS", [128, 3, 8 * REC], F32)
        HB = sb("HBc", [128, 1, D], BF16)
        OB = sb("OB", [128, 1, 512], BF16)
        OT = sb("OT", [128, 4, 128], BF16)
        HIDm = [sb("HIDm%d" % i, [128, 2, PASS_T], BF16) for i in range(2)]
        WDs = [sb("WDs%d" % i, [128, 2, D], BF16) for i in range(2)]
        SG = [sb("SG%d" % i, [128, 512], F32) for i in range(2)]
        mM = sb("mM", [128, 8], F32)
        cg = sb("cg", [128, 3, 8], F32)
        den = sb("den", [128, 8], F32)
        numt = sb("numt", [128, 8, 64], F32)
        tmp3 = sb("tmp3", [128, 8, 64], F32)
        lg = sb("lg", [128, 8], F32)
        m8 = sb("m8", [128, 8], F32)
        nm0 = sb("nm0", [128, 1], F32)
        gden = sb("gden", [128, 1], F32)
        ge = sb("ge", [128, 8], F32)
        w8_i = 0
        wd_i = 0
        def merge_tile(ps_i, jt):
            HTm = HTm2[ps_i % 2]
            GT = GT2[ps_i % 2]
            hb_ = ps_i % 2
            tile = ps_i * NT + jt
            r0 = tile * 128
            for g in range(3):
                p.dma('sp', RECS[:, g, :], scr_d[g, r0:r0 + 128, :], reads=[('scr', g, tile)], writes=[('RECS', g)])
            rk = [('RECS', g) for g in range(3)]
            R = lambda g: RECS[:, g, :].rearrange("p (h r) -> p h r", r=REC)
            p.op('dve', lambda e: e.tensor_tensor(out=mM[:], in0=R(0)[:, :, 65], in1=R(1)[:, :, 65], op=ALU.max), reads=rk, writes=['mM'])
            p.op('dve', lambda e: e.tensor_tensor(out=mM[:], in0=mM[:], in1=R(2)[:, :, 65], op=ALU.max), reads=rk + ['mM'], writes=['mM'])
            for g in range(3):
                p.op('dve', lambda e, g=g: e.tensor_tensor(out=cg[:, g, :], in0=R(g)[:, :, 65], in1=mM[:], op=ALU.subtract), reads=rk + ['mM'], writes=[('cg', g)])
                p.op('act', lambda e, g=g: e.activation(out=cg[:, g, :], in_=cg[:, g, :], func=AF.Exp), reads=[('cg', g)], writes=[('cg', g)])
            p.op('dve', lambda e: e.tensor_tensor(out=den[:], in0=cg[:, 0, :], in1=R(0)[:, :, 64], op=ALU.mult), reads=rk + [('cg', 0)], writes=['den'])
            p.op('dve', lambda e: e.tensor_tensor(out=numt[:], in0=R(0)[:, :, 0:64], in1=cg[:, 0, :].unsqueeze(2).to_broadcast([128, 8, 64]), op=ALU.mult), reads=rk + [('cg', 0)], writes=['numt'])
            for g in (1, 2):
                p.op('dve', lambda e, g=g: e.tensor_tensor(out=lg[:], in0=cg[:, g, :], in1=R(g)[:, :, 64], op=ALU.mult), reads=rk + [('cg', g)], writes=['lg'])
                p.op('dve', lambda e: e.tensor_tensor(out=den[:], in0=den[:], in1=lg[:], op=ALU.add), reads=['den', 'lg'], writes=['den'])
                p.op('dve', lambda e, g=g: e.tensor_tensor(out=tmp3[:], in0=R(g)[:, :, 0:64], in1=cg[:, g, :].unsqueeze(2).to_broadcast([128, 8, 64]), op=ALU.mult), reads=rk + [('cg', g)], writes=['tmp3'])
                p.op('dve', lambda e: e.tensor_tensor(out=numt[:], in0=numt[:], in1=tmp3[:], op=ALU.add), reads=['numt', 'tmp3'], writes=['numt'])
            p.op('dve', lambda e: e.reciprocal(out=den[:], in_=den[:]), reads=['den'], writes=['den'])
            p.op('dve', lambda e: e.tensor_tensor(out=OB[:, 0, :].rearrange("p (h d) -> p h d", d=64), in0=numt[:], in1=den[:].unsqueeze(2).to_broadcast([128, 8, 64]), op=ALU.mult),
                 reads=['numt', 'den'], writes=['OB'])
            bank, bkk = cx.ptbank()
            for c in range(4):
                p.op('pe', lambda e, c=c, bank=bank: e.transpose(out=bank[:, 0, c * 128:(c + 1) * 128], in_=OB[:, 0, c * 128:(c + 1) * 128], identity=ident[:]), reads=['OB', 'ident'], writes=[bkk])
            cx.copy('act', OT[:].rearrange("p a b -> p (a b)"), bank[:, 0, :], [bkk], ['OT'])
            p.dma('sp', XA[:], x2_d[r0:r0 + 128, :], writes=['XA'])
            for half in range(2):
                ps, pk = cx.psum()
                for c in range(4):
                    p.op('pe', lambda e, c=c, ps=ps, half=half: e.matmul(ps[:], lhsT=OT[:, c, :], rhs=WO[:, c, half * 512:(half + 1) * 512], start=(c == 0), stop=(c == 3)), reads=['OT', 'WO'], writes=[pk])
                sl = slice(half * 512, (half + 1) * 512)
                p.op('dve', lambda e, ps=ps, sl=sl: e.tensor_tensor(out=YB[:, sl], in0=ps[:], in1=T["G1"][:, sl], op=ALU.mult), reads=[pk, "G1"], writes=['YB'])
            p.op('dve', lambda e: e.scalar_tensor_tensor(out=XA[:], in0=XA[:], scalar=ALPHA, in1=YB[:], op0=ALU.mult, op1=ALU.add), reads=['XA', 'YB'], writes=['XA'])
            cx.layer_norm_tile(XA[:], 'XA', 'c')
            p.op('dve', lambda e: e.tensor_tensor(out=TMP[:], in0=XA[:], in1=T["G2"][:], op=ALU.mult), reads=['XA', "G2"], writes=['TMP'])
            p.op('dve', lambda e: e.tensor_tensor(out=TMP[:], in0=TMP[:], in1=T["B2"][:], op=ALU.add), reads=['TMP', "B2"], writes=['TMP'])
            p.op('act', lambda e: e.copy(out=HB[:, 0, :], in_=TMP[:]), reads=['TMP'], writes=[('HBc', 0)])
            p.op('dve', lambda e: e.tensor_tensor(out=XA[:], in0=XA[:], in1=T["Gb0"][:], op=ALU.mult), reads=['XA', "Gb0"], writes=['XA'])
            p.op('dve', lambda e: e.tensor_tensor(out=XA[:], in0=XA[:], in1=T["Bb0"][:], op=ALU.add), reads=['XA', "Bb0"], writes=['XA'])
            p.dma('sp', x3_d[r0:r0 + 128, :], XA[:], reads=['XA'], writes=[('x3', tile)])
            for e_ in range(8):
                p.op('dve', lambda e, e_=e_: e.tensor_tensor(out=YB[:], in0=TMP[:], in1=RB[:, e_, :], op=ALU.mult), reads=['TMP', ('RB', e_)], writes=['YB'])
                p.op('act', lambda e, e_=e_: e.activation(out=YB[:], in_=YB[:], func=AF.Identity, accum_out=lg[:, e_:e_ + 1]), reads=['YB'], writes=['YB', 'lg'])
            p.op('dve', lambda e: e.max(out=m8[:], in_=lg[:]), reads=['lg'], writes=['m8'])
            p.op('dve', lambda e: e.tensor_scalar(out=nm0[:], in0=m8[:, 0:1], scalar1=-1.0, scalar2=None, op0=ALU.mult), reads=['m8'], writes=['nm0'])
            p.op('act', lambda e: e.activation(out=ge[:], in_=lg[:], func=AF.Exp, bias=nm0[:], scale=1.0), reads=['lg', 'nm0'], writes=['ge'])
            p.op('dve', lambda e: e.scalar_tensor_tensor(out=ge[:], in0=lg[:], scalar=m8[:, 1:2], in1=ge[:], op0=ALU.is_ge, op1=ALU.mult), reads=['lg', 'm8', 'ge'], writes=['ge'])
            p.op('dve', lambda e: e.reduce_sum(out=gden[:], in_=ge[:], axis=AX.X), reads=['ge'], writes=['gden'])
            p.op('dve', lambda e: e.reciprocal(out=gden[:], in_=gden[:]), reads=['gden'], writes=['gden'])
            p.op('dve', lambda e, jt=jt: e.tensor_scalar(out=GT[:, jt, :], in0=ge[:], scalar1=gden[:], scalar2=None, op0=ALU.mult), reads=['ge', 'gden'], writes=[('GT', hb_, jt)])
            bank, bkk = None, None
            for c2 in range(0, 8, 4):
                bank, bkk = cx.ptbank()
                bv = bank[:].rearrange("p a b -> p (a b)")
                for cc in range(4):
                    p.op('pe', lambda e, c2=c2, cc=cc, bv=bv: e.transpose(out=bv[:, cc * 128:(cc + 1) * 128], in_=HB[:, 0, (c2 + cc) * 128:(c2 + cc + 1) * 128], identity=ident[:]),
                         reads=[('HBc', 0), 'ident'], writes=[bkk])
                cx.copy(cx.evac_eng(), HTm[:, c2:c2 + 4, jt * 128:(jt + 1) * 128], bv[:, 0:512].rearrange("p (a b) -> p a b", b=128), [bkk], [('HTm', hb_, jt)])

        for jt in range(NT):
            merge_tile(0, jt)
        for ps_i in range(npass):
            HTm = HTm2[ps_i % 2]
            GT = GT2[ps_i % 2]
            hb_ = ps_i % 2
            htk = [('HTm', hb_, jt) for jt in range(NT)]
            for jt in range(NT):
                p.op('pool', lambda e, jt=jt: e.memset(ACC[:, jt, :], 0.0), writes=[('ACC', jt)])
            for ex in range(nexp):
                gv = wgu_d[ex].rearrange("(c p) n -> p c n", p=128)
                dv = wdn_d[ex].rearrange("(c p) n -> p c n", p=128)
                for s in range(D_FFE // 256):
                    wb, wk = W8[w8_i % 2], ('W8', w8_i % 2)
                    w8_i += 1
                    p.dma('pool', wb[:, :, 0:256], gv[:, :, s * 256:(s + 1) * 256], writes=[wk])
                    p.dma('pool', wb[:, :, 256:512], gv[:, :, D_FFE + s * 256:D_FFE + (s + 1) * 256], writes=[wk])
                    wdb, wdk = WDs[wd_i % 2], ('WDs', wd_i % 2)
                    hb, hk = HIDm[wd_i % 2], ('HIDm', wd_i % 2)
                    wd_i += 1
                    p.dma('pool', wdb[:, :, 0:512], dv[:, 2 * s:2 * s + 2, 0:512], writes=[wdk])
                    p.dma('pool', wdb[:, :, 512:1024], dv[:, 2 * s:2 * s + 2, 512:1024], writes=[wdk])
                    for fc in range(2):
                        for tg in range(PASS_T // 512):
                            tsl = slice(tg * 512, (tg + 1) * 512)
                            psg, pkg = cx.psum()
                            for c in range(8):
                                p.op('pe', lambda e, c=c, fc=fc, psg=psg, wb=wb, tsl=tsl, hb_=hb_: e.matmul(psg[:], lhsT=wb[:, c, fc * 128:(fc + 1) * 128], rhs=HTm2[hb_][:, c, tsl], start=(c == 0), stop=(c == 7)),
                                     reads=htk + [wk], writes=[pkg])
                            psu, pku = cx.psum()
                            for c in range(8):
                                p.op('pe', lambda e, c=c, fc=fc, psu=psu, wb=wb, tsl=tsl, hb_=hb_: e.matmul(psu[:], lhsT=wb[:, c, 256 + fc * 128:256 + (fc + 1) * 128], rhs=HTm2[hb_][:, c, tsl], start=(c == 0), stop=(c == 7)),
                                     reads=htk + [wk], writes=[pku])
                            sgi = (fc * 2 + tg) % 2
                            sg, sgk = SG[sgi], ('SG', sgi)
                            p.op('act', lambda e, psg=psg, sg=sg: e.activation(out=sg[:], in_=psg[:], func=AF.Silu), reads=[pkg], writes=[sgk])
                            p.op('dve', lambda e, psu=psu, sg=sg, hb=hb, fc=fc, tsl=tsl: e.tensor_tensor(out=hb[:, fc, tsl], in0=psu[:], in1=sg[:], op=ALU.mult), reads=[pku, sgk], writes=[hk])
                    for jt in range(NT):
                        for half in range(2):
                            ps, pk = cx.psum()
                            for fc in range(2):
                                p.op('pe', lambda e, fc=fc, ps=ps, hb=hb, wdb=wdb, jt=jt, half=half: e.matmul(ps[:], lhsT=hb[:, fc, jt * 128:(jt + 1) * 128], rhs=wdb[:, fc, half * 512:(half + 1) * 512],
                                                                                                         start=(fc == 0), stop=(fc == 1)), reads=[hk, wdk], writes=[pk])
                            sl = slice(half * 512, (half + 1) * 512)
                            p.op('dve', lambda e, ps=ps, jt=jt, sl=sl, ex=ex, hb_=hb_: e.scalar_tensor_tensor(out=ACC[:, jt, sl], in0=ps[:], scalar=GT2[hb_][:, jt, ex:ex + 1], in1=ACC[:, jt, sl], op0=ALU.mult, op1=ALU.add),
                                 reads=[pk, ('GT', hb_, jt), ('ACC', jt)], writes=[('ACC', jt)])
                if ps_i + 1 < npass and ex < NT:
                    merge_tile(ps_i + 1, ex)
            if ps_i + 1 < npass:
                for jt in range(min(nexp, NT), NT):
                    merge_tile(ps_i + 1, jt)
            for jt in range(NT):
                tile = ps_i * NT + jt
                r0 = tile * 128
                p.dma('sp', XA[:], x3_d[r0:r0 + 128, :], reads=[('x3', tile)], writes=['XA'])
                p.op('dve', lambda e, jt=jt: e.tensor_tensor(out=ACC[:, jt, :], in0=ACC[:, jt, :], in1=T["G3g"][:], op=ALU.mult), reads=[('ACC', jt), "G3g"], writes=[('ACC', jt)])
                p.op('dve', lambda e, jt=jt: e.scalar_tensor_tensor(out=XA[:], in0=XA[:], scalar=ALPHA, in1=ACC[:, jt, :], op0=ALU.mult, op1=ALU.add), reads=['XA', ('ACC', jt)], writes=['XA'])
                cx.layer_norm_tile(XA[:], 'XA', 'd')
                p.op('dve', lambda e: e.tensor_tensor(out=XA[:], in0=XA[:], in1=T["Gb1"][:], op=ALU.mult), reads=['XA', "Gb1"], writes=['XA'])
                p.op('dve', lambda e: e.tensor_tensor(out=XA[:], in0=XA[:], in1=T["Bb1"][:], op=ALU.add), reads=['XA', "Bb1"], writes=['XA'])
                p.dma('sp', out_d[r0:r0 + 128, :], XA[:], reads=['XA'])
        p.finish()
        p.emit()
    return nc


def maps_C(inp, x2, q, k, v):
    idxs = [8, 9, 10, 11]
    aw, ab = _ada_blocks(inp['ada_w'], inp['ada_b'], idxs)
    maps = []
    for c in range(NCORE):
        b, s0 = c // 4, (c % 4) * TPC
        if s0 == 0:
            kh = np.concatenate([np.zeros((HALO, 1536), k.dtype), k[b, 0:TPC]], axis=0)
            vh = np.concatenate([np.zeros((HALO, 1536), v.dtype), v[b, 0:TPC]], axis=0)
        else:
            kh = np.ascontiguousarray(k[b, s0 - HALO:s0 + TPC])
            vh = np.ascontiguousarray(v[b, s0 - HALO:s0 + TPC])
        maps.append({
            "x2": np.ascontiguousarray(x2[b, s0:s0 + TPC]), "q": np.ascontiguousarray(q[b, s0:s0 + TPC]),
            "kh": kh, "vh": vh,
            "halo_bias": np.full((128, 1), NEG if s0 == 0 else 0.0, np.float32),
            "cT": _cT(inp['c'][b]), "adaw": aw, "adab": ab,
            "lng": np.ascontiguousarray(inp['ln_g'][1]), "lnb": np.ascontiguousarray(inp['ln_b'][1]),
            "w_o": inp['attn_w_o'][0], "router": np.ascontiguousarray(inp['moe_router'][0].T),
            "moe_gu": inp['moe_w_gu'][0], "moe_dn": inp['moe_w_down'][0],
        })
    return maps


def _run(nc, maps):
    return run_bass_kernel_spmd(nc, maps, core_ids=list(range(NCORE))).results


def kernel(**inputs):
    inp = {k: np.asarray(v) for k, v in inputs.items()}
    resA = _run(build_A(), maps_A(inp))
    yg = np.empty((2, SEQ, D), np.float32)
    for c in range(NCORE):
        b, hg = c // 4, c % 4
        yg[b, :, 256 * hg:256 * hg + 256] = np.asarray(resA[c]["yg"])
    del resA
    resB = _run(build_B(), maps_B(inp, yg))
    x2 = np.empty((2, SEQ, D), np.float32)
    q = np.empty((2, SEQ, 1536), ml_dtypes.bfloat16)
    k = np.empty((2, SEQ, 1536), ml_dtypes.bfloat16)
    v = np.empty((2, SEQ, 1536), ml_dtypes.bfloat16)
    for c in range(NCORE):
        b, s0 = c // 4, (c % 4) * TPC
        x2[b, s0:s0 + TPC] = np.asarray(resB[c]["x2"])
        q[b, s0:s0 + TPC] = np.asarray(resB[c]["q"])
        k[b, s0:s0 + TPC] = np.asarray(resB[c]["k"])
        v[b, s0:s0 + TPC] = np.asarray(resB[c]["v"])
    del resB
    resC = _run(build_C(), maps_C(inp, x2, q, k, v))
    out = np.empty((2, SEQ, D), np.float32)
    for c in range(NCORE):
        b, s0 = c // 4, (c % 4) * TPC
        out[b, s0:s0 + TPC] = np.asarray(resC[c]["out"])
    return out
```

```python
import math
import numpy as np
from contextlib import ExitStack
import concourse.bass as bass
import concourse.mybir as mybir
from concourse.bass_utils import run_bass_kernel_spmd
import ml_dtypes

F32 = mybir.dt.float32
BF16 = mybir.dt.bfloat16
I32 = mybir.dt.int32
AF = mybir.ActivationFunctionType
ALU = mybir.AluOpType
AX = mybir.AxisListType

D = 1024
SEQ = 16384
NCORE = 8
TPC = 4096
ALPHA = 4.0 ** 0.25
LN_EPS = 1e-5
D_FF = 2816
NEXP = 8
D_FFE = 3584
TWO_PI = 2.0 * math.pi


SAME_ENGINE_FIFO = False
EMBED_WAIT = True


class Prog:
    CE = ('pe', 'act', 'dve', 'pool')

    def __init__(self, nc, stack, ndma=12):
        self.nc = nc
        self.q = {e: [] for e in ('pe', 'act', 'dve', 'pool', 'sp')}
        self.esem = {e: stack.enter_context(nc.semaphore('s_' + e)) for e in self.CE}
        self.ecnt = {e: 0 for e in self.CE}
        self.dpool = {e: [stack.enter_context(nc.semaphore('d_%s%d' % (e, i))) for i in range(ndma)]
                      for e in ('sp', 'pool', 'act')}
        self.dnext = {e: 0 for e in self.dpool}
        self.dval = {}
        self.lastw = {}
        self.readers = {}
        self.seen = {e: {} for e in self.q}
        self.semobj = {}

    def _deps(self, eng, reads, writes):
        evs = []
        for k in reads:
            ev = self.lastw.get(k)
            if ev is not None:
                evs.append(ev)
        for k in writes:
            ev = self.lastw.get(k)
            if ev is not None:
                evs.append(ev)
            evs.extend(self.readers.get(k, ()))
        need = {}
        for (sem, val, src) in evs:
            if src == eng and (eng == 'pe' or (SAME_ENGINE_FIFO and eng in ('act', 'dve'))):
                continue
            if self.seen[eng].get(sem.num, 0) >= val:
                continue
            if need.get(sem.num, 0) < val:
                need[sem.num] = val
                self.semobj[sem.num] = sem
        for num, val in need.items():
            self.q[eng].append(('wait', self.semobj[num], val))
            self.seen[eng][num] = val

    def _commit(self, ev, reads, writes):
        ws = set(writes)
        for k in writes:
            self.lastw[k] = ev
            self.readers[k] = []
        for k in reads:
            if k in ws:
                continue
            lst = self.readers.setdefault(k, [])
            lst[:] = [e for e in lst if e[0].num != ev[0].num]
            lst.append(ev)

    def op(self, eng, fn, reads=(), writes=()):
        self._deps(eng, reads, writes)
        self.ecnt[eng] += 1
        ev = (self.esem[eng], self.ecnt[eng], eng)
        self.q[eng].append(('op', fn, ev))
        self._commit(ev, reads, writes)

    def dma(self, eng, out, in_, reads=(), writes=(), **kw):
        self._deps(eng, reads, writes)
        pool = self.dpool[eng]
        sem = pool[self.dnext[eng] % len(pool)]
        self.dnext[eng] += 1
        prev = self.dval.get(sem.num, 0)
        if prev > 0 and self.seen[eng].get(sem.num, 0) < prev:
            self.q[eng].append(('wait', sem, prev))
            self.seen[eng][sem.num] = prev
        self.dval[sem.num] = prev + 16
        ev = (sem, prev + 16, 'dma')
        self.q[eng].append(('dma', out, in_, ev, kw))
        self._commit(ev, reads, writes)

    def barrier(self):
        for eng in self.q:
            for src in self.CE:
                if self.ecnt[src] > 0 and self.seen[eng].get(self.esem[src].num, 0) < self.ecnt[src]:
                    self.q[eng].append(('wait', self.esem[src], self.ecnt[src]))
                    self.seen[eng][self.esem[src].num] = self.ecnt[src]
            for e2 in self.dpool:
                for sem in self.dpool[e2]:
                    v = self.dval.get(sem.num, 0)
                    if v > 0 and self.seen[eng].get(sem.num, 0) < v:
                        self.q[eng].append(('wait', sem, v))
                        self.seen[eng][sem.num] = v

    def finish(self):
        for e in self.dpool:
            for sem in self.dpool[e]:
                v = self.dval.get(sem.num, 0)
                if v > 0:
                    self.q['sp'].append(('wait', sem, v))
        for e in self.CE:
            if self.ecnt[e] > 0:
                self.q['sp'].append(('wait', self.esem[e], self.ecnt[e]))

    def emit(self):
        nc = self.nc
        with nc.Block() as block:
            def run(engname):
                def body(e):
                    pend = []
                    for it in self.q[engname]:
                        if it[0] == 'wait':
                            pend.append(it)
                            continue
                        emb = None
                        if EMBED_WAIT and pend:
                            emb = pend.pop()
                        for w in pend:
                            e.wait_ge(w[1], w[2])
                        pend = []
                        if it[0] == 'op':
                            ins = it[1](e)
                            if emb is not None:
                                ins = ins._wait_ge(emb[1], emb[2])
                            ins.then_inc(it[2][0], 1)
                        else:
                            ins = e.dma_start(out=it[1], in_=it[2], **it[4])
                            if emb is not None:
                                ins = ins._wait_ge(emb[1], emb[2])
                            ins.then_inc(it[3][0], 16)
                    for w in pend:
                        e.wait_ge(w[1], w[2])
                return body
            block.sync(run('sp'))
            block.tensor(run('pe'))
            block.scalar(run('act'))
            block.vector(run('dve'))
            block.gpsimd(run('pool'))


class Ctx:
    def __init__(self, nc, st):
        self.nc = nc
        self.st = st
        self.p = Prog(nc, st)
        self.ps = [st.enter_context(nc.psum_tensor("ps%d" % i, [128, 512], F32)) for i in range(5)]
        self.pt = [st.enter_context(nc.psum_tensor("pt%d" % i, [128, 2, 512], BF16)) for i in range(3)]
        self.psn = 0
        self.ptn = 0
        self.evn = 0
        self.ident = self.sb("ident", [128, 128], BF16)
        p = self.p
        p.op('pool', lambda e: e.memset(self.ident[:], 0.0), writes=['ident'])
        p.op('pool', lambda e: e.affine_select(out=self.ident[:], in_=self.ident[:], pattern=[[-1, 128]],
                                               compare_op=ALU.not_equal, fill=1.0, base=0, channel_multiplier=1),
             reads=['ident'], writes=['ident'])

    def sb(self, name, shape, dt):
        return self.st.enter_context(self.nc.sbuf_tensor(name, shape, dt))

    def psum(self):
        i = self.psn % len(self.ps)
        self.psn += 1
        return self.ps[i], ('ps', i)

    def ptbank(self):
        i = self.ptn % len(self.pt)
        self.ptn += 1
        return self.pt[i], ('pt', i)

    def evac_eng(self):
        self.evn += 1
        return 'dve' if self.evn % 2 else 'act'

    def copy(self, eng, out, in_, reads, writes):
        if eng == 'act':
            self.p.op('act', lambda e: e.copy(out=out, in_=in_), reads=reads, writes=writes)
        else:
            self.p.op(eng, lambda e: e.tensor_copy(out=out, in_=in_), reads=reads, writes=writes)

    def to_featmajor(self, src, srckeys, dst, dstkey, ntile, nchunk):
        p = self.p
        for c2 in range(0, nchunk, 2):
            bank, bk = self.ptbank()
            ncc = min(2, nchunk - c2)
            for cc in range(ncc):
                c = c2 + cc
                for j in range(ntile):
                    p.op('pe', lambda e, c=c, cc=cc, j=j, bank=bank: e.transpose(out=bank[:, cc, j * 128:(j + 1) * 128],
                                                                                 in_=src[:, j, c * 128:(c + 1) * 128],
                                                                                 identity=self.ident[:]),
                         reads=[srckeys[j], 'ident'], writes=[bk])
            self.copy(self.evac_eng(), dst[:, c2:c2 + ncc, 0:ntile * 128], bank[:, 0:ncc, 0:ntile * 128], [bk],
                      [(dstkey, c2 + cc) for cc in range(ncc)])

    def layer_norm_tile(self, z, zkey, tag):
        p = self.p
        if not hasattr(self, 'ln_tmp'):
            self.ln_stats = self.sb("ln_stats", [128, 2, 6], F32)
            self.ln_mv = self.sb("ln_mv", [128, 2], F32)
            self.ln_rs = self.sb("ln_rs", [128, 1], F32)
            self.ln_nm = self.sb("ln_nm", [128, 1], F32)
            self.ln_tmp = True
        st_, mv, rs, nm = self.ln_stats, self.ln_mv, self.ln_rs, self.ln_nm
        for h in range(2):
            p.op('dve', lambda e, h=h: e.bn_stats(out=st_[:, h, :], in_=z[:, h * 512:(h + 1) * 512]),
                 reads=[zkey], writes=[('lnst', h)])
        p.op('dve', lambda e: e.bn_aggr(out=mv[:], in_=st_[:].rearrange("p a b -> p (a b)")),
             reads=[('lnst', 0), ('lnst', 1)], writes=['lnmv'])
        p.op('act', lambda e: e.activation(out=rs[:], in_=mv[:, 1:2], func=AF.Sqrt, bias=LN_EPS, scale=1.0),
             reads=['lnmv'], writes=['lnrs'])
        p.op('dve', lambda e: e.reciprocal(out=rs[:], in_=rs[:]), reads=['lnrs'], writes=['lnrs'])
        p.op('dve', lambda e: e.tensor_scalar(out=nm[:], in0=mv[:, 0:1], scalar1=rs[:], scalar2=-1.0,
                                              op0=ALU.mult, op1=ALU.mult),
             reads=['lnmv', 'lnrs'], writes=['lnnm'])
        p.op('act', lambda e: e.activation(out=z, in_=z, func=AF.Identity, bias=nm[:], scale=rs[:]),
             reads=[zkey, 'lnrs', 'lnnm'], writes=[zkey])

    def bcast_row(self, dst, dstkey, row_ap, eng='sp'):
        self.p.dma(eng, dst, row_ap.partition_broadcast(128), writes=[dstkey])


def _silu_c_bcast(cx, cT_ap):
    p = cx.p
    ct = cx.sb("ct", [128, 8], F32)
    zer = cx.sb("zer", [128, 128], F32)
    scB = cx.sb("scB", [128, 8, 128], BF16)
    p.dma('sp', ct[:], cT_ap, writes=['ct'])
    p.op('act', lambda e: e.activation(out=ct[:], in_=ct[:], func=AF.Silu), reads=['ct'], writes=['ct'])
    p.op('pool', lambda e: e.memset(zer[:], 0.0), writes=['zer'])
    for c in range(8):
        p.op('act', lambda e, c=c: e.activation(out=scB[:, c, :], in_=zer[:], func=AF.Identity,
                                                bias=ct[:, c:c + 1], scale=1.0),
             reads=['ct', 'zer'], writes=['scB'])
    return scB


def _mod_tile(cx, scB, dst, dstkey, adaw_blk, adab_row, wbuf, wkey, tmp, tmpkey, plus_one):
    p = cx.p
    cx.bcast_row(tmp, tmpkey, adab_row)
    wv = adaw_blk.rearrange("(c p) n -> p c n", p=128)
    for half in range(2):
        p.dma('pool', wbuf[:, :, :], wv[:, :, half * 512:(half + 1) * 512], reads=[], writes=[wkey])
        ps, pk = cx.psum()
        for c in range(8):
            p.op('pe', lambda e, c=c, ps=ps: e.matmul(ps[:], lhsT=scB[:, c, :], rhs=wbuf[:, c, :],
                                                      start=(c == 0), stop=(c == 7)),
                 reads=['scB', wkey], writes=[pk])
        sl = slice(half * 512, (half + 1) * 512)
        p.op('dve', lambda e, ps=ps, sl=sl: e.scalar_tensor_tensor(out=dst[:, sl], in0=ps[:], scalar=(1.0 if plus_one else 0.0),
                                                                    in1=tmp[:, sl], op0=ALU.add, op1=ALU.add),
             reads=[pk, tmpkey], writes=[dstkey])


def _rope_tables(cx, posb_ap, ntile, ang, angkey, kk, kkkey):
    p = cx.p
    nc = cx.nc
    posi = cx.sb("posi", [128, ntile], I32)
    pos = cx.sb("pos", [128, ntile], F32)
    pb = cx.sb("pb", [128, 1], F32)
    cos = cx.sb("cos", [128, ntile, 32], F32)
    sin = cx.sb("sin", [128, ntile, 32], F32)
    p.dma('sp', pb[:], posb_ap, writes=['pb'])
    p.op('pool', lambda e: e.iota(posi[:], pattern=[[128, ntile]], base=0, channel_multiplier=1), writes=['posi'])
    p.op('dve', lambda e: e.tensor_copy(out=pos[:], in_=posi[:]), reads=['posi'], writes=['pos'])
    p.op('dve', lambda e: e.tensor_scalar(out=pos[:], in0=pos[:], scalar1=pb[:], scalar2=None, op0=ALU.add),
         reads=['pos', 'pb'], writes=['pos'])
    for i in range(32):
        invf = float(np.float32(10000.0) ** np.float32(-i / 32.0))
        p.op('dve', lambda e, i=i, invf=invf: e.tensor_scalar(out=ang[:, :, i], in0=pos[:], scalar1=invf, scalar2=None,
                                                              op0=ALU.mult),
             reads=['pos'], writes=[angkey])
    MAGIC = 12582912.0
    C1 = 6.28125
    C2 = TWO_PI - C1
    p.op('dve', lambda e: e.tensor_scalar(out=kk, in0=ang, scalar1=1.0 / TWO_PI, scalar2=MAGIC,
                                          op0=ALU.mult, op1=ALU.add), reads=[angkey], writes=[kkkey])
    p.op('dve', lambda e: e.tensor_scalar(out=kk, in0=kk, scalar1=-MAGIC, scalar2=None, op0=ALU.add),
         reads=[kkkey], writes=[kkkey])
    p.op('dve', lambda e: e.scalar_tensor_tensor(out=ang, in0=kk, scalar=-C1, in1=ang, op0=ALU.mult, op1=ALU.add),
         reads=[kkkey, angkey], writes=[angkey])
    p.op('dve', lambda e: e.scalar_tensor_tensor(out=ang, in0=kk, scalar=-C2, in1=ang, op0=ALU.mult, op1=ALU.add),
         reads=[kkkey, angkey], writes=[angkey])
    p.op('dve', lambda e: e.tensor_scalar(out=ang, in0=ang, scalar1=math.pi, scalar2=-math.pi, op0=ALU.min, op1=ALU.max),
         reads=[angkey], writes=[angkey])
    p.op('act', lambda e: e.activation(out=sin[:], in_=ang, func=AF.Sin), reads=[angkey], writes=['sin'])
    p.op('act', lambda e: e.activation(out=kk, in_=ang, func=AF.Abs), reads=[angkey], writes=[kkkey])
    p.op('act', lambda e: e.activation(out=cos[:], in_=kk, func=AF.Sin, bias=cx.halfpi[:], scale=-1.0),
         reads=[kkkey, 'halfpi'], writes=['cos'])
    return cos, sin


def _rope_apply(cx, ps, pk, out, outkey, cos_t, sin_t, nh, tmp):
    p = cx.p
    pv = ps.rearrange("p (h t d) -> p h t d", h=nh, t=2)
    ov = out.rearrange("p (h t d) -> p h t d", h=nh, t=2)
    cb = cos_t.unsqueeze(1).to_broadcast([128, nh, 32])
    sbb = sin_t.unsqueeze(1).to_broadcast([128, nh, 32])
    ta = tmp[:, 0, :].rearrange("p (h d) -> p h d", h=nh)
    tb = tmp[:, 1, :].rearrange("p (h d) -> p h d", h=nh)
    tk = ('ropetmp',)
    p.op('dve', lambda e: e.tensor_tensor(out=ta, in0=pv[:, :, 0, :], in1=cb, op=ALU.mult), reads=[pk, 'cos'], writes=[tk])
    p.op('dve', lambda e: e.tensor_tensor(out=tb, in0=pv[:, :, 1, :], in1=sbb, op=ALU.mult), reads=[pk, 'sin'], writes=[tk])
    p.op('dve', lambda e: e.tensor_tensor(out=ov[:, :, 0, :], in0=ta, in1=tb, op=ALU.subtract), reads=[tk], writes=[outkey])
    p.op('dve', lambda e: e.tensor_tensor(out=ta, in0=pv[:, :, 1, :], in1=cb, op=ALU.mult), reads=[pk, 'cos'], writes=[tk])
    p.op('dve', lambda e: e.tensor_tensor(out=tb, in0=pv[:, :, 0, :], in1=sbb, op=ALU.mult), reads=[pk, 'sin'], writes=[tk])
    p.op('dve', lambda e: e.tensor_tensor(out=ov[:, :, 1, :], in0=ta, in1=tb, op=ALU.add), reads=[tk], writes=[outkey])


def build_B(nblk=TPC // 512):
    nc = bass.Bass("TRN2", target_bir_lowering=False)
    dt = lambda name, shape, dtype, kind: nc.dram_tensor(name, shape, dtype, kind=kind).ap()
    x_d = dt("x", [TPC, D], F32, "ExternalInput")
    yg_d = dt("yg", [TPC, D], F32, "ExternalInput")
    cT_d = dt("cT", [128, 8], F32, "ExternalInput")
    adaw_d = dt("adaw", [8, D, D], F32, "ExternalInput")
    adab_d = dt("adab", [8, D], F32, "ExternalInput")
    lng_d = dt("lng", [2, D], F32, "ExternalInput")
    lnb_d = dt("lnb", [2, D], F32, "ExternalInput")
    wout_d = dt("w_out", [D, D], F32, "ExternalInput")
    wgu_d = dt("w_gu", [D, 2 * D_FF], F32, "ExternalInput")
    wdn_d = dt("w_down", [D_FF, D], F32, "ExternalInput")
    wkv_d = dt("w_kv", [D, 3072], F32, "ExternalInput")
    wq_d = dt("w_q", [D, 1536], F32, "ExternalInput")
    posb_d = dt("posb", [128, 1], F32, "ExternalInput")
    x2_d = dt("x2", [TPC, D], F32, "ExternalOutput")
    k_d = dt("k", [TPC, 1536], BF16, "ExternalOutput")
    v_d = dt("v", [TPC, 1536], BF16, "ExternalOutput")
    q_d = dt("q", [TPC, 1536], BF16, "ExternalOutput")
    bw8_d = dt("scr_bw8", [22, 128, 8 * 512], BF16, "Internal")
    bwd_d = dt("scr_bwd", [4, 128, 22 * 256], BF16, "Internal")

    with ExitStack() as st:
        cx = Ctx(nc, st)
        p = cx.p
        sb = cx.sb
        XA = sb("XA", [128, 4, D], F32)
        YB = sb("YB", [128, 4, D], F32)
        TB = sb("TB", [128, 4, D], BF16)
        HT = [sb("HT%d" % i, [128, 8, 512], BF16) for i in range(2)]
        HID = sb("HID", [128, 22, 512], BF16)
        SG = [sb("SG%d" % i, [128, 512], F32) for i in range(2)]
        W8 = [sb("W8_%d" % i, [128, 8, 512], BF16) for i in range(2)]
        WD = [sb("WD_%d" % i, [128, 22, 256], BF16) for i in range(2)]
        OUTB = sb("OUTB", [128, 4, 1536], BF16)
        TMP = sb("TMP", [128, D], F32)
        RT = sb("RT", [128, 2, 256], F32)
        cx.halfpi = sb("halfpi", [128, 1], F32)
        p.op('pool', lambda e: e.memset(cx.halfpi[:], math.pi / 2), writes=['halfpi'])
        names = ["G1", "Gb0", "Bb0", "G2", "B2", "G3g", "Gb1", "Bb1", "G4", "B4", "G5", "B5"]
        T = {n: sb(n, [128, D], F32) for n in names}

        scB = _silu_c_bcast(cx, cT_d)
        tm = {"sh1": XA[:, 0, :], "sc1": XA[:, 1, :], "shkv": XA[:, 2, :], "sckv": XA[:, 3, :],
              "shq": YB[:, 0, :], "scq": YB[:, 1, :], "bias": YB[:, 2, :]}
        tk = {'sh1': ('XA', 0), 'sc1': ('XA', 1), 'shkv': ('XA', 2), 'sckv': ('XA', 3), 'shq': ('YB', 0), 'scq': ('YB', 1), 'bias': ('YB', 2)}

        def mod(dst, dkey, blk, plus_one):
            _mod_tile(cx, scB, dst, dkey, adaw_d[blk], adab_d[blk:blk + 1, :], W8[0], ('W8', 0), tm["bias"], tk["bias"], plus_one)

        mod(T["G1"][:], "G1", 0, True)
        mod(tm["sh1"], tk["sh1"], 1, False)
        mod(tm["sc1"], tk["sc1"], 2, True)
        mod(T["G3g"][:], "G3g", 3, True)
        mod(tm["shkv"], tk["shkv"], 4, False)
        mod(tm["sckv"], tk["sckv"], 5, True)
        mod(tm["shq"], tk["shq"], 6, False)
        mod(tm["scq"], tk["scq"], 7, True)
        cx.bcast_row(T["Gb0"][:], "Gb0", lng_d[0:1, :])
        cx.bcast_row(T["Bb0"][:], "Bb0", lnb_d[0:1, :])
        cx.bcast_row(T["Gb1"][:], "Gb1", lng_d[1:2, :])
        cx.bcast_row(T["Bb1"][:], "Bb1", lnb_d[1:2, :])

        def fold(G, B, Gb, Bb, sc, sh):
            p.op('dve', lambda e: e.tensor_tensor(out=T[G][:], in0=T[Gb][:], in1=tm[sc], op=ALU.mult), reads=[Gb, tk[sc]], writes=[G])
            p.op('dve', lambda e: e.tensor_tensor(out=T[B][:], in0=T[Bb][:], in1=tm[sc], op=ALU.mult), reads=[Bb, tk[sc]], writes=[B])
            p.op('dve', lambda e: e.tensor_tensor(out=T[B][:], in0=T[B][:], in1=tm[sh], op=ALU.add), reads=[B, tk[sh]], writes=[B])

        fold("G2", "B2", "Gb0", "Bb0", "sc1", "sh1")
        fold("G4", "B4", "Gb1", "Bb1", "sckv", "shkv")
        fold("G5", "B5", "Gb1", "Bb1", "scq", "shq")
        cos, sin = _rope_tables(cx, posb_d, 32, TMP[:].rearrange("p (a b) -> p a b", b=32), 'TMP',
                                YB[:, 3, :].rearrange("p (a b) -> p a b", b=32), ('YB', 3))

        w8_list = []
        wd_list = []

        def w8_src(blk_i):
            res = []
            res.append((wout_d, [(0, 512, 0)]))
            res.append((wout_d, [(512, 512, 0)]))
            for s in range(11):
                res.append((wgu_d, [(256 * s, 256, 0), (D_FF + 256 * s, 256, 256)]))
            for s in range(6):
                res.append((wkv_d, [(512 * s, 512, 0)]))
            for s in range(3):
                res.append((wq_d, [(512 * s, 512, 0)]))
            return res

        NBLK = nblk
        for b in range(NBLK):
            w8_list.extend(w8_src(b))
            for s in range(4):
                wd_list.append(s)
        w8_state = {'issued': 0}
        wd_state = {'issued': 0}

        def w8_issue(upto):
            while w8_state['issued'] <= upto and w8_state['issued'] < len(w8_list):
                i = w8_state['issued']
                src, parts = w8_list[i]
                buf = W8[i % 2]
                sv = src.rearrange("(c p) n -> p c n", p=128)
                if i < 22:
                    for (lo, n, dlo) in parts:
                        p.dma('pool', buf[:, :, dlo:dlo + n], sv[:, :, lo:lo + n], writes=[('W8', i % 2)])
                    if NBLK > 1:
                        p.dma('sp', bw8_d[i], buf[:].rearrange("p a b -> p (a b)"), reads=[('W8', i % 2)], writes=[('bw8', i)])
                else:
                    p.dma('sp', buf[:].rearrange("p a b -> p (a b)"), bw8_d[i % 22], reads=[('bw8', i % 22)], writes=[('W8', i % 2)])
                w8_state['issued'] += 1

        def wd_issue(upto):
            while wd_state['issued'] <= upto and wd_state['issued'] < len(wd_list):
                i = wd_state['issued']
                s = wd_list[i]
                buf = WD[i % 2]
                sv = wdn_d.rearrange("(c p) n -> p c n", p=128)
                if i < 4:
                    p.dma('pool', buf[:, :, :], sv[:, :, s * 256:(s + 1) * 256], writes=[('WD', i % 2)])
                    if NBLK > 1:
                        p.dma('sp', bwd_d[i], buf[:].rearrange("p a b -> p (a b)"), reads=[('WD', i % 2)], writes=[('bwd', i)])
                else:
                    p.dma('sp', buf[:].rearrange("p a b -> p (a b)"), bwd_d[i % 4], reads=[('bwd', i % 4)], writes=[('WD', i % 2)])
                wd_state['issued'] += 1

        w8_i = 0
        wd_i = 0

        def post_norm(blk_tiles_z, Gb, Bb, hs):
            pass

        for blk in range(NBLK):
            r0 = blk * 512
            xv = x_d[r0:r0 + 512, :].rearrange("(j p) d -> p j d", p=128)
            ygv = yg_d[r0:r0 + 512, :].rearrange("(j p) d -> p j d", p=128)
            for j in range(4):
                p.dma('sp', XA[:, j, :], xv[:, j, :], writes=[('XA', j)])
                p.dma('sp', YB[:, j, :], ygv[:, j, :], writes=[('YB', j)])
            for j in range(4):
                cx.copy('act' if j % 2 else 'dve', TB[:, j, :], YB[:, j, :], [('YB', j)], [('TB', j)])
            cx.to_featmajor(TB, [('TB', j) for j in range(4)], HT[0], 'HT0', 4, 8)
            for n in range(2):
                w8_issue(w8_i + 1)
                wb, wk = W8[w8_i % 2], ('W8', w8_i % 2)
                for j in range(4):
                    ps, pk = cx.psum()
                    for c in range(8):
                        p.op('pe', lambda e, c=c, j=j, ps=ps, wb=wb: e.matmul(ps[:], lhsT=HT[0][:, c, j * 128:(j + 1) * 128], rhs=wb[:, c, :],
                                                                             start=(c == 0), stop=(c == 7)),
                             reads=[('HT0', c), wk], writes=[pk])
                    sl = slice(n * 512, (n + 1) * 512)
                    p.op('dve', lambda e, j=j, ps=ps, sl=sl: e.tensor_tensor(out=YB[:, j, sl], in0=ps[:], in1=T["G1"][:, sl], op=ALU.mult),
                         reads=[pk, "G1"], writes=[('YB', j)])
                w8_i += 1
            for j in range(4):
                p.op('dve', lambda e, j=j: e.scalar_tensor_tensor(out=XA[:, j, :], in0=XA[:, j, :], scalar=ALPHA, in1=YB[:, j, :],
                                                                  op0=ALU.mult, op1=ALU.add),
                     reads=[('XA', j), ('YB', j)], writes=[('XA', j)])
                cx.layer_norm_tile(XA[:, j, :], ('XA', j), 'a')
                p.op('dve', lambda e, j=j: e.tensor_tensor(out=TMP[:], in0=XA[:, j, :], in1=T["G2"][:], op=ALU.mult),
                     reads=[('XA', j), "G2"], writes=['TMP'])
                p.op('dve', lambda e, j=j: e.tensor_tensor(out=TB[:, j, :], in0=TMP[:], in1=T["B2"][:], op=ALU.add),
                     reads=['TMP', "B2"], writes=[('TB', j)])
                p.op('dve', lambda e, j=j: e.tensor_tensor(out=XA[:, j, :], in0=XA[:, j, :], in1=T["Gb0"][:], op=ALU.mult),
                     reads=[('XA', j), "Gb0"], writes=[('XA', j)])
                p.op('dve', lambda e, j=j: e.tensor_tensor(out=XA[:, j, :], in0=XA[:, j, :], in1=T["Bb0"][:], op=ALU.add),
                     reads=[('XA', j), "Bb0"], writes=[('XA', j)])
            cx.to_featmajor(TB, [('TB', j) for j in range(4)], HT[1], 'HT1', 4, 8)
            for s in range(11):
                w8_issue(w8_i + 1)
                wb, wk = W8[w8_i % 2], ('W8', w8_i % 2)
                for fc in range(2):
                    psg, pkg = cx.psum()
                    for c in range(8):
                        p.op('pe', lambda e, c=c, fc=fc, psg=psg, wb=wb: e.matmul(psg[:], lhsT=wb[:, c, fc * 128:(fc + 1) * 128], rhs=HT[1][:, c, :],
                                                                                 start=(c == 0), stop=(c == 7)),
                             reads=[('HT1', c), wk], writes=[pkg])
                    psu, pku = cx.psum()
                    for c in range(8):
                        p.op('pe', lambda e, c=c, fc=fc, psu=psu, wb=wb: e.matmul(psu[:], lhsT=wb[:, c, 256 + fc * 128:256 + (fc + 1) * 128], rhs=HT[1][:, c, :],
                                                                                 start=(c == 0), stop=(c == 7)),
                             reads=[('HT1', c), wk], writes=[pku])
                    f = s * 2 + fc
                    sg, sgk = SG[f % 2], ('SG', f % 2)
                    p.op('act', lambda e, psg=psg, sg=sg: e.activation(out=sg[:], in_=psg[:], func=AF.Silu), reads=[pkg], writes=[sgk])
                    p.op('dve', lambda e, psu=psu, sg=sg, f=f: e.tensor_tensor(out=HID[:, f, :], in0=psu[:], in1=sg[:], op=ALU.mult),
                         reads=[pku, sgk], writes=[('HID', f)])
                w8_i += 1
            for n in range(4):
                wd_issue(wd_i + 1)
                wb, wk = WD[wd_i % 2], ('WD', wd_i % 2)
                for j in range(4):
                    ps, pk = cx.psum()
                    for f in range(22):
                        p.op('pe', lambda e, f=f, j=j, ps=ps, wb=wb: e.matmul(ps[:, 0:256], lhsT=HID[:, f, j * 128:(j + 1) * 128], rhs=wb[:, f, :],
                                                                             start=(f == 0), stop=(f == 21)),
                             reads=[('HID', f), wk], writes=[pk])
                    sl = slice(n * 256, (n + 1) * 256)
                    p.op('dve', lambda e, j=j, ps=ps, sl=sl: e.tensor_tensor(out=YB[:, j, sl], in0=ps[:, 0:256], in1=T["G3g"][:, sl], op=ALU.mult),
                         reads=[pk, "G3g"], writes=[('YB', j)])
                wd_i += 1
            x2v = x2_d[r0:r0 + 512, :].rearrange("(j p) d -> p j d", p=128)
            for which in range(2):
                G, B = ("G4", "B4") if which == 0 else ("G5", "B5")
                for j in range(4):
                    if which == 0:
                        p.op('dve', lambda e, j=j: e.scalar_tensor_tensor(out=XA[:, j, :], in0=XA[:, j, :], scalar=ALPHA, in1=YB[:, j, :],
                                                                          op0=ALU.mult, op1=ALU.add),
                             reads=[('XA', j), ('YB', j)], writes=[('XA', j)])
                        cx.layer_norm_tile(XA[:, j, :], ('XA', j), 'b')
                    p.op('dve', lambda e, j=j, G=G: e.tensor_tensor(out=TMP[:], in0=XA[:, j, :], in1=T[G][:], op=ALU.mult),
                         reads=[('XA', j), G], writes=['TMP'])
                    p.op('dve', lambda e, j=j, B=B: e.tensor_tensor(out=TB[:, j, :], in0=TMP[:], in1=T[B][:], op=ALU.add),
                         reads=['TMP', B], writes=[('TB', j)])
                    if which == 1:
                        p.op('dve', lambda e, j=j: e.tensor_tensor(out=YB[:, j, :], in0=XA[:, j, :], in1=T["Gb1"][:], op=ALU.mult),
                             reads=[('XA', j), "Gb1"], writes=[('YB', j)])
                        p.op('dve', lambda e, j=j: e.tensor_tensor(out=YB[:, j, :], in0=YB[:, j, :], in1=T["Bb1"][:], op=ALU.add),
                             reads=[('YB', j), "Bb1"], writes=[('YB', j)])
                        p.dma('sp', x2v[:, j, :], YB[:, j, :], reads=[('YB', j)])
                cx.to_featmajor(TB, [('TB', j) for j in range(4)], HT[which], 'HT%d' % which, 4, 8)
                nslab = 6 if which == 0 else 3
                for s in range(nslab):
                    w8_issue(w8_i + 1)
                    wb, wk = W8[w8_i % 2], ('W8', w8_i % 2)
                    if which == 0 and s < 3:
                        dst_d, ds, rope = k_d, s, True
                    elif which == 0:
                        dst_d, ds, rope = v_d, s - 3, False
                    else:
                        dst_d, ds, rope = q_d, s, True
                    for j in range(4):
                        ps, pk = cx.psum()
                        for c in range(8):
                            p.op('pe', lambda e, c=c, j=j, ps=ps, wb=wb, which=which: e.matmul(ps[:], lhsT=HT[which][:, c, j * 128:(j + 1) * 128], rhs=wb[:, c, :],
                                                                                              start=(c == 0), stop=(c == 7)),
                                 reads=[('HT%d' % which, c), wk], writes=[pk])
                        oap = OUTB[:, j, ds * 512:(ds + 1) * 512]
                        ok = ('OUTB', j, ds)
                        if rope:
                            tile = blk * 4 + j
                            _rope_apply(cx, ps[:], pk, oap, ok, cos[:, tile, :], sin[:, tile, :], 8, RT)
                        else:
                            cx.copy('act', oap, ps[:], [pk], [ok])
                    w8_i += 1
                    if ds == 2:
                        dv = dst_d[r0:r0 + 512, :].rearrange("(j p) d -> p j d", p=128)
                        for j in range(4):
                            p.dma('sp', dv[:, j, :], OUTB[:, j, :], reads=[('OUTB', j, 0), ('OUTB', j, 1), ('OUTB', j, 2)])
        p.finish()
        p.emit()
    return nc


def _cT(c_row):
    return np.ascontiguousarray(c_row.reshape(8, 128).T)


def _ada_blocks(ada_w, ada_b, idxs):
    w = np.ascontiguousarray(np.stack([ada_w[:, i * D:(i + 1) * D] for i in idxs]))
    b = np.ascontiguousarray(np.stack([ada_b[i * D:(i + 1) * D] for i in idxs]))
    return w, b


def maps_B(inp, yg):
    idxs = [2, 3, 4, 5, 12, 13, 6, 7]
    aw, ab = _ada_blocks(inp['ada_w'], inp['ada_b'], idxs)
    maps = []
    for c in range(NCORE):
        b, s0 = c // 4, (c % 4) * TPC
        maps.append({
            "x": np.ascontiguousarray(inp['x'][b, s0:s0 + TPC]),
            "yg": np.ascontiguousarray(yg[b, s0:s0 + TPC]),
            "cT": _cT(inp['c'][b]),
            "adaw": aw, "adab": ab,
            "lng": np.ascontiguousarray(inp['ln_g'][0]), "lnb": np.ascontiguousarray(inp['ln_b'][0]),
            "w_out": inp['rw_w_out'][0], "w_gu": inp['ffn_w_gu'][0], "w_down": inp['ffn_w_down'][0],
            "w_kv": inp['kv_w'], "w_q": inp['attn_w_q'][0],
            "posb": np.full((128, 1), float(s0), np.float32),
        })
    return maps


CH = 128
DECAY_SCALE = -math.exp(-0.5)
LNX_EPS = 64e-5


def build_A(nblk=SEQ // 512):
    nc = bass.Bass("TRN2", target_bir_lowering=False)
    dt = lambda name, shape, dtype, kind: nc.dram_tensor(name, shape, dtype, kind=kind).ap()
    xp_d = dt("xpad", [SEQ + 1, D], F32, "ExternalInput")
    cT_d = dt("cT", [128, 8], F32, "ExternalInput")
    adaw_d = dt("adaw", [2, D, D], F32, "ExternalInput")
    adab_d = dt("adab", [2, D], F32, "ExternalInput")
    muT_d = dt("muT", [128, 6, 8], F32, "ExternalInput")
    wrkv_d = dt("w_rkv", [D, 768], F32, "ExternalInput")
    wl1_d = dt("w_l1", [D, 288], F32, "ExternalInput")
    w2a_d = dt("w2a", [128, 256], F32, "ExternalInput")
    g2_d = dt("g2", [160, 256], F32, "ExternalInput")
    chv_d = dt("chv", [128, 2, 6], F32, "ExternalInput")
    lnx_d = dt("lnx", [2, 256], F32, "ExternalInput")
    yg_d = dt("yg", [SEQ, 256], F32, "ExternalOutput")

    with ExitStack() as st:
        cx = Ctx(nc, st)
        p = cx.p
        sb = cx.sb
        ident = cx.ident
        X = sb("X", [128, 4, D], F32)
        XP = sb("XP", [128, 4, D], F32)
        HB = sb("HB", [128, 4, D], BF16)
        XXB = sb("XXB", [128, 4, D], BF16)
        hT = sb("hT", [128, 8, 512], BF16)
        xxT = sb("xxT", [128, 8, 512], BF16)
        SC1 = sb("SC1", [128, D], F32)
        SH = sb("SH", [128, D], F32)
        W = sb("W", [128, 8, 1056], BF16)
        WM = sb("WM", [128, 8, 1056], BF16)
        muT = sb("muT_s", [128, 6, 8], F32)
        W2A = sb("W2A", [128, 256], BF16)
        G2a = sb("G2a", [128, 256], BF16)
        G2b = sb("G2b", [32, 256], BF16)
        chv = sb("chv_s", [128, 2, 6], F32)
        LNXG = sb("LNXG", [128, 256], F32)
        LNXB = sb("LNXB", [128, 256], F32)
        identf = sb("identf", [128, 128], F32)
        M1 = sb("M1", [128, 256], BF16)
        ML = sb("ML", [128, 128], BF16)
        RMASK = sb("RMASK", [128, 512], BF16)
        HSEL = sb("HSEL", [128, 2], F32)
        BONES = sb("BONES", [128, 128], F32)
        S = sb("S", [64, 4, 64], F32)
        Wbuf = hT
        WST = XP[:].rearrange("p a b -> p (a b)")[:, 0:1056]
        p.op('dve', lambda e: e.tensor_copy(out=identf[:], in_=ident[:]), reads=['ident'], writes=['identf'])
        p.op('pool', lambda e: e.memset(M1[:], 1.0), writes=['M1'])
        p.op('pool', lambda e: e.affine_select(out=M1[:, 0:128], in_=M1[:, 0:128], pattern=[[1, 128]], compare_op=ALU.is_gt, fill=0.0,
                                               base=0, channel_multiplier=-1), reads=['M1'], writes=['M1'])
        p.op('pool', lambda e: e.affine_select(out=M1[:, 128:256], in_=M1[:, 128:256], pattern=[[1, 128]], compare_op=ALU.is_ge, fill=0.0,
                                               base=0, channel_multiplier=-1), reads=['M1'], writes=['M1'])
        p.op('pool', lambda e: e.memset(ML[:], 1.0), writes=['ML'])
        p.op('pool', lambda e: e.affine_select(out=ML[:], in_=ML[:], pattern=[[-1, 128]], compare_op=ALU.is_gt, fill=0.0,
                                               base=0, channel_multiplier=1), reads=['ML'], writes=['ML'])
        p.op('pool', lambda e: e.memset(RMASK[:], 1.0), writes=['RMASK'])
        for q in range(4):
            p.op('pool', lambda e, q=q: e.memset(RMASK[:, q * 128:q * 128 + 1], 0.0), reads=['RMASK'], writes=['RMASK'])
        p.op('pool', lambda e: e.memset(HSEL[:], 0.0), writes=['HSEL'])
        p.op('pool', lambda e: e.memset(HSEL[0:64, 0:1], 1.0), reads=['HSEL'], writes=['HSEL'])
        p.op('pool', lambda e: e.memset(HSEL[64:128, 1:2], 1.0), reads=['HSEL'], writes=['HSEL'])
        p.op('pool', lambda e: e.memset(BONES[:], 0.0), writes=['BONES'])
        p.op('pool', lambda e: e.memset(BONES[0:64, 0:64], 1.0), reads=['BONES'], writes=['BONES'])
        p.op('pool', lambda e: e.memset(BONES[64:128, 64:128], 1.0), reads=['BONES'], writes=['BONES'])
        p.op('pool', lambda e: e.memset(S[:], 0.0), writes=[('S', h) for h in range(4)])
        p.dma('sp', muT[:], muT_d, writes=['muT'])
        p.dma('sp', chv[:], chv_d, writes=['chv'])
        p.op('dve', lambda e: e.tensor_scalar(out=chv[:, :, 5], in0=chv[:, :, 3], scalar1=-1.0, scalar2=1.0, op0=ALU.mult, op1=ALU.add),
             reads=['chv'], writes=['chv'])
        cx.bcast_row(LNXG[:], 'LNXG', lnx_d[0:1, :])
        cx.bcast_row(LNXB[:], 'LNXB', lnx_d[1:2, :])
        p.dma('pool', W2A[:], w2a_d, writes=['W2A'])
        p.dma('pool', G2a[:], g2_d[0:128, :], writes=['G2a'])
        p.dma('pool', G2b[:], g2_d[128:160, :], writes=['G2b'])
        scB = _silu_c_bcast(cx, cT_d)
        _mod_tile(cx, scB, SH[:], 'SH', adaw_d[0], adab_d[0:1, :], Wbuf, 'Wbuf', X[:, 0, :], ('X', 0), False)
        _mod_tile(cx, scB, SC1[:], 'SC1', adaw_d[1], adab_d[1:2, :], Wbuf, 'Wbuf', X[:, 1, :], ('X', 1), True)
        wr = wrkv_d.rearrange("(c p) n -> p c n", p=128)
        wl = wl1_d.rearrange("(c p) n -> p c n", p=128)
        groups = [(0, 256, 0), (256, 512, 1), (512, 768, 2), (768, 832, 3), (832, 896, 4), (896, 1056, 5)]
        for c in range(8):
            p.dma('sp', WST[:, 0:768], wr[:, c, :], writes=['WST'])
            p.dma('sp', WST[:, 768:1056], wl[:, c, :], writes=['WST'])
            p.op('act', lambda e, c=c: e.copy(out=W[:, c, :], in_=WST), reads=['WST'], writes=['W'])
            for (lo, hi, mi) in groups:
                p.op('dve', lambda e, c=c, lo=lo, hi=hi, mi=mi: e.tensor_scalar(out=WM[:, c, lo:hi], in0=WST[:, lo:hi], scalar1=muT[:, mi, c:c + 1],
                                                                                scalar2=None, op0=ALU.mult),
                     reads=['WST', 'muT'], writes=['WM'])

        p.barrier()
        fnames = ["Rf", "Kf", "Af", "LW", "CS", "E1", "E2", "E3", "E4", "KK", "KM", "Bf", "T1"]
        Fm = {n: sb("F_" + n, [128, 512], F32) for n in fnames}
        PROD = [sb("PROD%d" % m, [128, 512], F32) for m in range(2)]
        bnames = ["RT", "KT", "BT", "AT", "KH", "BH", "VT"]
        Bm = [{n: sb("B_%s%d" % (n, m), [128, 512], BF16) for n in bnames} for m in range(2)]
        LA = sb("LA", [128, 512], BF16)
        GS = sb("GS", [128, 2, 512], BF16)
        Gtok = sb("Gtok", [128, 4, 256], F32)
        TMs2 = [[sb("TMs%d_%d" % (m, i), [128, 512], BF16) for i in range(2)] for m in range(2)]
        G1s_ = [sb("G1s%d" % h, [128, 256], BF16) for h in range(8)]
        G2s_ = [sb("G2s%d" % h, [128, 256], BF16) for h in range(8)]
        Lb_ = [[sb("Lb%d_%d" % (h, i), [128, 128], BF16) for i in range(2)] for h in range(8)]
        Gb_ = [[sb("Gb%d_%d" % (h, i), [128, 128], BF16) for i in range(2)] for h in range(8)]
        TTb_ = [[sb("TTb%d_%d" % (h, i), [128, 128], BF16) for i in range(2)] for h in range(8)]
        Xs_ = [sb("Xs%d" % h, [128, 64], BF16) for h in range(8)]
        WU_ = [sb("WU%d" % h, [128, 128], BF16) for h in range(8)]
        DP_ = [sb("DP%d" % h, [128, 64], F32) for h in range(8)]
        MT_ = [sb("MT%d" % h, [64, 64], F32) for h in range(8)]
        Ns_ = [sb("Ns%d" % h, [64, 64], F32) for h in range(8)]
        QT_ = [sb("QT%d" % h, [64, 128], F32) for h in range(8)]
        gst_ = [sb("gst%d" % h, [128, 6], F32) for h in range(8)]
        gmv_ = [sb("gmv%d" % h, [128, 2], F32) for h in range(8)]
        grs_ = [sb("grs%d" % h, [128, 1], F32) for h in range(8)]
        gnm_ = [sb("gnm%d" % h, [128, 1], F32) for h in range(8)]
        YO2 = [sb("YO%d" % i, [128, 256], F32) for i in range(2)]
        SBON2 = [sb("SBON%d" % i, [128, 4], F32) for i in range(2)]
        PCs = [sb("PCs%d" % m, [128, 4], F32) for m in range(2)]
        cx.lnxeps = sb("lnxeps", [128, 1], F32)
        p.op('pool', lambda e: e.memset(cx.lnxeps[:], LNX_EPS), writes=['lnxeps'])

        def proj_fm(ps, pk, lo, n):
            for c in range(8):
                p.op('pe', lambda e, c=c: e.matmul(ps[0:n, :], lhsT=W[:, c, lo:lo + n], rhs=hT[:, c, :], start=(c == 0), stop=False),
                     reads=[('hT', c), 'W'], writes=[pk])
            for c in range(8):
                p.op('pe', lambda e, c=c: e.matmul(ps[0:n, :], lhsT=WM[:, c, lo:lo + n], rhs=xxT[:, c, :], start=False, stop=(c == 7)),
                     reads=[('xxT', c), 'WM'], writes=[pk])


        def head_chain(m, hl, q, cs_):
            B_ = Bm[m]
            bk = lambda n: ('B', n, m)
            hh = 2 * m + hl
            P = slice(64 * hl, 64 * hl + 64)
            TMq = TMs2[m][q % 2]
            YO = YO2[q % 2]
            SBON = SBON2[q % 2]
            At_h = TMq[:, 64 * hl:64 * hl + 64]
            Bh_h = TMq[:, 128 + 64 * hl:128 + 64 * hl + 64]
            Kh_h = TMq[:, 256 + 64 * hl:256 + 64 * hl + 64]
            V_h = TMq[:, 384 + 64 * hl:384 + 64 * hl + 64]
            tmk = ('TMs', m, q % 2)
            hb = hh + 4 * (q % 2)
            G1s, G2s, Lb, Gb, TTb, Xs, WU, DP, MT, Ns, QT = G1s_[hb], G2s_[hb], Lb_[hb], Gb_[hb], TTb_[hb], Xs_[hb], WU_[hb], DP_[hb], MT_[hb], Ns_[hb], QT_[hb]
            gst, gmv, grs, gnm = gst_[hb], gmv_[hb], grs_[hb], gnm_[hb]
            K_ = lambda n: (n, hb)
            ps, pk = cx.psum()
            p.op('pe', lambda e, ps=ps: e.matmul(ps[:, 0:128], lhsT=B_["BT"][P, cs_], rhs=B_["AT"][P, cs_], start=True, stop=True), reads=[bk("BT"), bk("AT")], writes=[pk])
            p.op('pe', lambda e, ps=ps: e.matmul(ps[:, 128:256], lhsT=B_["BT"][P, cs_], rhs=B_["RT"][P, cs_], start=True, stop=True), reads=[bk("BT"), bk("RT")], writes=[pk])
            p.op('dve', lambda e, ps=ps: e.tensor_tensor(out=G1s[:], in0=ps[:, 0:256], in1=M1[:], op=ALU.mult), reads=[pk, 'M1'], writes=[K_('G1s')])
            ps, pk = cx.psum()
            p.op('pe', lambda e, ps=ps: e.matmul(ps[:, 0:128], lhsT=B_["KT"][P, cs_], rhs=B_["AT"][P, cs_], start=True, stop=True), reads=[bk("KT"), bk("AT")], writes=[pk])
            p.op('pe', lambda e, ps=ps: e.matmul(ps[:, 128:256], lhsT=B_["KT"][P, cs_], rhs=B_["RT"][P, cs_], start=True, stop=True), reads=[bk("KT"), bk("RT")], writes=[pk])
            p.op('dve', lambda e, ps=ps: e.tensor_tensor(out=G2s[:], in0=ps[:, 0:256], in1=M1[:], op=ALU.mult), reads=[pk, 'M1'], writes=[K_('G2s')])
            ps, pk = cx.psum()
            p.op('pe', lambda e, ps=ps: e.matmul(ps[:, 0:128], lhsT=B_["AT"][P, cs_], rhs=B_["BT"][P, cs_], start=True, stop=True), reads=[bk("BT"), bk("AT")], writes=[pk])
            p.op('dve', lambda e, ps=ps: e.tensor_tensor(out=Lb[0][:], in0=ps[:, 0:128], in1=ML[:], op=ALU.mult), reads=[pk, 'ML'], writes=[K_('Lb0')])
            yield
            p.op('act', lambda e: e.copy(out=Gb[0][:], in_=G1s[:, 0:128]), reads=[K_('G1s')], writes=[K_('Gb0')])
            p.op('dve', lambda e: e.tensor_tensor(out=TTb[0][:], in0=G1s[:, 0:128], in1=ident[:], op=ALU.add), reads=[K_('G1s'), 'ident'], writes=[K_('TTb0')])
            ps, pk = cx.psum()
            p.op('pe', lambda e, ps=ps: e.matmul(ps[:, 0:64], lhsT=G2s[:, 0:128], rhs=V_h, start=True, stop=True), reads=[K_('G2s'), tmk], writes=[pk])
            p.op('act', lambda e, ps=ps: e.copy(out=Xs[:], in_=ps[:, 0:64]), reads=[pk], writes=[K_('Xs')])
            yield
            cur = 0
            for lvl in range(1, 7):
                nxt = 1 - cur
                ps, pk = cx.psum()
                p.op('pe', lambda e, ps=ps, cur=cur: e.matmul(ps[:, 0:128], lhsT=Gb[cur][:], rhs=Lb[cur][:], start=True, stop=True), reads=[K_('Gb%d' % cur), K_('Lb%d' % cur)], writes=[pk])
                p.op('act', lambda e, ps=ps, nxt=nxt: e.copy(out=Lb[nxt][:], in_=ps[:, 0:128]), reads=[pk], writes=[K_('Lb%d' % nxt)])
                if lvl < 6:
                    ps2, pk2 = cx.psum()
                    p.op('pe', lambda e, ps2=ps2, cur=cur: e.matmul(ps2[:, 0:128], lhsT=Lb[cur][:], rhs=Gb[cur][:], start=True, stop=True), reads=[K_('Gb%d' % cur), K_('Lb%d' % cur)], writes=[pk2])
                    p.op('dve', lambda e, ps2=ps2, nxt=nxt: e.tensor_copy(out=Gb[nxt][:], in_=ps2[:, 0:128]), reads=[pk2], writes=[K_('Gb%d' % nxt)])
                yield
                ps3, pk3 = cx.psum()
                p.op('pe', lambda e, ps3=ps3, cur=cur, nxt=nxt: e.matmul(ps3[:, 0:128], lhsT=Lb[nxt][:], rhs=TTb[cur][:], start=True, stop=True), reads=[K_('Lb%d' % nxt), K_('TTb%d' % cur)], writes=[pk3])
                p.op('dve', lambda e, ps3=ps3, cur=cur, nxt=nxt: e.tensor_tensor(out=TTb[nxt][:], in0=ps3[:, 0:128], in1=TTb[cur][:], op=ALU.add), reads=[pk3, K_('TTb%d' % cur)], writes=[K_('TTb%d' % nxt)])
                cur = nxt
                yield
            TT = TTb[cur]
            ttk = K_('TTb%d' % cur)
            ps, pk = cx.psum()
            p.op('pe', lambda e, ps=ps: e.matmul(ps[:, 0:64], lhsT=TT[:], rhs=At_h, start=True, stop=True), reads=[ttk, tmk], writes=[pk])
            p.op('pe', lambda e, ps=ps: e.matmul(ps[:, 64:128], lhsT=TT[:], rhs=Xs[:], start=True, stop=True), reads=[ttk, K_('Xs')], writes=[pk])
            p.op('dve', lambda e, ps=ps: e.tensor_copy(out=WU[:], in_=ps[:, 0:128]), reads=[pk], writes=[K_('WU')])
            p.op('dve', lambda e: e.tensor_scalar(out=DP[P, :], in0=identf[P, 64 * hl:64 * hl + 64], scalar1=PCs[m][P, q:q + 1], scalar2=None, op0=ALU.mult),
                 reads=['identf', ('PC', m)], writes=[K_('DP')])
            yield
            ps, pk = cx.psum()
            p.op('pe', lambda e, ps=ps: e.matmul(ps[0:64, 0:64], lhsT=WU[:, 0:64], rhs=Bh_h, start=True, stop=True), reads=[K_('WU'), tmk], writes=[pk])
            p.op('pe', lambda e, ps=ps: e.matmul(ps[0:64, 64:128], lhsT=identf[P, 64 * hl:64 * hl + 64], rhs=DP[P, :], start=True, stop=True), reads=['identf', K_('DP')], writes=[pk])
            p.op('act', lambda e, ps=ps: e.copy(out=MT[:], in_=ps[0:64, 0:64]), reads=[pk], writes=[K_('MT')])
            p.op('dve', lambda e, ps=ps: e.tensor_tensor(out=MT[:], in0=ps[0:64, 64:128], in1=MT[:], op=ALU.add), reads=[pk, K_('MT')], writes=[K_('MT')])
            ps, pk = cx.psum()
            p.op('pe', lambda e, ps=ps: e.matmul(ps[0:64, 0:64], lhsT=Bh_h, rhs=WU[:, 64:128], start=True, stop=False), reads=[K_('WU'), tmk], writes=[pk])
            p.op('pe', lambda e, ps=ps: e.matmul(ps[0:64, 0:64], lhsT=Kh_h, rhs=V_h, start=False, stop=True), reads=[tmk], writes=[pk])
            p.op('act', lambda e, ps=ps: e.copy(out=Ns[:], in_=ps[0:64, 0:64]), reads=[pk], writes=[K_('Ns')])
            ps, pk = cx.psum()
            p.op('pe', lambda e, ps=ps: e.matmul(ps[0:64, 0:128], lhsT=ident[:, 64 * hl:64 * hl + 64], rhs=B_["RT"][:, cs_], start=True, stop=False), reads=['ident', bk("RT")], writes=[pk])
            p.op('pe', lambda e, ps=ps: e.matmul(ps[0:64, 0:128], lhsT=WU[:, 0:64], rhs=G1s[:, 128:256], start=False, stop=True), reads=[K_('WU'), K_('G1s')], writes=[pk])
            p.op('act', lambda e, ps=ps: e.copy(out=QT[:], in_=ps[0:64, 0:128]), reads=[pk], writes=[K_('QT')])
            yield
            ps, pk = cx.psum()
            p.op('pe', lambda e, ps=ps: e.matmul(ps[:, 0:64], lhsT=G1s[:, 128:256], rhs=WU[:, 64:128], start=True, stop=False), reads=[K_('G1s'), K_('WU')], writes=[pk])
            p.op('pe', lambda e, ps=ps: e.matmul(ps[:, 0:64], lhsT=G2s[:, 128:256], rhs=V_h, start=False, stop=False), reads=[K_('G2s'), tmk], writes=[pk])
            p.op('pe', lambda e, ps=ps: e.matmul(ps[:, 0:64], lhsT=QT[:], rhs=S[:, hh, :], start=False, stop=True), reads=[K_('QT'), ('S', hh)], writes=[pk])
            yo = YO[:, hh * 64:(hh + 1) * 64]
            yk = ('YO', hh, q % 2)
            p.op('act', lambda e, ps=ps: e.copy(out=yo, in_=ps[:, 0:64]), reads=[pk], writes=[yk])
            ps, pk = cx.psum()
            p.op('pe', lambda e, ps=ps: e.matmul(ps[0:64, 0:64], lhsT=MT[:], rhs=S[:, hh, :], start=True, stop=True), reads=[K_('MT'), ('S', hh)], writes=[pk])
            p.op('dve', lambda e, ps=ps: e.tensor_tensor(out=S[:, hh, :], in0=ps[0:64, 0:64], in1=Ns[:], op=ALU.add), reads=[pk, K_('Ns')], writes=[('S', hh)])
            yield
            p.op('dve', lambda e: e.bn_stats(out=gst[:], in_=yo), reads=[yk], writes=[K_('gst')])
            p.op('dve', lambda e: e.bn_aggr(out=gmv[:], in_=gst[:]), reads=[K_('gst')], writes=[K_('gmv')])
            p.op('act', lambda e: e.activation(out=grs[:], in_=gmv[:, 1:2], func=AF.Sqrt, bias=cx.lnxeps[:], scale=1.0), reads=[K_('gmv'), 'lnxeps'], writes=[K_('grs')])
            yield
            p.op('dve', lambda e: e.reciprocal(out=grs[:], in_=grs[:]), reads=[K_('grs')], writes=[K_('grs')])
            p.op('dve', lambda e: e.tensor_scalar(out=gnm[:], in0=gmv[:, 0:1], scalar1=grs[:], scalar2=-1.0, op0=ALU.mult, op1=ALU.mult), reads=[K_('gmv'), K_('grs')], writes=[K_('gnm')])
            p.op('act', lambda e: e.activation(out=yo, in_=yo, func=AF.Identity, bias=gnm[:], scale=grs[:]), reads=[yk, K_('grs'), K_('gnm')], writes=[yk])
            yield
            sl = slice(hh * 64, (hh + 1) * 64)
            p.op('dve', lambda e: e.tensor_tensor(out=yo, in0=yo, in1=LNXG[:, sl], op=ALU.mult), reads=[yk, 'LNXG'], writes=[yk])
            p.op('dve', lambda e: e.tensor_tensor(out=yo, in0=yo, in1=LNXB[:, sl], op=ALU.add), reads=[yk, 'LNXB'], writes=[yk])
            p.op('dve', lambda e: e.scalar_tensor_tensor(out=yo, in0=V_h, scalar=SBON[:, hh:hh + 1], in1=yo, op0=ALU.mult, op1=ALU.add),
                 reads=[yk, tmk, ('SBON', m, q % 2)], writes=[yk])

        for blk in range(nblk):
            r0 = blk * 512
            xv = xp_d[r0 + 1:r0 + 513, :].rearrange("(j p) d -> p j d", p=128)
            xpv = xp_d[r0:r0 + 512, :].rearrange("(j p) d -> p j d", p=128)
            for j in range(4):
                p.dma('sp', X[:, j, :], xv[:, j, :], writes=[('X', j)])
                p.dma('sp', XP[:, j, :], xpv[:, j, :], writes=[('XP', j)])
            for j in range(4):
                p.op('dve', lambda e, j=j: e.tensor_tensor(out=X[:, j, :], in0=X[:, j, :], in1=SC1[:], op=ALU.mult), reads=[('X', j), 'SC1'], writes=[('X', j)])
                p.op('pool', lambda e, j=j: e.tensor_tensor(out=XP[:, j, :], in0=XP[:, j, :], in1=SC1[:], op=ALU.mult), reads=[('XP', j), 'SC1'], writes=[('XP', j)])
                p.op('dve', lambda e, j=j: e.tensor_tensor(out=HB[:, j, :], in0=X[:, j, :], in1=SH[:], op=ALU.add), reads=[('X', j), 'SH'], writes=[('HB', j)])
                p.op('pool', lambda e, j=j: e.tensor_tensor(out=XXB[:, j, :], in0=XP[:, j, :], in1=X[:, j, :], op=ALU.subtract), reads=[('XP', j), ('X', j)], writes=[('XXB', j)])
            if blk == 0:
                p.op('dve', lambda e: e.tensor_scalar(out=XXB[0:1, 0, :], in0=HB[0:1, 0, :], scalar1=-1.0, scalar2=None, op0=ALU.mult),
                     reads=[('HB', 0), ('XXB', 0)], writes=[('XXB', 0)])
            cx.to_featmajor(HB, [('HB', j) for j in range(4)], hT, 'hT', 4, 8)
            cx.to_featmajor(XXB, [('XXB', j) for j in range(4)], xxT, 'xxT', 4, 8)
            ps, pk = cx.psum()
            proj_fm(ps, pk, 768, 128)
            p.op('act', lambda e, ps=ps: e.activation(out=LA[0:64, :], in_=ps[0:64, :], func=AF.Tanh), reads=[pk], writes=['LA'])
            p.op('act', lambda e, ps=ps: e.copy(out=LA[64:128, :], in_=ps[64:128, :]), reads=[pk], writes=['LA'])
            ps, pk = cx.psum()
            proj_fm(ps, pk, 896, 128)
            p.op('act', lambda e, ps=ps: e.activation(out=GS[:, 0, :], in_=ps[:], func=AF.Sigmoid), reads=[pk], writes=['GS'])
            ps, pk = cx.psum()
            proj_fm(ps, pk, 1024, 32)
            p.op('act', lambda e, ps=ps: e.activation(out=GS[0:32, 1, :], in_=ps[0:32, :], func=AF.Sigmoid), reads=[pk], writes=['GS'])
            for j in range(4):
                ps, pk = cx.psum()
                p.op('pe', lambda e, j=j, ps=ps: e.matmul(ps[:, 0:256], lhsT=GS[:, 0, j * 128:(j + 1) * 128], rhs=G2a[:], start=True, stop=False),
                     reads=['GS', 'G2a'], writes=[pk])
                p.op('pe', lambda e, j=j, ps=ps: e.matmul(ps[:, 0:256], lhsT=GS[0:32, 1, j * 128:(j + 1) * 128], rhs=G2b[:], start=False, stop=True),
                     reads=['GS', 'G2b'], writes=[pk])
                p.op('act', lambda e, j=j, ps=ps: e.copy(out=Gtok[:, j, :], in_=ps[:, 0:256]), reads=[pk], writes=[('Gtok', j)])
            for m in range(2):
                B_ = Bm[m]
                bk = lambda n, m=m: ('B', n, m)
                ps, pk = cx.psum()
                proj_fm(ps, pk, m * 128, 128)
                p.op('act', lambda e, ps=ps: e.copy(out=Fm["Rf"][:], in_=ps[:]), reads=[pk], writes=['Rf'])
                ps, pk = cx.psum()
                proj_fm(ps, pk, 256 + m * 128, 128)
                p.op('act', lambda e, ps=ps: e.copy(out=Fm["Kf"][:], in_=ps[:]), reads=[pk], writes=['Kf'])
                ps, pk = cx.psum()
                proj_fm(ps, pk, 512 + m * 128, 128)
                p.op('act', lambda e, ps=ps, B_=B_: e.copy(out=B_["VT"][:], in_=ps[:]), reads=[pk], writes=[bk("VT")])
                ps, pk = cx.psum()
                p.op('pe', lambda e, ps=ps, m=m: e.matmul(ps[:], lhsT=W2A[0:64, m * 128:(m + 1) * 128], rhs=LA[0:64, :], start=True, stop=True),
                     reads=['W2A', 'LA'], writes=[pk])
                p.op('act', lambda e, ps=ps, m=m: e.activation(out=Fm["LW"][:], in_=ps[:], func=AF.Sigmoid, bias=chv[:, m, 0:1], scale=1.0),
                     reads=[pk, 'chv'], writes=['LW'])
                ps, pk = cx.psum()
                p.op('pe', lambda e, ps=ps, m=m: e.matmul(ps[:], lhsT=W2A[64:128, m * 128:(m + 1) * 128], rhs=LA[64:128, :], start=True, stop=True),
                     reads=['W2A', 'LA'], writes=[pk])
                p.op('act', lambda e, ps=ps, m=m: e.activation(out=Fm["Af"][:], in_=ps[:], func=AF.Sigmoid, bias=chv[:, m, 1:2], scale=1.0),
                     reads=[pk, 'chv'], writes=['Af'])
                F_ = Fm
                p.op('dve', lambda e: e.tensor_scalar(out=F_["LW"][:], in0=F_["LW"][:], scalar1=DECAY_SCALE, scalar2=None, op0=ALU.mult), reads=['LW'], writes=['LW'])
                p.op('dve', lambda e: e.tensor_tensor_scan(out=F_["CS"][:], data0=RMASK[:], data1=F_["LW"][:], initial=0.0, op0=ALU.mult, op1=ALU.add),
                     reads=['LW', 'RMASK'], writes=['CS'])
                p.op('act', lambda e: e.activation(out=F_["E1"][:], in_=F_["CS"][:], func=AF.Exp), reads=['CS'], writes=['E1'])
                p.op('act', lambda e: e.activation(out=F_["E2"][:], in_=F_["CS"][:], func=AF.Exp, scale=-1.0), reads=['CS'], writes=['E2'])
                p.op('pool', lambda e: e.tensor_tensor(out=F_["E3"][:], in0=F_["CS"][:], in1=F_["LW"][:], op=ALU.subtract), reads=['CS', 'LW'], writes=['E3'])
                p.op('act', lambda e: e.activation(out=F_["E3"][:], in_=F_["E3"][:], func=AF.Exp), reads=['E3'], writes=['E3'])
                for q in range(4):
                    p.op('dve', lambda e, q=q: e.tensor_scalar(out=F_["E4"][:, q * 128:(q + 1) * 128], in0=F_["E2"][:, q * 128:(q + 1) * 128],
                                                               scalar1=F_["E1"][:, q * 128 + 127:q * 128 + 128], scalar2=None, op0=ALU.mult),
                         reads=['E1', 'E2'], writes=['E4'])
                p.op('dve', lambda e, m=m: e.tensor_scalar(out=F_["KK"][:], in0=F_["Kf"][:], scalar1=chv[:, m, 2:3], scalar2=None, op0=ALU.mult), reads=['Kf', 'chv'], writes=['KK'])
                p.op('pool', lambda e: e.tensor_tensor(out=F_["T1"][:], in0=F_["KK"][:], in1=F_["KK"][:], op=ALU.mult), reads=['KK'], writes=['T1'])
                ps, pk = cx.psum()
                p.op('pe', lambda e, ps=ps: e.matmul(ps[:], lhsT=BONES[:], rhs=F_["T1"][:], start=True, stop=True), reads=['BONES', 'T1'], writes=[pk])
                p.op('act', lambda e, ps=ps: e.activation(out=F_["T1"][:], in_=ps[:], func=AF.Sqrt), reads=[pk], writes=['T1'])
                p.op('dve', lambda e: e.tensor_scalar(out=F_["T1"][:], in0=F_["T1"][:], scalar1=1e-12, scalar2=None, op0=ALU.max), reads=['T1'], writes=['T1'])
                p.op('dve', lambda e: e.reciprocal(out=F_["T1"][:], in_=F_["T1"][:]), reads=['T1'], writes=['T1'])
                p.op('dve', lambda e: e.tensor_tensor(out=F_["KK"][:], in0=F_["KK"][:], in1=F_["T1"][:], op=ALU.mult), reads=['KK', 'T1'], writes=['KK'])
                p.op('dve', lambda e, m=m: e.tensor_scalar(out=F_["T1"][:], in0=F_["Af"][:], scalar1=chv[:, m, 3:4], scalar2=chv[:, m, 5:6], op0=ALU.mult, op1=ALU.add),
                     reads=['Af', 'chv'], writes=['T1'])
                p.op('pool', lambda e: e.tensor_tensor(out=F_["KM"][:], in0=F_["Kf"][:], in1=F_["T1"][:], op=ALU.mult), reads=['Kf', 'T1'], writes=['KM'])
                p.op('pool', lambda e: e.tensor_tensor(out=F_["Bf"][:], in0=F_["KK"][:], in1=F_["Af"][:], op=ALU.mult), reads=['KK', 'Af'], writes=['Bf'])
                p.op('dve', lambda e, B_=B_: e.tensor_tensor(out=B_["RT"][:], in0=F_["Rf"][:], in1=F_["E1"][:], op=ALU.mult), reads=['Rf', 'E1'], writes=[bk("RT")])
                p.op('pool', lambda e, B_=B_: e.tensor_tensor(out=B_["KT"][:], in0=F_["KM"][:], in1=F_["E2"][:], op=ALU.mult), reads=['KM', 'E2'], writes=[bk("KT")])
                p.op('dve', lambda e, B_=B_: e.tensor_tensor(out=B_["BT"][:], in0=F_["Bf"][:], in1=F_["E2"][:], op=ALU.mult), reads=['Bf', 'E2'], writes=[bk("BT")])
                p.op('dve', lambda e, B_=B_: e.scalar_tensor_tensor(out=B_["AT"][:], in0=F_["KK"][:], scalar=-1.0, in1=F_["E3"][:], op0=ALU.mult, op1=ALU.mult),
                     reads=['KK', 'E3'], writes=[bk("AT")])
                p.op('pool', lambda e, B_=B_: e.tensor_tensor(out=B_["KH"][:], in0=F_["KM"][:], in1=F_["E4"][:], op=ALU.mult), reads=['KM', 'E4'], writes=[bk("KH")])
                p.op('dve', lambda e, B_=B_: e.tensor_tensor(out=B_["BH"][:], in0=F_["Bf"][:], in1=F_["E4"][:], op=ALU.mult), reads=['Bf', 'E4'], writes=[bk("BH")])
                p.op('dve', lambda e, m=m: e.scalar_tensor_tensor(out=PROD[m][:], in0=F_["Rf"][:], scalar=chv[:, m, 4:5], in1=F_["KM"][:], op0=ALU.mult, op1=ALU.mult),
                     reads=['Rf', 'KM', 'chv'], writes=[('PROD', m)])
                for q in range(4):
                    pass
                p.op('act', lambda e, m=m: e.copy(out=PCs[m][:], in_=F_["E1"][:].rearrange("p (q t) -> p q t", t=128)[:, :, 127]), reads=['E1'], writes=[('PC', m)])

            for q0 in (0, 2):
                for q in (q0, q0 + 1):
                    cs_ = slice(q * 128, (q + 1) * 128)
                    for m in range(2):
                        B_ = Bm[m]
                        bk = lambda n, m=m: ('B', n, m)
                        bank, bkk = cx.ptbank()
                        bv = bank[:].rearrange("p a b -> p (a b)")
                        for i, n in enumerate(["AT", "BH", "KH", "VT"]):
                            p.op('pe', lambda e, cs_=cs_, q=q, i=i, n=n, B_=B_, bv=bv: e.transpose(out=bv[:, i * 128:(i + 1) * 128], in_=B_[n][:, cs_], identity=ident[:]),
                                 reads=[bk(n), 'ident'], writes=[bkk])
                        cx.copy('act', TMs2[m][q % 2][:], bv[:, 0:512], [bkk], [('TMs', m, q % 2)])
                        ps, pk = cx.psum()
                        p.op('pe', lambda e, cs_=cs_, q=q, ps=ps, m=m: e.matmul(ps[:, 0:2], lhsT=PROD[m][:, cs_], rhs=HSEL[:], start=True, stop=True), reads=[('PROD', m), 'HSEL'], writes=[pk])
                        p.op('dve', lambda e, cs_=cs_, q=q, ps=ps, m=m: e.tensor_copy(out=SBON2[q % 2][:, 2 * m:2 * m + 2], in_=ps[:, 0:2]), reads=[pk], writes=[('SBON', m, q % 2)])

                alive = [head_chain(m, hl, q, slice(q * 128, (q + 1) * 128)) for q in (q0, q0 + 1) for m in range(2) for hl in range(2)]
                while alive:
                    nxt_alive = []
                    for g_ in alive:
                        try:
                            next(g_)
                            nxt_alive.append(g_)
                        except StopIteration:
                            pass
                    alive = nxt_alive
                for q in (q0, q0 + 1):
                    cs_ = slice(q * 128, (q + 1) * 128)
                    yks = [('YO', h, q % 2) for h in range(4)]
                    p.op('dve', lambda e, cs_=cs_, q=q: e.tensor_tensor(out=YO2[q % 2][:], in0=YO2[q % 2][:], in1=Gtok[:, q, :], op=ALU.mult), reads=yks + [('Gtok', q)], writes=yks)
                    p.dma('sp', yg_d[r0 + q * 128:r0 + (q + 1) * 128, :], YO2[q % 2][:], reads=yks)
        p.finish()
        p.emit()
    return nc


def maps_A(inp):
    aw, ab = _ada_blocks(inp['ada_w'], inp['ada_b'], [0, 1])
    muT = np.ascontiguousarray(inp['rw_mu'][0].reshape(6, 8, 128).transpose(2, 0, 1))
    wl1 = np.ascontiguousarray(np.concatenate([inp['rw_w1'][0], inp['rw_a1'][0], inp['rw_g1'][0]], axis=1))
    maps = []
    for c in range(NCORE):
        b, hg = c // 4, c % 4
        cols = slice(256 * hg, 256 * hg + 256)
        wr = inp['rw_w_rkv'][0]
        wrkv = np.ascontiguousarray(np.concatenate([wr[:, 0 * D:1 * D][:, cols], wr[:, 1 * D:2 * D][:, cols], wr[:, 2 * D:3 * D][:, cols]], axis=1))
        w2a = np.ascontiguousarray(np.concatenate([inp['rw_w2'][0][:, cols], inp['rw_a2'][0][:, cols]], axis=0))
        vec = lambda v: v.reshape(-1)[cols].reshape(2, 128).T
        chv = np.zeros((128, 2, 6), np.float32)
        for i, v in enumerate([inp['rw_w0'][0], inp['rw_a0'][0], inp['rw_k_k'][0], inp['rw_k_a'][0], inp['rw_r_k'][0]]):
            chv[:, :, i] = vec(v)
        xpad = np.concatenate([np.zeros((1, D), np.float32), inp['x'][b]], axis=0)
        maps.append({
            "xpad": xpad, "cT": _cT(inp['c'][b]), "adaw": aw, "adab": ab, "muT": muT,
            "w_rkv": wrkv, "w_l1": wl1, "w2a": w2a, "g2": np.ascontiguousarray(inp['rw_g2'][0][:, cols]),
            "chv": chv, "lnx": np.ascontiguousarray(np.stack([inp['rw_lnx_g'][0][cols], inp['rw_lnx_b'][0][cols]])),
        })
    return maps


HALO = 2048
NEG = -30000.0
REC = 66
PASS_T = 1024


def build_C(do_attn=True, npass=TPC // PASS_T, nexp=NEXP, ntile_attn=32):
    nc = bass.Bass("TRN2", target_bir_lowering=False)
    dt = lambda name, shape, dtype, kind: nc.dram_tensor(name, shape, dtype, kind=kind).ap()
    x2_d = dt("x2", [TPC, D], F32, "ExternalInput")
    q_d = dt("q", [TPC, 1536], BF16, "ExternalInput")
    kh_d = dt("kh", [TPC + HALO, 1536], BF16, "ExternalInput")
    vh_d = dt("vh", [TPC + HALO, 1536], BF16, "ExternalInput")
    hb_d = dt("halo_bias", [128, 1], F32, "ExternalInput")
    cT_d = dt("cT", [128, 8], F32, "ExternalInput")
    adaw_d = dt("adaw", [4, D, D], F32, "ExternalInput")
    adab_d = dt("adab", [4, D], F32, "ExternalInput")
    lng_d = dt("lng", [2, D], F32, "ExternalInput")
    lnb_d = dt("lnb", [2, D], F32, "ExternalInput")
    wo_d = dt("w_o", [512, D], F32, "ExternalInput")
    rt_d = dt("router", [8, D], F32, "ExternalInput")
    wgu_d = dt("moe_gu", [NEXP, D, 2 * D_FFE], F32, "ExternalInput")
    wdn_d = dt("moe_dn", [NEXP, D_FFE, D], F32, "ExternalInput")
    out_d = dt("out", [TPC, D], F32, "ExternalOutput")
    scr_d = dt("scr_att", [3, TPC, 8 * REC], F32, "Internal")
    x3_d = dt("scr_x3", [TPC, D], F32, "Internal")
    sw8_d = dt("scr_w8", [NEXP * (D_FFE // 256), 128, 8 * 512], BF16, "Internal")
    swd_d = dt("scr_wd", [NEXP * (D_FFE // 256), 128, 2 * D], BF16, "Internal")

    with ExitStack() as st:
        cx = Ctx(nc, st)
        p = cx.p
        sb = cx.sb
        ident = cx.ident
        names = ["G1", "Gb0", "Bb0", "G2", "B2", "G3g", "Gb1", "Bb1"]
        T = {n: sb(n, [128, D], F32) for n in names}
        RB = sb("RB", [128, 8, D], F32)
        XA = sb("XA", [128, D], F32)
        YB = sb("YB", [128, D], F32)
        TMP = sb("TMP", [128, D], F32)
        W8 = [sb("W8_%d" % i, [128, 8, 512], BF16) for i in range(2)]
        scB = _silu_c_bcast(cx, cT_d)

        def mod(dst, dkey, blk, plus_one):
            _mod_tile(cx, scB, dst, dkey, adaw_d[blk], adab_d[blk:blk + 1, :], W8[0], ('W8', 0), TMP[:], 'TMP', plus_one)

        mod(T["G1"][:], "G1", 0, True)
        mod(XA[:], 'XA', 1, False)
        mod(YB[:], 'YB', 2, True)
        mod(T["G3g"][:], "G3g", 3, True)
        cx.bcast_row(T["Gb0"][:], "Gb0", lng_d[0:1, :])
        cx.bcast_row(T["Bb0"][:], "Bb0", lnb_d[0:1, :])
        cx.bcast_row(T["Gb1"][:], "Gb1", lng_d[1:2, :])
        cx.bcast_row(T["Bb1"][:], "Bb1", lnb_d[1:2, :])
        p.op('dve', lambda e: e.tensor_tensor(out=T["G2"][:], in0=T["Gb0"][:], in1=YB[:], op=ALU.mult), reads=["Gb0", 'YB'], writes=["G2"])
        p.op('dve', lambda e: e.tensor_tensor(out=T["B2"][:], in0=T["Bb0"][:], in1=YB[:], op=ALU.mult), reads=["Bb0", 'YB'], writes=["B2"])
        p.op('dve', lambda e: e.tensor_tensor(out=T["B2"][:], in0=T["B2"][:], in1=XA[:], op=ALU.add), reads=["B2", 'XA'], writes=["B2"])
        for e_ in range(8):
            cx.bcast_row(RB[:, e_, :], ('RB', e_), rt_d[e_:e_ + 1, :])
        WO = sb("WO", [128, 4, D], BF16)
        p.dma('pool', WO[:, :, 0:512], wo_d.rearrange("(c p) n -> p c n", p=128)[:, :, 0:512], writes=['WO'])
        p.dma('pool', WO[:, :, 512:1024], wo_d.rearrange("(c p) n -> p c n", p=128)[:, :, 512:1024], writes=['WO'])

        if do_attn:
          with ExitStack() as st2:
            sb2 = lambda name, shape, dt_: st2.enter_context(nc.sbuf_tensor(name, shape, dt_))
            MASK = sb2("MASK", [128, 256], F32)
            MASKF = sb2("MASKF", [128, 256], F32)
            hbias = sb2("hbias", [128, 1], F32)
            p.dma('sp', hbias[:], hb_d, writes=['hbias'])
            p.op('pool', lambda e: e.memset(MASK[:], 0.0), writes=['MASK'])
            p.op('pool', lambda e: e.affine_select(out=MASK[:], in_=MASK[:], pattern=[[1, 256]], compare_op=ALU.is_ge, fill=NEG, base=0, channel_multiplier=-1),
                 reads=['MASK'], writes=['MASK'])
            p.op('pool', lambda e: e.affine_select(out=MASK[:], in_=MASK[:], pattern=[[-1, 256]], compare_op=ALU.is_ge, fill=NEG, base=128, channel_multiplier=1),
                 reads=['MASK'], writes=['MASK'])
            p.op('dve', lambda e: e.tensor_copy(out=MASKF[:], in_=MASK[:]), reads=['MASK'], writes=['MASKF'])
            p.op('dve', lambda e: e.tensor_scalar(out=MASKF[:, 0:128], in0=MASKF[:, 0:128], scalar1=hbias[:], scalar2=None, op0=ALU.add),
                 reads=['MASKF', 'hbias'], writes=['MASKF'])
            QB = [sb2("QB%d" % i, [128, 512], BF16) for i in range(2)]
            KB = [sb2("KB%d" % i, [128, 2, 512], BF16) for i in range(2)]
            VB = [sb2("VB%d" % i, [128, 2, 512], BF16) for i in range(2)]
            QTs = [sb2("QT%d" % i, [128, 4, 128], BF16) for i in range(2)]
            KTs = [sb2("KT%d" % i, [128, 4, 256], BF16) for i in range(2)]
            SMs = [sb2("SM%d" % h, [128, 256], F32) for h in range(8)]
            PBs = [sb2("PB%d" % h, [128, 256], BF16) for h in range(8)]
            PTs = [sb2("PT%d" % h, [128, 2, 128], BF16) for h in range(8)]
            OUTA = [sb2("OUTA%d" % i, [128, 8, REC], F32) for i in range(2)]
            mxs = [sb2("mx%d" % h, [128, 1], F32) for h in range(8)]
            nmxs = [sb2("nmx%d" % h, [128, 1], F32) for h in range(8)]

            def attn_head(h, bi, mk, mkk, oa):
                hp, hl = h // 2, h % 2
                P = slice(64 * hl, 64 * hl + 64)
                QT, KT, SM, PB, PT, mx, nmx = QTs[bi], KTs[bi], SMs[h], PBs[h], PTs[h], mxs[h], nmxs[h]
                qk, kk_, oak = ('QT', bi), ('KT', bi), ('OUTA', bi, h)
                K_ = lambda n: (n, h)
                ps, pk = cx.psum()
                p.op('pe', lambda e, ps=ps: e.matmul(ps[:, 0:256], lhsT=QT[P, hp, :], rhs=KT[P, hp, :], start=True, stop=True), reads=[qk, kk_], writes=[pk])
                p.op('dve', lambda e, ps=ps: e.tensor_tensor(out=SM[:], in0=ps[:, 0:256], in1=mk[:], op=ALU.add), reads=[pk, mkk], writes=[K_('SM')])
                yield
                p.op('dve', lambda e: e.reduce_max(out=mx[:], in_=SM[:], axis=AX.X), reads=[K_('SM')], writes=[K_('mx')])
                p.op('dve', lambda e: e.tensor_scalar(out=nmx[:], in0=mx[:], scalar1=-0.125, scalar2=None, op0=ALU.mult), reads=[K_('mx')], writes=[K_('nmx')])
                yield
                p.op('act', lambda e: e.activation(out=PB[:], in_=SM[:], func=AF.Exp, bias=nmx[:], scale=0.125, accum_out=oa[:, h, 64:65]),
                     reads=[K_('SM'), K_('nmx')], writes=[K_('PB'), oak])
                p.op('dve', lambda e: e.tensor_scalar(out=oa[:, h, 65:66], in0=mx[:], scalar1=0.125, scalar2=None, op0=ALU.mult), reads=[K_('mx'), oak], writes=[oak])
                yield
                bank, bkk = cx.ptbank()
                for a in range(2):
                    p.op('pe', lambda e, a=a, bank=bank: e.transpose(out=bank[:, 0, a * 128:(a + 1) * 128], in_=PB[:, a * 128:(a + 1) * 128], identity=ident[:]),
                         reads=[K_('PB'), 'ident'], writes=[bkk])
                cx.copy('act', PT[:].rearrange("p a b -> p (a b)"), bank[:, 0, 0:256], [bkk], [K_('PT')])
                yield
                ps2, pk2 = cx.psum()
                for a in range(2):
                    p.op('pe', lambda e, a=a, ps2=ps2: e.matmul(ps2[:, 0:64], lhsT=PT[:, a, :], rhs=VB[bi][:, a, h * 64:(h + 1) * 64], start=(a == 0), stop=(a == 1)),
                         reads=[K_('PT'), ('VB', bi)], writes=[pk2])
                p.op('dve', lambda e, ps2=ps2: e.tensor_copy(out=oa[:, h, 0:64], in_=ps2[:, 0:64]), reads=[pk2, oak], writes=[oak])

            nb_i = 0
            for g, d in enumerate((1, 4, 16)):
                nblocks = TPC // (128 * d)
                for n in range(nblocks):
                    if n * d * 128 >= ntile_attn * 128:
                        continue
                    for ph in range(d):
                        bi = nb_i % 2
                        nb_i += 1
                        t0 = 128 * d * n + ph
                        qv = q_d[t0:t0 + 127 * d + 1:d, g * 512:(g + 1) * 512]
                        r0 = HALO + 128 * d * (n - 1) + ph
                        kv_ = kh_d[r0:r0 + 255 * d + 1:d, g * 512:(g + 1) * 512].rearrange("(a p) c -> p a c", a=2)
                        vv_ = vh_d[r0:r0 + 255 * d + 1:d, g * 512:(g + 1) * 512].rearrange("(a p) c -> p a c", a=2)
                        p.dma('sp', QB[bi][:], qv, writes=[('QB', bi)])
                        p.dma('sp', KB[bi][:], kv_, writes=[('KB', bi)])
                        p.dma('sp', VB[bi][:], vv_, writes=[('VB', bi)])
                        bank, bkk = cx.ptbank()
                        bv = bank[:].rearrange("p a b -> p (a b)")
                        for hp in range(4):
                            p.op('pe', lambda e, hp=hp, bv=bv, bi=bi: e.transpose(out=bv[:, hp * 128:(hp + 1) * 128], in_=QB[bi][:, hp * 128:(hp + 1) * 128], identity=ident[:]),
                                 reads=[('QB', bi), 'ident'], writes=[bkk])
                        cx.copy('act', QTs[bi][:].rearrange("p a b -> p (a b)"), bv[:, 0:512], [bkk], [('QT', bi)])
                        bank, bkk = cx.ptbank()
                        bv = bank[:].rearrange("p a b -> p (a b)")
                        for hp in range(4):
                            for a in range(2):
                                p.op('pe', lambda e, hp=hp, a=a, bv=bv, bi=bi: e.transpose(out=bv[:, hp * 256 + a * 128:hp * 256 + (a + 1) * 128],
                                                                                         in_=KB[bi][:, a, hp * 128:(hp + 1) * 128], identity=ident[:]),
                                     reads=[('KB', bi), 'ident'], writes=[bkk])
                        cx.copy('dve', KTs[bi][:].rearrange("p a b -> p (a b)"), bv[:, 0:1024], [bkk], [('KT', bi)])
                        first = (n == 0)
                        mk, mkk = (MASKF, 'MASKF') if first else (MASK, 'MASK')
                        oa = OUTA[bi]
                        alive = [attn_head(h, bi, mk, mkk, oa) for h in range(8)]
                        while alive:
                            nxt_alive = []
                            for g_ in alive:
                                try:
                                    next(g_)
                                    nxt_alive.append(g_)
                                except StopIteration:
                                    pass
                            alive = nxt_alive
                        sv = scr_d[g, t0:t0 + 127 * d + 1:d, :]
                        p.dma('sp', sv, oa[:].rearrange("p a b -> p (a b)"), reads=[('OUTA', bi, h) for h in range(8)],
                              writes=[('scr', g, (t0 + i * d) // 128) for i in range(0, 128, max(1, 128 // d))] if d > 1 else [('scr', g, t0 // 128)])
            p.barrier()

        NT = PASS_T // 128
        HTm2 = [sb("HTm%d" % i, [128, 8, PASS_T], BF16) for i in range(2)]
        ACC = sb("ACC", [128, NT, D], F32)
        GT2 = [sb("GT%d" % i, [128, NT, 8], F32) for i in range(2)]
        RECS = sb("RECS", [128, 3, 8 * REC], F32)
        HB = sb("HBc", [128, 1, D], BF16)
        OB = sb("OB", [128, 1, 512], BF16)
        OT = sb("OT", [128, 4, 128], BF16)
        HIDm = [sb("HIDm%d" % i, [128, 2, PASS_T], BF16) for i in range(2)]
        WDs = [sb("WDs%d" % i, [128, 2, D], BF16) for i in range(2)]
        SG = [sb("SG%d" % i, [128, 512], F32) for i in range(2)]
        mM = sb("mM", [128, 8], F32)
        cg = sb("cg", [128, 3, 8], F32)
        den = sb("den", [128, 8], F32)
        numt = sb("numt", [128, 8, 64], F32)
        tmp3 = sb("tmp3", [128, 8, 64], F32)
        lg = sb("lg", [128, 8], F32)
        m8 = sb("m8", [128, 8], F32)
        nm0 = sb("nm0", [128, 1], F32)
        gden = sb("gden", [128, 1], F32)
        ge = sb("ge", [128, 8], F32)
        w8_i = 0
        wd_i = 0
        def merge_tile(ps_i, jt):
            HTm = HTm2[ps_i % 2]
            GT = GT2[ps_i % 2]
            hb_ = ps_i % 2
            tile = ps_i * NT + jt
            r0 = tile * 128
            for g in range(3):
                p.dma('sp', RECS[:, g, :], scr_d[g, r0:r0 + 128, :], reads=[('scr', g, tile)], writes=[('RECS', g)])
            rk = [('RECS', g) for g in range(3)]
            R = lambda g: RECS[:, g, :].rearrange("p (h r) -> p h r", r=REC)
            p.op('dve', lambda e: e.tensor_tensor(out=mM[:], in0=R(0)[:, :, 65], in1=R(1)[:, :, 65], op=ALU.max), reads=rk, writes=['mM'])
            p.op('dve', lambda e: e.tensor_tensor(out=mM[:], in0=mM[:], in1=R(2)[:, :, 65], op=ALU.max), reads=rk + ['mM'], writes=['mM'])
            for g in range(3):
                p.op('dve', lambda e, g=g: e.tensor_tensor(out=cg[:, g, :], in0=R(g)[:, :, 65], in1=mM[:], op=ALU.subtract), reads=rk + ['mM'], writes=[('cg', g)])
                p.op('act', lambda e, g=g: e.activation(out=cg[:, g, :], in_=cg[:, g, :], func=AF.Exp), reads=[('cg', g)], writes=[('cg', g)])
            p.op('dve', lambda e: e.tensor_tensor(out=den[:], in0=cg[:, 0, :], in1=R(0)[:, :, 64], op=ALU.mult), reads=rk + [('cg', 0)], writes=['den'])
            p.op('dve', lambda e: e.tensor_tensor(out=numt[:], in0=R(0)[:, :, 0:64], in1=cg[:, 0, :].unsqueeze(2).to_broadcast([128, 8, 64]), op=ALU.mult), reads=rk + [('cg', 0)], writes=['numt'])
            for g in (1, 2):
                p.op('dve', lambda e, g=g: e.tensor_tensor(out=lg[:], in0=cg[:, g, :], in1=R(g)[:, :, 64], op=ALU.mult), reads=rk + [('cg', g)], writes=['lg'])
                p.op('dve', lambda e: e.tensor_tensor(out=den[:], in0=den[:], in1=lg[:], op=ALU.add), reads=['den', 'lg'], writes=['den'])
                p.op('dve', lambda e, g=g: e.tensor_tensor(out=tmp3[:], in0=R(g)[:, :, 0:64], in1=cg[:, g, :].unsqueeze(2).to_broadcast([128, 8, 64]), op=ALU.mult), reads=rk + [('cg', g)], writes=['tmp3'])
                p.op('dve', lambda e: e.tensor_tensor(out=numt[:], in0=numt[:], in1=tmp3[:], op=ALU.add), reads=['numt', 'tmp3'], writes=['numt'])
            p.op('dve', lambda e: e.reciprocal(out=den[:], in_=den[:]), reads=['den'], writes=['den'])
            p.op('dve', lambda e: e.tensor_tensor(out=OB[:, 0, :].rearrange("p (h d) -> p h d", d=64), in0=numt[:], in1=den[:].unsqueeze(2).to_broadcast([128, 8, 64]), op=ALU.mult),
                 reads=['numt', 'den'], writes=['OB'])
            bank, bkk = cx.ptbank()
            for c in range(4):
                p.op('pe', lambda e, c=c, bank=bank: e.transpose(out=bank[:, 0, c * 128:(c + 1) * 128], in_=OB[:, 0, c * 128:(c + 1) * 128], identity=ident[:]), reads=['OB', 'ident'], writes=[bkk])
            cx.copy('act', OT[:].rearrange("p a b -> p (a b)"), bank[:, 0, :], [bkk], ['OT'])
            p.dma('sp', XA[:], x2_d[r0:r0 + 128, :], writes=['XA'])
            for half in range(2):
                ps, pk = cx.psum()
                for c in range(4):
                    p.op('pe', lambda e, c=c, ps=ps, half=half: e.matmul(ps[:], lhsT=OT[:, c, :], rhs=WO[:, c, half * 512:(half + 1) * 512], start=(c == 0), stop=(c == 3)), reads=['OT', 'WO'], writes=[pk])
                sl = slice(half * 512, (half + 1) * 512)
                p.op('dve', lambda e, ps=ps, sl=sl: e.tensor_tensor(out=YB[:, sl], in0=ps[:], in1=T["G1"][:, sl], op=ALU.mult), reads=[pk, "G1"], writes=['YB'])
            p.op('dve', lambda e: e.scalar_tensor_tensor(out=XA[:], in0=XA[:], scalar=ALPHA, in1=YB[:], op0=ALU.mult, op1=ALU.add), reads=['XA', 'YB'], writes=['XA'])
            cx.layer_norm_tile(XA[:], 'XA', 'c')
            p.op('dve', lambda e: e.tensor_tensor(out=TMP[:], in0=XA[:], in1=T["G2"][:], op=ALU.mult), reads=['XA', "G2"], writes=['TMP'])
            p.op('dve', lambda e: e.tensor_tensor(out=TMP[:], in0=TMP[:], in1=T["B2"][:], op=ALU.add), reads=['TMP', "B2"], writes=['TMP'])
            p.op('act', lambda e: e.copy(out=HB[:, 0, :], in_=TMP[:]), reads=['TMP'], writes=[('HBc', 0)])
            p.op('dve', lambda e: e.tensor_tensor(out=XA[:], in0=XA[:], in1=T["Gb0"][:], op=ALU.mult), reads=['XA', "Gb0"], writes=['XA'])
            p.op('dve', lambda e: e.tensor_tensor(out=XA[:], in0=XA[:], in1=T["Bb0"][:], op=ALU.add), reads=['XA', "Bb0"], writes=['XA'])
            p.dma('sp', x3_d[r0:r0 + 128, :], XA[:], reads=['XA'], writes=[('x3', tile)])
            for e_ in range(8):
                p.op('dve', lambda e, e_=e_: e.tensor_tensor(out=YB[:], in0=TMP[:], in1=RB[:, e_, :], op=ALU.mult), reads=['TMP', ('RB', e_)], writes=['YB'])
                p.op('act', lambda e, e_=e_: e.activation(out=YB[:], in_=YB[:], func=AF.Identity, accum_out=lg[:, e_:e_ + 1]), reads=['YB'], writes=['YB', 'lg'])
            p.op('dve', lambda e: e.max(out=m8[:], in_=lg[:]), reads=['lg'], writes=['m8'])
            p.op('dve', lambda e: e.tensor_scalar(out=nm0[:], in0=m8[:, 0:1], scalar1=-1.0, scalar2=None, op0=ALU.mult), reads=['m8'], writes=['nm0'])
            p.op('act', lambda e: e.activation(out=ge[:], in_=lg[:], func=AF.Exp, bias=nm0[:], scale=1.0), reads=['lg', 'nm0'], writes=['ge'])
            p.op('dve', lambda e: e.scalar_tensor_tensor(out=ge[:], in0=lg[:], scalar=m8[:, 1:2], in1=ge[:], op0=ALU.is_ge, op1=ALU.mult), reads=['lg', 'm8', 'ge'], writes=['ge'])
            p.op('dve', lambda e: e.reduce_sum(out=gden[:], in_=ge[:], axis=AX.X), reads=['ge'], writes=['gden'])
            p.op('dve', lambda e: e.reciprocal(out=gden[:], in_=gden[:]), reads=['gden'], writes=['gden'])
            p.op('dve', lambda e, jt=jt: e.tensor_scalar(out=GT[:, jt, :], in0=ge[:], scalar1=gden[:], scalar2=None, op0=ALU.mult), reads=['ge', 'gden'], writes=[('GT', hb_, jt)])
            bank, bkk = None, None
            for c2 in range(0, 8, 4):
                bank, bkk = cx.ptbank()
                bv = bank[:].rearrange("p a b -> p (a b)")
                for cc in range(4):
                    p.op('pe', lambda e, c2=c2, cc=cc, bv=bv: e.transpose(out=bv[:, cc * 128:(cc + 1) * 128], in_=HB[:, 0, (c2 + cc) * 128:(c2 + cc + 1) * 128], identity=ident[:]),
                         reads=[('HBc', 0), 'ident'], writes=[bkk])
                cx.copy(cx.evac_eng(), HTm[:, c2:c2 + 4, jt * 128:(jt + 1) * 128], bv[:, 0:512].rearrange("p (a b) -> p a b", b=128), [bkk], [('HTm', hb_, jt)])

        for jt in range(NT):
            merge_tile(0, jt)
        for ps_i in range(npass):
            HTm = HTm2[ps_i % 2]
            GT = GT2[ps_i % 2]
            hb_ = ps_i % 2
            htk = [('HTm', hb_, jt) for jt in range(NT)]
            for jt in range(NT):
                p.op('pool', lambda e, jt=jt: e.memset(ACC[:, jt, :], 0.0), writes=[('ACC', jt)])
            for ex in range(nexp):
                gv = wgu_d[ex].rearrange("(c p) n -> p c n", p=128)
                dv = wdn_d[ex].rearrange("(c p) n -> p c n", p=128)
                for s in range(D_FFE // 256):
                    wb, wk = W8[w8_i % 2], ('W8', w8_i % 2)
                    w8_i += 1
                    wdb, wdk = WDs[wd_i % 2], ('WDs', wd_i % 2)
                    hb, hk = HIDm[wd_i % 2], ('HIDm', wd_i % 2)
                    wd_i += 1
                    si = ex * (D_FFE // 256) + s
                    if ps_i == 0:
                        p.dma('pool', wb[:, :, 0:256], gv[:, :, s * 256:(s + 1) * 256], writes=[wk])
                        p.dma('pool', wb[:, :, 256:512], gv[:, :, D_FFE + s * 256:D_FFE + (s + 1) * 256], writes=[wk])
                        p.dma('pool', wdb[:, :, 0:512], dv[:, 2 * s:2 * s + 2, 0:512], writes=[wdk])
                        p.dma('pool', wdb[:, :, 512:1024], dv[:, 2 * s:2 * s + 2, 512:1024], writes=[wdk])
                        if npass > 1:
                            p.dma('sp', sw8_d[si], wb[:].rearrange("p a b -> p (a b)"), reads=[wk], writes=[('sw8', si)])
                            p.dma('sp', swd_d[si], wdb[:].rearrange("p a b -> p (a b)"), reads=[wdk], writes=[('swd', si)])
                    else:
                        p.dma('sp', wb[:].rearrange("p a b -> p (a b)"), sw8_d[si], reads=[('sw8', si)], writes=[wk])
                        p.dma('sp', wdb[:].rearrange("p a b -> p (a b)"), swd_d[si], reads=[('swd', si)], writes=[wdk])
                    for fc in range(2):
                        for tg in range(PASS_T // 512):
                            tsl = slice(tg * 512, (tg + 1) * 512)
                            psg, pkg = cx.psum()
                            for c in range(8):
                                p.op('pe', lambda e, c=c, fc=fc, psg=psg, wb=wb, tsl=tsl, hb_=hb_: e.matmul(psg[:], lhsT=wb[:, c, fc * 128:(fc + 1) * 128], rhs=HTm2[hb_][:, c, tsl], start=(c == 0), stop=(c == 7)),
                                     reads=htk + [wk], writes=[pkg])
                            psu, pku = cx.psum()
                            for c in range(8):
                                p.op('pe', lambda e, c=c, fc=fc, psu=psu, wb=wb, tsl=tsl, hb_=hb_: e.matmul(psu[:], lhsT=wb[:, c, 256 + fc * 128:256 + (fc + 1) * 128], rhs=HTm2[hb_][:, c, tsl], start=(c == 0), stop=(c == 7)),
                                     reads=htk + [wk], writes=[pku])
                            sgi = (fc * 2 + tg) % 2
                            sg, sgk = SG[sgi], ('SG', sgi)
                            p.op('act', lambda e, psg=psg, sg=sg: e.activation(out=sg[:], in_=psg[:], func=AF.Silu), reads=[pkg], writes=[sgk])
                            p.op('dve', lambda e, psu=psu, sg=sg, hb=hb, fc=fc, tsl=tsl: e.tensor_tensor(out=hb[:, fc, tsl], in0=psu[:], in1=sg[:], op=ALU.mult), reads=[pku, sgk], writes=[hk])
                    for jt in range(NT):
                        for half in range(2):
                            ps, pk = cx.psum()
                            for fc in range(2):
                                p.op('pe', lambda e, fc=fc, ps=ps, hb=hb, wdb=wdb, jt=jt, half=half: e.matmul(ps[:], lhsT=hb[:, fc, jt * 128:(jt + 1) * 128], rhs=wdb[:, fc, half * 512:(half + 1) * 512],
                                                                                                         start=(fc == 0), stop=(fc == 1)), reads=[hk, wdk], writes=[pk])
                            sl = slice(half * 512, (half + 1) * 512)
                            p.op('dve', lambda e, ps=ps, jt=jt, sl=sl, ex=ex, hb_=hb_: e.scalar_tensor_tensor(out=ACC[:, jt, sl], in0=ps[:], scalar=GT2[hb_][:, jt, ex:ex + 1], in1=ACC[:, jt, sl], op0=ALU.mult, op1=ALU.add),
                                 reads=[pk, ('GT', hb_, jt), ('ACC', jt)], writes=[('ACC', jt)])
                if ps_i + 1 < npass and ex < NT:
                    merge_tile(ps_i + 1, ex)
            if ps_i + 1 < npass:
                for jt in range(min(nexp, NT), NT):
                    merge_tile(ps_i + 1, jt)
            for jt in range(NT):
                tile = ps_i * NT + jt
                r0 = tile * 128
                p.dma('sp', XA[:], x3_d[r0:r0 + 128, :], reads=[('x3', tile)], writes=['XA'])
                p.op('dve', lambda e, jt=jt: e.tensor_tensor(out=ACC[:, jt, :], in0=ACC[:, jt, :], in1=T["G3g"][:], op=ALU.mult), reads=[('ACC', jt), "G3g"], writes=[('ACC', jt)])
                p.op('dve', lambda e, jt=jt: e.scalar_tensor_tensor(out=XA[:], in0=XA[:], scalar=ALPHA, in1=ACC[:, jt, :], op0=ALU.mult, op1=ALU.add), reads=['XA', ('ACC', jt)], writes=['XA'])
                cx.layer_norm_tile(XA[:], 'XA', 'd')
                p.op('dve', lambda e: e.tensor_tensor(out=XA[:], in0=XA[:], in1=T["Gb1"][:], op=ALU.mult), reads=['XA', "Gb1"], writes=['XA'])
                p.op('dve', lambda e: e.tensor_tensor(out=XA[:], in0=XA[:], in1=T["Bb1"][:], op=ALU.add), reads=['XA', "Bb1"], writes=['XA'])
                p.dma('sp', out_d[r0:r0 + 128, :], XA[:], reads=['XA'])
        p.finish()
        p.emit()
    return nc


def maps_C(inp, x2, q, k, v):
    idxs = [8, 9, 10, 11]
    aw, ab = _ada_blocks(inp['ada_w'], inp['ada_b'], idxs)
    maps = []
    for c in range(NCORE):
        b, s0 = c // 4, (c % 4) * TPC
        if s0 == 0:
            kh = np.concatenate([np.zeros((HALO, 1536), k.dtype), k[b, 0:TPC]], axis=0)
            vh = np.concatenate([np.zeros((HALO, 1536), v.dtype), v[b, 0:TPC]], axis=0)
        else:
            kh = np.ascontiguousarray(k[b, s0 - HALO:s0 + TPC])
            vh = np.ascontiguousarray(v[b, s0 - HALO:s0 + TPC])
        maps.append({
            "x2": np.ascontiguousarray(x2[b, s0:s0 + TPC]), "q": np.ascontiguousarray(q[b, s0:s0 + TPC]),
            "kh": kh, "vh": vh,
            "halo_bias": np.full((128, 1), NEG if s0 == 0 else 0.0, np.float32),
            "cT": _cT(inp['c'][b]), "adaw": aw, "adab": ab,
            "lng": np.ascontiguousarray(inp['ln_g'][1]), "lnb": np.ascontiguousarray(inp['ln_b'][1]),
            "w_o": inp['attn_w_o'][0], "router": np.ascontiguousarray(inp['moe_router'][0].T),
            "moe_gu": inp['moe_w_gu'][0], "moe_dn": inp['moe_w_down'][0],
        })
    return maps


def _run(nc, maps):
    return run_bass_kernel_spmd(nc, maps, core_ids=list(range(NCORE))).results


def kernel(**inputs):
    inp = {k: np.asarray(v) for k, v in inputs.items()}
    resA = _run(build_A(), maps_A(inp))
    yg = np.empty((2, SEQ, D), np.float32)
    for c in range(NCORE):
        b, hg = c // 4, c % 4
        yg[b, :, 256 * hg:256 * hg + 256] = np.asarray(resA[c]["yg"])
    del resA
    resB = _run(build_B(), maps_B(inp, yg))
    x2 = np.empty((2, SEQ, D), np.float32)
    q = np.empty((2, SEQ, 1536), ml_dtypes.bfloat16)
    k = np.empty((2, SEQ, 1536), ml_dtypes.bfloat16)
    v = np.empty((2, SEQ, 1536), ml_dtypes.bfloat16)
    for c in range(NCORE):
        b, s0 = c // 4, (c % 4) * TPC
        x2[b, s0:s0 + TPC] = np.asarray(resB[c]["x2"])
        q[b, s0:s0 + TPC] = np.asarray(resB[c]["q"])
        k[b, s0:s0 + TPC] = np.asarray(resB[c]["k"])
        v[b, s0:s0 + TPC] = np.asarray(resB[c]["v"])
    del resB
    resC = _run(build_C(), maps_C(inp, x2, q, k, v))
    out = np.empty((2, SEQ, D), np.float32)
    for c in range(NCORE):
        b, s0 = c // 4, (c % 4) * TPC
        out[b, s0:s0 + TPC] = np.asarray(resC[c]["out"])
    return out
```

```python
import math
import numpy as np
from contextlib import ExitStack
import concourse.bass as bass
import concourse.mybir as mybir
from concourse.bass_utils import run_bass_kernel_spmd
import ml_dtypes

F32 = mybir.dt.float32
BF16 = mybir.dt.bfloat16
I32 = mybir.dt.int32
AF = mybir.ActivationFunctionType
ALU = mybir.AluOpType
AX = mybir.AxisListType

D = 1024
SEQ = 16384
NCORE = 8
TPC = 4096
ALPHA = 4.0 ** 0.25
LN_EPS = 1e-5
D_FF = 2816
NEXP = 8
D_FFE = 3584
TWO_PI = 2.0 * math.pi


SAME_ENGINE_FIFO = False
EMBED_WAIT = True


class Prog:
    CE = ('pe', 'act', 'dve', 'pool')

    def __init__(self, nc, stack, ndma=12):
        self.nc = nc
        self.q = {e: [] for e in ('pe', 'act', 'dve', 'pool', 'sp')}
        self.esem = {e: stack.enter_context(nc.semaphore('s_' + e)) for e in self.CE}
        self.ecnt = {e: 0 for e in self.CE}
        self.dpool = {e: [stack.enter_context(nc.semaphore('d_%s%d' % (e, i))) for i in range(ndma)]
                      for e in ('sp', 'pool', 'act')}
        self.dnext = {e: 0 for e in self.dpool}
        self.dval = {}
        self.lastw = {}
        self.readers = {}
        self.seen = {e: {} for e in self.q}
        self.semobj = {}

    def _deps(self, eng, reads, writes):
        evs = []
        for k in reads:
            ev = self.lastw.get(k)
            if ev is not None:
                evs.append(ev)
        for k in writes:
            ev = self.lastw.get(k)
            if ev is not None:
                evs.append(ev)
            evs.extend(self.readers.get(k, ()))
        need = {}
        for (sem, val, src) in evs:
            if src == eng and (eng == 'pe' or (SAME_ENGINE_FIFO and eng in ('act', 'dve'))):
                continue
            if self.seen[eng].get(sem.num, 0) >= val:
                continue
            if need.get(sem.num, 0) < val:
                need[sem.num] = val
                self.semobj[sem.num] = sem
        for num, val in need.items():
            self.q[eng].append(('wait', self.semobj[num], val))
            self.seen[eng][num] = val

    def _commit(self, ev, reads, writes):
        ws = set(writes)
        for k in writes:
            self.lastw[k] = ev
            self.readers[k] = []
        for k in reads:
            if k in ws:
                continue
            lst = self.readers.setdefault(k, [])
            lst[:] = [e for e in lst if e[0].num != ev[0].num]
            lst.append(ev)

    def op(self, eng, fn, reads=(), writes=()):
        self._deps(eng, reads, writes)
        self.ecnt[eng] += 1
        ev = (self.esem[eng], self.ecnt[eng], eng)
        self.q[eng].append(('op', fn, ev))
        self._commit(ev, reads, writes)

    def dma(self, eng, out, in_, reads=(), writes=(), **kw):
        self._deps(eng, reads, writes)
        pool = self.dpool[eng]
        sem = pool[self.dnext[eng] % len(pool)]
        self.dnext[eng] += 1
        prev = self.dval.get(sem.num, 0)
        if prev > 0 and self.seen[eng].get(sem.num, 0) < prev:
            self.q[eng].append(('wait', sem, prev))
            self.seen[eng][sem.num] = prev
        self.dval[sem.num] = prev + 16
        ev = (sem, prev + 16, 'dma')
        self.q[eng].append(('dma', out, in_, ev, kw))
        self._commit(ev, reads, writes)

    def barrier(self):
        for eng in self.q:
            for src in self.CE:
                if self.ecnt[src] > 0 and self.seen[eng].get(self.esem[src].num, 0) < self.ecnt[src]:
                    self.q[eng].append(('wait', self.esem[src], self.ecnt[src]))
                    self.seen[eng][self.esem[src].num] = self.ecnt[src]
            for e2 in self.dpool:
                for sem in self.dpool[e2]:
                    v = self.dval.get(sem.num, 0)
                    if v > 0 and self.seen[eng].get(sem.num, 0) < v:
                        self.q[eng].append(('wait', sem, v))
                        self.seen[eng][sem.num] = v

    def finish(self):
        for e in self.dpool:
            for sem in self.dpool[e]:
                v = self.dval.get(sem.num, 0)
                if v > 0:
                    self.q['sp'].append(('wait', sem, v))
        for e in self.CE:
            if self.ecnt[e] > 0:
                self.q['sp'].append(('wait', self.esem[e], self.ecnt[e]))

    def emit(self):
        nc = self.nc
        with nc.Block() as block:
            def run(engname):
                def body(e):
                    pend = []
                    for it in self.q[engname]:
                        if it[0] == 'wait':
                            pend.append(it)
                            continue
                        emb = None
                        if EMBED_WAIT and pend:
                            emb = pend.pop()
                        for w in pend:
                            e.wait_ge(w[1], w[2])
                        pend = []
                        if it[0] == 'op':
                            ins = it[1](e)
                            if emb is not None:
                                ins = ins._wait_ge(emb[1], emb[2])
                            ins.then_inc(it[2][0], 1)
                        else:
                            ins = e.dma_start(out=it[1], in_=it[2], **it[4])
                            if emb is not None:
                                ins = ins._wait_ge(emb[1], emb[2])
                            ins.then_inc(it[3][0], 16)
                    for w in pend:
                        e.wait_ge(w[1], w[2])
                return body
            block.sync(run('sp'))
            block.tensor(run('pe'))
            block.scalar(run('act'))
            block.vector(run('dve'))
            block.gpsimd(run('pool'))


class Ctx:
    def __init__(self, nc, st):
        self.nc = nc
        self.st = st
        self.p = Prog(nc, st)
        self.ps = [st.enter_context(nc.psum_tensor("ps%d" % i, [128, 512], F32)) for i in range(5)]
        self.pt = [st.enter_context(nc.psum_tensor("pt%d" % i, [128, 2, 512], BF16)) for i in range(3)]
        self.psn = 0
        self.ptn = 0
        self.evn = 0
        self.ident = self.sb("ident", [128, 128], BF16)
        p = self.p
        p.op('pool', lambda e: e.memset(self.ident[:], 0.0), writes=['ident'])
        p.op('pool', lambda e: e.affine_select(out=self.ident[:], in_=self.ident[:], pattern=[[-1, 128]],
                                               compare_op=ALU.not_equal, fill=1.0, base=0, channel_multiplier=1),
             reads=['ident'], writes=['ident'])

    def sb(self, name, shape, dt):
        return self.st.enter_context(self.nc.sbuf_tensor(name, shape, dt))

    def psum(self):
        i = self.psn % len(self.ps)
        self.psn += 1
        return self.ps[i], ('ps', i)

    def ptbank(self):
        i = self.ptn % len(self.pt)
        self.ptn += 1
        return self.pt[i], ('pt', i)

    def evac_eng(self):
        self.evn += 1
        return 'dve' if self.evn % 2 else 'act'

    def copy(self, eng, out, in_, reads, writes):
        if eng == 'act':
            self.p.op('act', lambda e: e.copy(out=out, in_=in_), reads=reads, writes=writes)
        else:
            self.p.op(eng, lambda e: e.tensor_copy(out=out, in_=in_), reads=reads, writes=writes)

    def to_featmajor(self, src, srckeys, dst, dstkey, ntile, nchunk):
        p = self.p
        for c2 in range(0, nchunk, 2):
            bank, bk = self.ptbank()
            ncc = min(2, nchunk - c2)
            for cc in range(ncc):
                c = c2 + cc
                for j in range(ntile):
                    p.op('pe', lambda e, c=c, cc=cc, j=j, bank=bank: e.transpose(out=bank[:, cc, j * 128:(j + 1) * 128],
                                                                                 in_=src[:, j, c * 128:(c + 1) * 128],
                                                                                 identity=self.ident[:]),
                         reads=[srckeys[j], 'ident'], writes=[bk])
            self.copy(self.evac_eng(), dst[:, c2:c2 + ncc, 0:ntile * 128], bank[:, 0:ncc, 0:ntile * 128], [bk],
                      [(dstkey, c2 + cc) for cc in range(ncc)])

    def layer_norm_tile(self, z, zkey, tag):
        p = self.p
        if not hasattr(self, 'ln_tmp'):
            self.ln_stats = self.sb("ln_stats", [128, 2, 6], F32)
            self.ln_mv = self.sb("ln_mv", [128, 2], F32)
            self.ln_rs = self.sb("ln_rs", [128, 1], F32)
            self.ln_nm = self.sb("ln_nm", [128, 1], F32)
            self.ln_tmp = True
        st_, mv, rs, nm = self.ln_stats, self.ln_mv, self.ln_rs, self.ln_nm
        for h in range(2):
            p.op('dve', lambda e, h=h: e.bn_stats(out=st_[:, h, :], in_=z[:, h * 512:(h + 1) * 512]),
                 reads=[zkey], writes=[('lnst', h)])
        p.op('dve', lambda e: e.bn_aggr(out=mv[:], in_=st_[:].rearrange("p a b -> p (a b)")),
             reads=[('lnst', 0), ('lnst', 1)], writes=['lnmv'])
        p.op('act', lambda e: e.activation(out=rs[:], in_=mv[:, 1:2], func=AF.Sqrt, bias=LN_EPS, scale=1.0),
             reads=['lnmv'], writes=['lnrs'])
        p.op('dve', lambda e: e.reciprocal(out=rs[:], in_=rs[:]), reads=['lnrs'], writes=['lnrs'])
        p.op('dve', lambda e: e.tensor_scalar(out=nm[:], in0=mv[:, 0:1], scalar1=rs[:], scalar2=-1.0,
                                              op0=ALU.mult, op1=ALU.mult),
             reads=['lnmv', 'lnrs'], writes=['lnnm'])
        p.op('act', lambda e: e.activation(out=z, in_=z, func=AF.Identity, bias=nm[:], scale=rs[:]),
             reads=[zkey, 'lnrs', 'lnnm'], writes=[zkey])

    def bcast_row(self, dst, dstkey, row_ap, eng='sp'):
        self.p.dma(eng, dst, row_ap.partition_broadcast(128), writes=[dstkey])


def _silu_c_bcast(cx, cT_ap):
    p = cx.p
    ct = cx.sb("ct", [128, 8], F32)
    zer = cx.sb("zer", [128, 128], F32)
    scB = cx.sb("scB", [128, 8, 128], BF16)
    p.dma('sp', ct[:], cT_ap, writes=['ct'])
    p.op('act', lambda e: e.activation(out=ct[:], in_=ct[:], func=AF.Silu), reads=['ct'], writes=['ct'])
    p.op('pool', lambda e: e.memset(zer[:], 0.0), writes=['zer'])
    for c in range(8):
        p.op('act', lambda e, c=c: e.activation(out=scB[:, c, :], in_=zer[:], func=AF.Identity,
                                                bias=ct[:, c:c + 1], scale=1.0),
             reads=['ct', 'zer'], writes=['scB'])
    return scB


def _mod_tile(cx, scB, dst, dstkey, adaw_blk, adab_row, wbuf, wkey, tmp, tmpkey, plus_one):
    p = cx.p
    cx.bcast_row(tmp, tmpkey, adab_row)
    wv = adaw_blk.rearrange("(c p) n -> p c n", p=128)
    for half in range(2):
        p.dma('pool', wbuf[:, :, :], wv[:, :, half * 512:(half + 1) * 512], reads=[], writes=[wkey])
        ps, pk = cx.psum()
        for c in range(8):
            p.op('pe', lambda e, c=c, ps=ps: e.matmul(ps[:], lhsT=scB[:, c, :], rhs=wbuf[:, c, :],
                                                      start=(c == 0), stop=(c == 7)),
                 reads=['scB', wkey], writes=[pk])
        sl = slice(half * 512, (half + 1) * 512)
        p.op('dve', lambda e, ps=ps, sl=sl: e.scalar_tensor_tensor(out=dst[:, sl], in0=ps[:], scalar=(1.0 if plus_one else 0.0),
                                                                    in1=tmp[:, sl], op0=ALU.add, op1=ALU.add),
             reads=[pk, tmpkey], writes=[dstkey])


def _rope_tables(cx, posb_ap, ntile, ang, angkey, kk, kkkey):
    p = cx.p
    nc = cx.nc
    posi = cx.sb("posi", [128, ntile], I32)
    pos = cx.sb("pos", [128, ntile], F32)
    pb = cx.sb("pb", [128, 1], F32)
    cos = cx.sb("cos", [128, ntile, 32], F32)
    sin = cx.sb("sin", [128, ntile, 32], F32)
    p.dma('sp', pb[:], posb_ap, writes=['pb'])
    p.op('pool', lambda e: e.iota(posi[:], pattern=[[128, ntile]], base=0, channel_multiplier=1), writes=['posi'])
    p.op('dve', lambda e: e.tensor_copy(out=pos[:], in_=posi[:]), reads=['posi'], writes=['pos'])
    p.op('dve', lambda e: e.tensor_scalar(out=pos[:], in0=pos[:], scalar1=pb[:], scalar2=None, op0=ALU.add),
         reads=['pos', 'pb'], writes=['pos'])
    for i in range(32):
        invf = float(np.float32(10000.0) ** np.float32(-i / 32.0))
        p.op('dve', lambda e, i=i, invf=invf: e.tensor_scalar(out=ang[:, :, i], in0=pos[:], scalar1=invf, scalar2=None,
                                                              op0=ALU.mult),
             reads=['pos'], writes=[angkey])
    MAGIC = 12582912.0
    C1 = 6.28125
    C2 = TWO_PI - C1
    p.op('dve', lambda e: e.tensor_scalar(out=kk, in0=ang, scalar1=1.0 / TWO_PI, scalar2=MAGIC,
                                          op0=ALU.mult, op1=ALU.add), reads=[angkey], writes=[kkkey])
    p.op('dve', lambda e: e.tensor_scalar(out=kk, in0=kk, scalar1=-MAGIC, scalar2=None, op0=ALU.add),
         reads=[kkkey], writes=[kkkey])
    p.op('dve', lambda e: e.scalar_tensor_tensor(out=ang, in0=kk, scalar=-C1, in1=ang, op0=ALU.mult, op1=ALU.add),
         reads=[kkkey, angkey], writes=[angkey])
    p.op('dve', lambda e: e.scalar_tensor_tensor(out=ang, in0=kk, scalar=-C2, in1=ang, op0=ALU.mult, op1=ALU.add),
         reads=[kkkey, angkey], writes=[angkey])
    p.op('dve', lambda e: e.tensor_scalar(out=ang, in0=ang, scalar1=math.pi, scalar2=-math.pi, op0=ALU.min, op1=ALU.max),
         reads=[angkey], writes=[angkey])
    p.op('act', lambda e: e.activation(out=sin[:], in_=ang, func=AF.Sin), reads=[angkey], writes=['sin'])
    p.op('act', lambda e: e.activation(out=kk, in_=ang, func=AF.Abs), reads=[angkey], writes=[kkkey])
    p.op('act', lambda e: e.activation(out=cos[:], in_=kk, func=AF.Sin, bias=cx.halfpi[:], scale=-1.0),
         reads=[kkkey, 'halfpi'], writes=['cos'])
    return cos, sin


def _rope_apply(cx, ps, pk, out, outkey, cos_t, sin_t, nh, tmp):
    p = cx.p
    pv = ps.rearrange("p (h t d) -> p h t d", h=nh, t=2)
    ov = out.rearrange("p (h t d) -> p h t d", h=nh, t=2)
    cb = cos_t.unsqueeze(1).to_broadcast([128, nh, 32])
    sbb = sin_t.unsqueeze(1).to_broadcast([128, nh, 32])
    ta = tmp[:, 0, :].rearrange("p (h d) -> p h d", h=nh)
    tb = tmp[:, 1, :].rearrange("p (h d) -> p h d", h=nh)
    tk = ('ropetmp',)
    p.op('dve', lambda e: e.tensor_tensor(out=ta, in0=pv[:, :, 0, :], in1=cb, op=ALU.mult), reads=[pk, 'cos'], writes=[tk])
    p.op('dve', lambda e: e.tensor_tensor(out=tb, in0=pv[:, :, 1, :], in1=sbb, op=ALU.mult), reads=[pk, 'sin'], writes=[tk])
    p.op('dve', lambda e: e.tensor_tensor(out=ov[:, :, 0, :], in0=ta, in1=tb, op=ALU.subtract), reads=[tk], writes=[outkey])
    p.op('dve', lambda e: e.tensor_tensor(out=ta, in0=pv[:, :, 1, :], in1=cb, op=ALU.mult), reads=[pk, 'cos'], writes=[tk])
    p.op('dve', lambda e: e.tensor_tensor(out=tb, in0=pv[:, :, 0, :], in1=sbb, op=ALU.mult), reads=[pk, 'sin'], writes=[tk])
    p.op('dve', lambda e: e.tensor_tensor(out=ov[:, :, 1, :], in0=ta, in1=tb, op=ALU.add), reads=[tk], writes=[outkey])


def build_B(nblk=TPC // 512):
    nc = bass.Bass("TRN2", target_bir_lowering=False)
    dt = lambda name, shape, dtype, kind: nc.dram_tensor(name, shape, dtype, kind=kind).ap()
    x_d = dt("x", [TPC, D], F32, "ExternalInput")
    yg_d = dt("yg", [TPC, D], F32, "ExternalInput")
    cT_d = dt("cT", [128, 8], F32, "ExternalInput")
    adaw_d = dt("adaw", [8, D, D], F32, "ExternalInput")
    adab_d = dt("adab", [8, D], F32, "ExternalInput")
    lng_d = dt("lng", [2, D], F32, "ExternalInput")
    lnb_d = dt("lnb", [2, D], F32, "ExternalInput")
    wout_d = dt("w_out", [D, D], F32, "ExternalInput")
    wgu_d = dt("w_gu", [D, 2 * D_FF], F32, "ExternalInput")
    wdn_d = dt("w_down", [D_FF, D], F32, "ExternalInput")
    wkv_d = dt("w_kv", [D, 3072], F32, "ExternalInput")
    wq_d = dt("w_q", [D, 1536], F32, "ExternalInput")
    posb_d = dt("posb", [128, 1], F32, "ExternalInput")
    x2_d = dt("x2", [TPC, D], F32, "ExternalOutput")
    k_d = dt("k", [TPC, 1536], BF16, "ExternalOutput")
    v_d = dt("v", [TPC, 1536], BF16, "ExternalOutput")
    q_d = dt("q", [TPC, 1536], BF16, "ExternalOutput")
    bw8_d = dt("scr_bw8", [22, 128, 8 * 512], BF16, "Internal")
    bwd_d = dt("scr_bwd", [4, 128, 22 * 256], BF16, "Internal")

    with ExitStack() as st:
        cx = Ctx(nc, st)
        p = cx.p
        sb = cx.sb
        XA = sb("XA", [128, 4, D], F32)
        YB = sb("YB", [128, 4, D], F32)
        TB = sb("TB", [128, 4, D], BF16)
        HT = [sb("HT%d" % i, [128, 8, 512], BF16) for i in range(2)]
        HID = sb("HID", [128, 22, 512], BF16)
        SG = [sb("SG%d" % i, [128, 512], F32) for i in range(2)]
        W8 = [sb("W8_%d" % i, [128, 8, 512], BF16) for i in range(2)]
        WD = [sb("WD_%d" % i, [128, 22, 256], BF16) for i in range(2)]
        OUTB = sb("OUTB", [128, 4, 1536], BF16)
        TMP = sb("TMP", [128, D], F32)
        RT = sb("RT", [128, 2, 256], F32)
        cx.halfpi = sb("halfpi", [128, 1], F32)
        p.op('pool', lambda e: e.memset(cx.halfpi[:], math.pi / 2), writes=['halfpi'])
        names = ["G1", "Gb0", "Bb0", "G2", "B2", "G3g", "Gb1", "Bb1", "G4", "B4", "G5", "B5"]
        T = {n: sb(n, [128, D], F32) for n in names}

        scB = _silu_c_bcast(cx, cT_d)
        tm = {"sh1": XA[:, 0, :], "sc1": XA[:, 1, :], "shkv": XA[:, 2, :], "sckv": XA[:, 3, :],
              "shq": YB[:, 0, :], "scq": YB[:, 1, :], "bias": YB[:, 2, :]}
        tk = {'sh1': ('XA', 0), 'sc1': ('XA', 1), 'shkv': ('XA', 2), 'sckv': ('XA', 3), 'shq': ('YB', 0), 'scq': ('YB', 1), 'bias': ('YB', 2)}

        def mod(dst, dkey, blk, plus_one):
            _mod_tile(cx, scB, dst, dkey, adaw_d[blk], adab_d[blk:blk + 1, :], W8[0], ('W8', 0), tm["bias"], tk["bias"], plus_one)

        mod(T["G1"][:], "G1", 0, True)
        mod(tm["sh1"], tk["sh1"], 1, False)
        mod(tm["sc1"], tk["sc1"], 2, True)
        mod(T["G3g"][:], "G3g", 3, True)
        mod(tm["shkv"], tk["shkv"], 4, False)
        mod(tm["sckv"], tk["sckv"], 5, True)
        mod(tm["shq"], tk["shq"], 6, False)
        mod(tm["scq"], tk["scq"], 7, True)
        cx.bcast_row(T["Gb0"][:], "Gb0", lng_d[0:1, :])
        cx.bcast_row(T["Bb0"][:], "Bb0", lnb_d[0:1, :])
        cx.bcast_row(T["Gb1"][:], "Gb1", lng_d[1:2, :])
        cx.bcast_row(T["Bb1"][:], "Bb1", lnb_d[1:2, :])

        def fold(G, B, Gb, Bb, sc, sh):
            p.op('dve', lambda e: e.tensor_tensor(out=T[G][:], in0=T[Gb][:], in1=tm[sc], op=ALU.mult), reads=[Gb, tk[sc]], writes=[G])
            p.op('dve', lambda e: e.tensor_tensor(out=T[B][:], in0=T[Bb][:], in1=tm[sc], op=ALU.mult), reads=[Bb, tk[sc]], writes=[B])
            p.op('dve', lambda e: e.tensor_tensor(out=T[B][:], in0=T[B][:], in1=tm[sh], op=ALU.add), reads=[B, tk[sh]], writes=[B])

        fold("G2", "B2", "Gb0", "Bb0", "sc1", "sh1")
        fold("G4", "B4", "Gb1", "Bb1", "sckv", "shkv")
        fold("G5", "B5", "Gb1", "Bb1", "scq", "shq")
        cos, sin = _rope_tables(cx, posb_d, 32, TMP[:].rearrange("p (a b) -> p a b", b=32), 'TMP',
                                YB[:, 3, :].rearrange("p (a b) -> p a b", b=32), ('YB', 3))

        w8_list = []
        wd_list = []

        def w8_src(blk_i):
            res = []
            res.append((wout_d, [(0, 512, 0)]))
            res.append((wout_d, [(512, 512, 0)]))
            for s in range(11):
                res.append((wgu_d, [(256 * s, 256, 0), (D_FF + 256 * s, 256, 256)]))
            for s in range(6):
                res.append((wkv_d, [(512 * s, 512, 0)]))
            for s in range(3):
                res.append((wq_d, [(512 * s, 512, 0)]))
            return res

        NBLK = nblk
        for b in range(NBLK):
            w8_list.extend(w8_src(b))
            for s in range(4):
                wd_list.append(s)
        w8_state = {'issued': 0}
        wd_state = {'issued': 0}

        def w8_issue(upto):
            while w8_state['issued'] <= upto and w8_state['issued'] < len(w8_list):
                i = w8_state['issued']
                src, parts = w8_list[i]
                buf = W8[i % 2]
                sv = src.rearrange("(c p) n -> p c n", p=128)
                if i < 22:
                    for (lo, n, dlo) in parts:
                        p.dma('pool', buf[:, :, dlo:dlo + n], sv[:, :, lo:lo + n], writes=[('W8', i % 2)])
                    if NBLK > 1:
                        p.dma('sp', bw8_d[i], buf[:].rearrange("p a b -> p (a b)"), reads=[('W8', i % 2)], writes=[('bw8', i)])
                else:
                    p.dma('sp', buf[:].rearrange("p a b -> p (a b)"), bw8_d[i % 22], reads=[('bw8', i % 22)], writes=[('W8', i % 2)])
                w8_state['issued'] += 1

        def wd_issue(upto):
            while wd_state['issued'] <= upto and wd_state['issued'] < len(wd_list):
                i = wd_state['issued']
                s = wd_list[i]
                buf = WD[i % 2]
                sv = wdn_d.rearrange("(c p) n -> p c n", p=128)
                if i < 4:
                    p.dma('pool', buf[:, :, :], sv[:, :, s * 256:(s + 1) * 256], writes=[('WD', i % 2)])
                    if NBLK > 1:
                        p.dma('sp', bwd_d[i], buf[:].rearrange("p a b -> p (a b)"), reads=[('WD', i % 2)], writes=[('bwd', i)])
                else:
                    p.dma('sp', buf[:].rearrange("p a b -> p (a b)"), bwd_d[i % 4], reads=[('bwd', i % 4)], writes=[('WD', i % 2)])
                wd_state['issued'] += 1

        w8_i = 0
        wd_i = 0

        def post_norm(blk_tiles_z, Gb, Bb, hs):
            pass

        for blk in range(NBLK):
            r0 = blk * 512
            xv = x_d[r0:r0 + 512, :].rearrange("(j p) d -> p j d", p=128)
            ygv = yg_d[r0:r0 + 512, :].rearrange("(j p) d -> p j d", p=128)
            for j in range(4):
                p.dma('sp', XA[:, j, :], xv[:, j, :], writes=[('XA', j)])
                p.dma('sp', YB[:, j, :], ygv[:, j, :], writes=[('YB', j)])
            for j in range(4):
                cx.copy('act' if j % 2 else 'dve', TB[:, j, :], YB[:, j, :], [('YB', j)], [('TB', j)])
            cx.to_featmajor(TB, [('TB', j) for j in range(4)], HT[0], 'HT0', 4, 8)
            for n in range(2):
                w8_issue(w8_i + 1)
                wb, wk = W8[w8_i % 2], ('W8', w8_i % 2)
                for j in range(4):
                    ps, pk = cx.psum()
                    for c in range(8):
                        p.op('pe', lambda e, c=c, j=j, ps=ps, wb=wb: e.matmul(ps[:], lhsT=HT[0][:, c, j * 128:(j + 1) * 128], rhs=wb[:, c, :],
                                                                             start=(c == 0), stop=(c == 7)),
                             reads=[('HT0', c), wk], writes=[pk])
                    sl = slice(n * 512, (n + 1) * 512)
                    p.op('dve', lambda e, j=j, ps=ps, sl=sl: e.tensor_tensor(out=YB[:, j, sl], in0=ps[:], in1=T["G1"][:, sl], op=ALU.mult),
                         reads=[pk, "G1"], writes=[('YB', j)])
                w8_i += 1
            for j in range(4):
                p.op('dve', lambda e, j=j: e.scalar_tensor_tensor(out=XA[:, j, :], in0=XA[:, j, :], scalar=ALPHA, in1=YB[:, j, :],
                                                                  op0=ALU.mult, op1=ALU.add),
                     reads=[('XA', j), ('YB', j)], writes=[('XA', j)])
                cx.layer_norm_tile(XA[:, j, :], ('XA', j), 'a')
                p.op('dve', lambda e, j=j: e.tensor_tensor(out=TMP[:], in0=XA[:, j, :], in1=T["G2"][:], op=ALU.mult),
                     reads=[('XA', j), "G2"], writes=['TMP'])
                p.op('dve', lambda e, j=j: e.tensor_tensor(out=TB[:, j, :], in0=TMP[:], in1=T["B2"][:], op=ALU.add),
                     reads=['TMP', "B2"], writes=[('TB', j)])
                p.op('dve', lambda e, j=j: e.tensor_tensor(out=XA[:, j, :], in0=XA[:, j, :], in1=T["Gb0"][:], op=ALU.mult),
                     reads=[('XA', j), "Gb0"], writes=[('XA', j)])
                p.op('dve', lambda e, j=j: e.tensor_tensor(out=XA[:, j, :], in0=XA[:, j, :], in1=T["Bb0"][:], op=ALU.add),
                     reads=[('XA', j), "Bb0"], writes=[('XA', j)])
            cx.to_featmajor(TB, [('TB', j) for j in range(4)], HT[1], 'HT1', 4, 8)
            for s in range(11):
                w8_issue(w8_i + 1)
                wb, wk = W8[w8_i % 2], ('W8', w8_i % 2)
                for fc in range(2):
                    psg, pkg = cx.psum()
                    for c in range(8):
                        p.op('pe', lambda e, c=c, fc=fc, psg=psg, wb=wb: e.matmul(psg[:], lhsT=wb[:, c, fc * 128:(fc + 1) * 128], rhs=HT[1][:, c, :],
                                                                                 start=(c == 0), stop=(c == 7)),
                             reads=[('HT1', c), wk], writes=[pkg])
                    psu, pku = cx.psum()
                    for c in range(8):
                        p.op('pe', lambda e, c=c, fc=fc, psu=psu, wb=wb: e.matmul(psu[:], lhsT=wb[:, c, 256 + fc * 128:256 + (fc + 1) * 128], rhs=HT[1][:, c, :],
                                                                                 start=(c == 0), stop=(c == 7)),
                             reads=[('HT1', c), wk], writes=[pku])
                    f = s * 2 + fc
                    sg, sgk = SG[f % 2], ('SG', f % 2)
                    p.op('act', lambda e, psg=psg, sg=sg: e.activation(out=sg[:], in_=psg[:], func=AF.Silu), reads=[pkg], writes=[sgk])
                    p.op('dve', lambda e, psu=psu, sg=sg, f=f: e.tensor_tensor(out=HID[:, f, :], in0=psu[:], in1=sg[:], op=ALU.mult),
                         reads=[pku, sgk], writes=[('HID', f)])
                w8_i += 1
            for n in range(4):
                wd_issue(wd_i + 1)
                wb, wk = WD[wd_i % 2], ('WD', wd_i % 2)
                for j in range(4):
                    ps, pk = cx.psum()
                    for f in range(22):
                        p.op('pe', lambda e, f=f, j=j, ps=ps, wb=wb: e.matmul(ps[:, 0:256], lhsT=HID[:, f, j * 128:(j + 1) * 128], rhs=wb[:, f, :],
                                                                             start=(f == 0), stop=(f == 21)),
                             reads=[('HID', f), wk], writes=[pk])
                    sl = slice(n * 256, (n + 1) * 256)
                    p.op('dve', lambda e, j=j, ps=ps, sl=sl: e.tensor_tensor(out=YB[:, j, sl], in0=ps[:, 0:256], in1=T["G3g"][:, sl], op=ALU.mult),
                         reads=[pk, "G3g"], writes=[('YB', j)])
                wd_i += 1
            x2v = x2_d[r0:r0 + 512, :].rearrange("(j p) d -> p j d", p=128)
            for which in range(2):
                G, B = ("G4", "B4") if which == 0 else ("G5", "B5")
                for j in range(4):
                    if which == 0:
                        p.op('dve', lambda e, j=j: e.scalar_tensor_tensor(out=XA[:, j, :], in0=XA[:, j, :], scalar=ALPHA, in1=YB[:, j, :],
                                                                          op0=ALU.mult, op1=ALU.add),
                             reads=[('XA', j), ('YB', j)], writes=[('XA', j)])
                        cx.layer_norm_tile(XA[:, j, :], ('XA', j), 'b')
                    p.op('dve', lambda e, j=j, G=G: e.tensor_tensor(out=TMP[:], in0=XA[:, j, :], in1=T[G][:], op=ALU.mult),
                         reads=[('XA', j), G], writes=['TMP'])
                    p.op('dve', lambda e, j=j, B=B: e.tensor_tensor(out=TB[:, j, :], in0=TMP[:], in1=T[B][:], op=ALU.add),
                         reads=['TMP', B], writes=[('TB', j)])
                    if which == 1:
                        p.op('dve', lambda e, j=j: e.tensor_tensor(out=YB[:, j, :], in0=XA[:, j, :], in1=T["Gb1"][:], op=ALU.mult),
                             reads=[('XA', j), "Gb1"], writes=[('YB', j)])
                        p.op('dve', lambda e, j=j: e.tensor_tensor(out=YB[:, j, :], in0=YB[:, j, :], in1=T["Bb1"][:], op=ALU.add),
                             reads=[('YB', j), "Bb1"], writes=[('YB', j)])
                        p.dma('sp', x2v[:, j, :], YB[:, j, :], reads=[('YB', j)])
                cx.to_featmajor(TB, [('TB', j) for j in range(4)], HT[which], 'HT%d' % which, 4, 8)
                nslab = 6 if which == 0 else 3
                for s in range(nslab):
                    w8_issue(w8_i + 1)
                    wb, wk = W8[w8_i % 2], ('W8', w8_i % 2)
                    if which == 0 and s < 3:
                        dst_d, ds, rope = k_d, s, True
                    elif which == 0:
                        dst_d, ds, rope = v_d, s - 3, False
                    else:
                        dst_d, ds, rope = q_d, s, True
                    for j in range(4):
                        ps, pk = cx.psum()
                        for c in range(8):
                            p.op('pe', lambda e, c=c, j=j, ps=ps, wb=wb, which=which: e.matmul(ps[:], lhsT=HT[which][:, c, j * 128:(j + 1) * 128], rhs=wb[:, c, :],
                                                                                              start=(c == 0), stop=(c == 7)),
                                 reads=[('HT%d' % which, c), wk], writes=[pk])
                        oap = OUTB[:, j, ds * 512:(ds + 1) * 512]
                        ok = ('OUTB', j, ds)
                        if rope:
                            tile = blk * 4 + j
                            _rope_apply(cx, ps[:], pk, oap, ok, cos[:, tile, :], sin[:, tile, :], 8, RT)
                        else:
                            cx.copy('act', oap, ps[:], [pk], [ok])
                    w8_i += 1
                    if ds == 2:
                        dv = dst_d[r0:r0 + 512, :].rearrange("(j p) d -> p j d", p=128)
                        for j in range(4):
                            p.dma('sp', dv[:, j, :], OUTB[:, j, :], reads=[('OUTB', j, 0), ('OUTB', j, 1), ('OUTB', j, 2)])
        p.finish()
        p.emit()
    return nc


def _cT(c_row):
    return np.ascontiguousarray(c_row.reshape(8, 128).T)


def _ada_blocks(ada_w, ada_b, idxs):
    w = np.ascontiguousarray(np.stack([ada_w[:, i * D:(i + 1) * D] for i in idxs]))
    b = np.ascontiguousarray(np.stack([ada_b[i * D:(i + 1) * D] for i in idxs]))
    return w, b


def maps_B(inp, yg):
    idxs = [2, 3, 4, 5, 12, 13, 6, 7]
    aw, ab = _ada_blocks(inp['ada_w'], inp['ada_b'], idxs)
    maps = []
    for c in range(NCORE):
        b, s0 = c // 4, (c % 4) * TPC
        maps.append({
            "x": np.ascontiguousarray(inp['x'][b, s0:s0 + TPC]),
            "yg": np.ascontiguousarray(yg[b, s0:s0 + TPC]),
            "cT": _cT(inp['c'][b]),
            "adaw": aw, "adab": ab,
            "lng": np.ascontiguousarray(inp['ln_g'][0]), "lnb": np.ascontiguousarray(inp['ln_b'][0]),
            "w_out": inp['rw_w_out'][0], "w_gu": inp['ffn_w_gu'][0], "w_down": inp['ffn_w_down'][0],
            "w_kv": inp['kv_w'], "w_q": inp['attn_w_q'][0],
            "posb": np.full((128, 1), float(s0), np.float32),
        })
    return maps


CH = 128
DECAY_SCALE = -math.exp(-0.5)
LNX_EPS = 64e-5


def build_A(nblk=SEQ // 512):
    nc = bass.Bass("TRN2", target_bir_lowering=False)
    dt = lambda name, shape, dtype, kind: nc.dram_tensor(name, shape, dtype, kind=kind).ap()
    xp_d = dt("xpad", [SEQ + 1, D], F32, "ExternalInput")
    cT_d = dt("cT", [128, 8], F32, "ExternalInput")
    adaw_d = dt("adaw", [2, D, D], F32, "ExternalInput")
    adab_d = dt("adab", [2, D], F32, "ExternalInput")
    muT_d = dt("muT", [128, 6, 8], F32, "ExternalInput")
    wrkv_d = dt("w_rkv", [D, 768], F32, "ExternalInput")
    wl1_d = dt("w_l1", [D, 288], F32, "ExternalInput")
    w2a_d = dt("w2a", [128, 256], F32, "ExternalInput")
    g2_d = dt("g2", [160, 256], F32, "ExternalInput")
    chv_d = dt("chv", [128, 2, 6], F32, "ExternalInput")
    lnx_d = dt("lnx", [2, 256], F32, "ExternalInput")
    yg_d = dt("yg", [SEQ, 256], F32, "ExternalOutput")

    with ExitStack() as st:
        cx = Ctx(nc, st)
        p = cx.p
        sb = cx.sb
        ident = cx.ident
        X = sb("X", [128, 4, D], F32)
        XP = sb("XP", [128, 4, D], F32)
        HB = sb("HB", [128, 4, D], BF16)
        XXB = sb("XXB", [128, 4, D], BF16)
        hT = sb("hT", [128, 8, 512], BF16)
        xxT = sb("xxT", [128, 8, 512], BF16)
        SC1 = sb("SC1", [128, D], F32)
        SH = sb("SH", [128, D], F32)
        W = sb("W", [128, 8, 1056], BF16)
        WM = sb("WM", [128, 8, 1056], BF16)
        muT = sb("muT_s", [128, 6, 8], F32)
        W2A = sb("W2A", [128, 256], BF16)
        G2a = sb("G2a", [128, 256], BF16)
        G2b = sb("G2b", [32, 256], BF16)
        chv = sb("chv_s", [128, 2, 6], F32)
        LNXG = sb("LNXG", [128, 256], F32)
        LNXB = sb("LNXB", [128, 256], F32)
        identf = sb("identf", [128, 128], F32)
        M1 = sb("M1", [128, 256], BF16)
        ML = sb("ML", [128, 128], BF16)
        RMASK = sb("RMASK", [128, 512], BF16)
        HSEL = sb("HSEL", [128, 2], F32)
        BONES = sb("BONES", [128, 128], F32)
        S = sb("S", [64, 4, 64], F32)
        Wbuf = hT
        WST = XP[:].rearrange("p a b -> p (a b)")[:, 0:1056]
        p.op('dve', lambda e: e.tensor_copy(out=identf[:], in_=ident[:]), reads=['ident'], writes=['identf'])
        p.op('pool', lambda e: e.memset(M1[:], 1.0), writes=['M1'])
        p.op('pool', lambda e: e.affine_select(out=M1[:, 0:128], in_=M1[:, 0:128], pattern=[[1, 128]], compare_op=ALU.is_gt, fill=0.0,
                                               base=0, channel_multiplier=-1), reads=['M1'], writes=['M1'])
        p.op('pool', lambda e: e.affine_select(out=M1[:, 128:256], in_=M1[:, 128:256], pattern=[[1, 128]], compare_op=ALU.is_ge, fill=0.0,
                                               base=0, channel_multiplier=-1), reads=['M1'], writes=['M1'])
        p.op('pool', lambda e: e.memset(ML[:], 1.0), writes=['ML'])
        p.op('pool', lambda e: e.affine_select(out=ML[:], in_=ML[:], pattern=[[-1, 128]], compare_op=ALU.is_gt, fill=0.0,
                                               base=0, channel_multiplier=1), reads=['ML'], writes=['ML'])
        p.op('pool', lambda e: e.memset(RMASK[:], 1.0), writes=['RMASK'])
        for q in range(4):
            p.op('pool', lambda e, q=q: e.memset(RMASK[:, q * 128:q * 128 + 1], 0.0), reads=['RMASK'], writes=['RMASK'])
        p.op('pool', lambda e: e.memset(HSEL[:], 0.0), writes=['HSEL'])
        p.op('pool', lambda e: e.memset(HSEL[0:64, 0:1], 1.0), reads=['HSEL'], writes=['HSEL'])
        p.op('pool', lambda e: e.memset(HSEL[64:128, 1:2], 1.0), reads=['HSEL'], writes=['HSEL'])
        p.op('pool', lambda e: e.memset(BONES[:], 0.0), writes=['BONES'])
        p.op('pool', lambda e: e.memset(BONES[0:64, 0:64], 1.0), reads=['BONES'], writes=['BONES'])
        p.op('pool', lambda e: e.memset(BONES[64:128, 64:128], 1.0), reads=['BONES'], writes=['BONES'])
        p.op('pool', lambda e: e.memset(S[:], 0.0), writes=[('S', h) for h in range(4)])
        p.dma('sp', muT[:], muT_d, writes=['muT'])
        p.dma('sp', chv[:], chv_d, writes=['chv'])
        p.op('dve', lambda e: e.tensor_scalar(out=chv[:, :, 5], in0=chv[:, :, 3], scalar1=-1.0, scalar2=1.0, op0=ALU.mult, op1=ALU.add),
             reads=['chv'], writes=['chv'])
        cx.bcast_row(LNXG[:], 'LNXG', lnx_d[0:1, :])
        cx.bcast_row(LNXB[:], 'LNXB', lnx_d[1:2, :])
        p.dma('pool', W2A[:], w2a_d, writes=['W2A'])
        p.dma('pool', G2a[:], g2_d[0:128, :], writes=['G2a'])
        p.dma('pool', G2b[:], g2_d[128:160, :], writes=['G2b'])
        scB = _silu_c_bcast(cx, cT_d)
        _mod_tile(cx, scB, SH[:], 'SH', adaw_d[0], adab_d[0:1, :], Wbuf, 'Wbuf', X[:, 0, :], ('X', 0), False)
        _mod_tile(cx, scB, SC1[:], 'SC1', adaw_d[1], adab_d[1:2, :], Wbuf, 'Wbuf', X[:, 1, :], ('X', 1), True)
        wr = wrkv_d.rearrange("(c p) n -> p c n", p=128)
        wl = wl1_d.rearrange("(c p) n -> p c n", p=128)
        groups = [(0, 256, 0), (256, 512, 1), (512, 768, 2), (768, 832, 3), (832, 896, 4), (896, 1056, 5)]
        for c in range(8):
            p.dma('sp', WST[:, 0:768], wr[:, c, :], writes=['WST'])
            p.dma('sp', WST[:, 768:1056], wl[:, c, :], writes=['WST'])
            p.op('act', lambda e, c=c: e.copy(out=W[:, c, :], in_=WST), reads=['WST'], writes=['W'])
            for (lo, hi, mi) in groups:
                p.op('dve', lambda e, c=c, lo=lo, hi=hi, mi=mi: e.tensor_scalar(out=WM[:, c, lo:hi], in0=WST[:, lo:hi], scalar1=muT[:, mi, c:c + 1],
                                                                                scalar2=None, op0=ALU.mult),
                     reads=['WST', 'muT'], writes=['WM'])

        p.barrier()
        fnames = ["Rf", "Kf", "Af", "LW", "CS", "E1", "E2", "E3", "E4", "KK", "KM", "Bf", "T1"]
        Fm = {n: sb("F_" + n, [128, 512], F32) for n in fnames}
        PROD = [sb("PROD%d" % m, [128, 512], F32) for m in range(2)]
        bnames = ["RT", "KT", "BT", "AT", "KH", "BH", "VT"]
        Bm = [{n: sb("B_%s%d" % (n, m), [128, 512], BF16) for n in bnames} for m in range(2)]
        LA = sb("LA", [128, 512], BF16)
        GS = sb("GS", [128, 2, 512], BF16)
        Gtok = sb("Gtok", [128, 4, 256], F32)
        TMs2 = [[sb("TMs%d_%d" % (m, i), [128, 512], BF16) for i in range(2)] for m in range(2)]
        G1s_ = [sb("G1s%d" % h, [128, 256], BF16) for h in range(8)]
        G2s_ = [sb("G2s%d" % h, [128, 256], BF16) for h in range(8)]
        Lb_ = [[sb("Lb%d_%d" % (h, i), [128, 128], BF16) for i in range(2)] for h in range(8)]
        Gb_ = [[sb("Gb%d_%d" % (h, i), [128, 128], BF16) for i in range(2)] for h in range(8)]
        TTb_ = [[sb("TTb%d_%d" % (h, i), [128, 128], BF16) for i in range(2)] for h in range(8)]
        Xs_ = [sb("Xs%d" % h, [128, 64], BF16) for h in range(8)]
        WU_ = [sb("WU%d" % h, [128, 128], BF16) for h in range(8)]
        DP_ = [sb("DP%d" % h, [128, 64], F32) for h in range(8)]
        MT_ = [sb("MT%d" % h, [64, 64], F32) for h in range(8)]
        Ns_ = [sb("Ns%d" % h, [64, 64], F32) for h in range(8)]
        QT_ = [sb("QT%d" % h, [64, 128], F32) for h in range(8)]
        gst_ = [sb("gst%d" % h, [128, 6], F32) for h in range(8)]
        gmv_ = [sb("gmv%d" % h, [128, 2], F32) for h in range(8)]
        grs_ = [sb("grs%d" % h, [128, 1], F32) for h in range(8)]
        gnm_ = [sb("gnm%d" % h, [128, 1], F32) for h in range(8)]
        YO2 = [sb("YO%d" % i, [128, 256], F32) for i in range(2)]
        SBON2 = [sb("SBON%d" % i, [128, 4], F32) for i in range(2)]
        PCs = [sb("PCs%d" % m, [128, 4], F32) for m in range(2)]
        cx.lnxeps = sb("lnxeps", [128, 1], F32)
        p.op('pool', lambda e: e.memset(cx.lnxeps[:], LNX_EPS), writes=['lnxeps'])

        def proj_fm(ps, pk, lo, n):
            for c in range(8):
                p.op('pe', lambda e, c=c: e.matmul(ps[0:n, :], lhsT=W[:, c, lo:lo + n], rhs=hT[:, c, :], start=(c == 0), stop=False),
                     reads=[('hT', c), 'W'], writes=[pk])
            for c in range(8):
                p.op('pe', lambda e, c=c: e.matmul(ps[0:n, :], lhsT=WM[:, c, lo:lo + n], rhs=xxT[:, c, :], start=False, stop=(c == 7)),
                     reads=[('xxT', c), 'WM'], writes=[pk])


        def head_chain(m, hl, q, cs_):
            B_ = Bm[m]
            bk = lambda n: ('B', n, m)
            hh = 2 * m + hl
            P = slice(64 * hl, 64 * hl + 64)
            TMq = TMs2[m][q % 2]
            YO = YO2[q % 2]
            SBON = SBON2[q % 2]
            At_h = TMq[:, 64 * hl:64 * hl + 64]
            Bh_h = TMq[:, 128 + 64 * hl:128 + 64 * hl + 64]
            Kh_h = TMq[:, 256 + 64 * hl:256 + 64 * hl + 64]
            V_h = TMq[:, 384 + 64 * hl:384 + 64 * hl + 64]
            tmk = ('TMs', m, q % 2)
            hb = hh + 4 * (q % 2)
            G1s, G2s, Lb, Gb, TTb, Xs, WU, DP, MT, Ns, QT = G1s_[hb], G2s_[hb], Lb_[hb], Gb_[hb], TTb_[hb], Xs_[hb], WU_[hb], DP_[hb], MT_[hb], Ns_[hb], QT_[hb]
            gst, gmv, grs, gnm = gst_[hb], gmv_[hb], grs_[hb], gnm_[hb]
            K_ = lambda n: (n, hb)
            ps, pk = cx.psum()
            p.op('pe', lambda e, ps=ps: e.matmul(ps[:, 0:128], lhsT=B_["BT"][P, cs_], rhs=B_["AT"][P, cs_], start=True, stop=True), reads=[bk("BT"), bk("AT")], writes=[pk])
            p.op('pe', lambda e, ps=ps: e.matmul(ps[:, 128:256], lhsT=B_["BT"][P, cs_], rhs=B_["RT"][P, cs_], start=True, stop=True), reads=[bk("BT"), bk("RT")], writes=[pk])
            p.op('dve', lambda e, ps=ps: e.tensor_tensor(out=G1s[:], in0=ps[:, 0:256], in1=M1[:], op=ALU.mult), reads=[pk, 'M1'], writes=[K_('G1s')])
            ps, pk = cx.psum()
            p.op('pe', lambda e, ps=ps: e.matmul(ps[:, 0:128], lhsT=B_["KT"][P, cs_], rhs=B_["AT"][P, cs_], start=True, stop=True), reads=[bk("KT"), bk("AT")], writes=[pk])
            p.op('pe', lambda e, ps=ps: e.matmul(ps[:, 128:256], lhsT=B_["KT"][P, cs_], rhs=B_["RT"][P, cs_], start=True, stop=True), reads=[bk("KT"), bk("RT")], writes=[pk])
            p.op('dve', lambda e, ps=ps: e.tensor_tensor(out=G2s[:], in0=ps[:, 0:256], in1=M1[:], op=ALU.mult), reads=[pk, 'M1'], writes=[K_('G2s')])
            ps, pk = cx.psum()
            p.op('pe', lambda e, ps=ps: e.matmul(ps[:, 0:128], lhsT=B_["AT"][P, cs_], rhs=B_["BT"][P, cs_], start=True, stop=True), reads=[bk("BT"), bk("AT")], writes=[pk])
            p.op('dve', lambda e, ps=ps: e.tensor_tensor(out=Lb[0][:], in0=ps[:, 0:128], in1=ML[:], op=ALU.mult), reads=[pk, 'ML'], writes=[K_('Lb0')])
            yield
            p.op('act', lambda e: e.copy(out=Gb[0][:], in_=G1s[:, 0:128]), reads=[K_('G1s')], writes=[K_('Gb0')])
            p.op('pool', lambda e: e.tensor_tensor(out=TTb[0][:], in0=G1s[:, 0:128], in1=ident[:], op=ALU.add), reads=[K_('G1s'), 'ident'], writes=[K_('TTb0')])
            ps, pk = cx.psum()
            p.op('pe', lambda e, ps=ps: e.matmul(ps[:, 0:64], lhsT=G2s[:, 0:128], rhs=V_h, start=True, stop=True), reads=[K_('G2s'), tmk], writes=[pk])
            p.op('act', lambda e, ps=ps: e.copy(out=Xs[:], in_=ps[:, 0:64]), reads=[pk], writes=[K_('Xs')])
            yield
            cur = 0
            for lvl in range(1, 7):
                nxt = 1 - cur
                ps, pk = cx.psum()
                p.op('pe', lambda e, ps=ps, cur=cur: e.matmul(ps[:, 0:128], lhsT=Gb[cur][:], rhs=Lb[cur][:], start=True, stop=True), reads=[K_('Gb%d' % cur), K_('Lb%d' % cur)], writes=[pk])
                p.op('act', lambda e, ps=ps, nxt=nxt: e.copy(out=Lb[nxt][:], in_=ps[:, 0:128]), reads=[pk], writes=[K_('Lb%d' % nxt)])
                if lvl < 6:
                    ps2, pk2 = cx.psum()
                    p.op('pe', lambda e, ps2=ps2, cur=cur: e.matmul(ps2[:, 0:128], lhsT=Lb[cur][:], rhs=Gb[cur][:], start=True, stop=True), reads=[K_('Gb%d' % cur), K_('Lb%d' % cur)], writes=[pk2])
                    p.op('act', lambda e, ps2=ps2, nxt=nxt: e.copy(out=Gb[nxt][:], in_=ps2[:, 0:128]), reads=[pk2], writes=[K_('Gb%d' % nxt)])
                yield
                ps3, pk3 = cx.psum()
                p.op('pe', lambda e, ps3=ps3, cur=cur, nxt=nxt: e.matmul(ps3[:, 0:128], lhsT=Lb[nxt][:], rhs=TTb[cur][:], start=True, stop=True), reads=[K_('Lb%d' % nxt), K_('TTb%d' % cur)], writes=[pk3])
                p.op('dve', lambda e, ps3=ps3, cur=cur, nxt=nxt: e.tensor_tensor(out=TTb[nxt][:], in0=ps3[:, 0:128], in1=TTb[cur][:], op=ALU.add), reads=[pk3, K_('TTb%d' % cur)], writes=[K_('TTb%d' % nxt)])
                cur = nxt
                yield
            TT = TTb[cur]
            ttk = K_('TTb%d' % cur)
            ps, pk = cx.psum()
            p.op('pe', lambda e, ps=ps: e.matmul(ps[:, 0:64], lhsT=TT[:], rhs=At_h, start=True, stop=True), reads=[ttk, tmk], writes=[pk])
            p.op('pe', lambda e, ps=ps: e.matmul(ps[:, 64:128], lhsT=TT[:], rhs=Xs[:], start=True, stop=True), reads=[ttk, K_('Xs')], writes=[pk])
            p.op('dve', lambda e, ps=ps: e.tensor_copy(out=WU[:], in_=ps[:, 0:128]), reads=[pk], writes=[K_('WU')])
            p.op('pool', lambda e: e.tensor_scalar(out=DP[P, :], in0=identf[P, 64 * hl:64 * hl + 64], scalar1=PCs[m][P, q:q + 1], scalar2=None, op0=ALU.mult),
                 reads=['identf', ('PC', m)], writes=[K_('DP')])
            yield
            ps, pk = cx.psum()
            p.op('pe', lambda e, ps=ps: e.matmul(ps[0:64, 0:64], lhsT=WU[:, 0:64], rhs=Bh_h, start=True, stop=True), reads=[K_('WU'), tmk], writes=[pk])
            p.op('pe', lambda e, ps=ps: e.matmul(ps[0:64, 64:128], lhsT=identf[P, 64 * hl:64 * hl + 64], rhs=DP[P, :], start=True, stop=True), reads=['identf', K_('DP')], writes=[pk])
            p.op('act', lambda e, ps=ps: e.copy(out=MT[:], in_=ps[0:64, 0:64]), reads=[pk], writes=[K_('MT')])
            p.op('dve', lambda e, ps=ps: e.tensor_tensor(out=MT[:], in0=ps[0:64, 64:128], in1=MT[:], op=ALU.add), reads=[pk, K_('MT')], writes=[K_('MT')])
            ps, pk = cx.psum()
            p.op('pe', lambda e, ps=ps: e.matmul(ps[0:64, 0:64], lhsT=Bh_h, rhs=WU[:, 64:128], start=True, stop=False), reads=[K_('WU'), tmk], writes=[pk])
            p.op('pe', lambda e, ps=ps: e.matmul(ps[0:64, 0:64], lhsT=Kh_h, rhs=V_h, start=False, stop=True), reads=[tmk], writes=[pk])
            p.op('act', lambda e, ps=ps: e.copy(out=Ns[:], in_=ps[0:64, 0:64]), reads=[pk], writes=[K_('Ns')])
            ps, pk = cx.psum()
            p.op('pe', lambda e, ps=ps: e.matmul(ps[0:64, 0:128], lhsT=ident[:, 64 * hl:64 * hl + 64], rhs=B_["RT"][:, cs_], start=True, stop=False), reads=['ident', bk("RT")], writes=[pk])
            p.op('pe', lambda e, ps=ps: e.matmul(ps[0:64, 0:128], lhsT=WU[:, 0:64], rhs=G1s[:, 128:256], start=False, stop=True), reads=[K_('WU'), K_('G1s')], writes=[pk])
            p.op('act', lambda e, ps=ps: e.copy(out=QT[:], in_=ps[0:64, 0:128]), reads=[pk], writes=[K_('QT')])
            yield
            ps, pk = cx.psum()
            p.op('pe', lambda e, ps=ps: e.matmul(ps[:, 0:64], lhsT=G1s[:, 128:256], rhs=WU[:, 64:128], start=True, stop=False), reads=[K_('G1s'), K_('WU')], writes=[pk])
            p.op('pe', lambda e, ps=ps: e.matmul(ps[:, 0:64], lhsT=G2s[:, 128:256], rhs=V_h, start=False, stop=False), reads=[K_('G2s'), tmk], writes=[pk])
            p.op('pe', lambda e, ps=ps: e.matmul(ps[:, 0:64], lhsT=QT[:], rhs=S[:, hh, :], start=False, stop=True), reads=[K_('QT'), ('S', hh)], writes=[pk])
            yo = YO[:, hh * 64:(hh + 1) * 64]
            yk = ('YO', hh, q % 2)
            p.op('act', lambda e, ps=ps: e.copy(out=yo, in_=ps[:, 0:64]), reads=[pk], writes=[yk])
            ps, pk = cx.psum()
            p.op('pe', lambda e, ps=ps: e.matmul(ps[0:64, 0:64], lhsT=MT[:], rhs=S[:, hh, :], start=True, stop=True), reads=[K_('MT'), ('S', hh)], writes=[pk])
            p.op('dve', lambda e, ps=ps: e.tensor_tensor(out=S[:, hh, :], in0=ps[0:64, 0:64], in1=Ns[:], op=ALU.add), reads=[pk, K_('Ns')], writes=[('S', hh)])
            yield
            p.op('dve', lambda e: e.bn_stats(out=gst[:], in_=yo), reads=[yk], writes=[K_('gst')])
            p.op('dve', lambda e: e.bn_aggr(out=gmv[:], in_=gst[:]), reads=[K_('gst')], writes=[K_('gmv')])
            p.op('act', lambda e: e.activation(out=grs[:], in_=gmv[:, 1:2], func=AF.Sqrt, bias=cx.lnxeps[:], scale=1.0), reads=[K_('gmv'), 'lnxeps'], writes=[K_('grs')])
            yield
            p.op('dve', lambda e: e.reciprocal(out=grs[:], in_=grs[:]), reads=[K_('grs')], writes=[K_('grs')])
            p.op('pool', lambda e: e.tensor_scalar(out=gnm[:], in0=gmv[:, 0:1], scalar1=grs[:], scalar2=-1.0, op0=ALU.mult, op1=ALU.mult), reads=[K_('gmv'), K_('grs')], writes=[K_('gnm')])
            p.op('act', lambda e: e.activation(out=yo, in_=yo, func=AF.Identity, bias=gnm[:], scale=grs[:]), reads=[yk, K_('grs'), K_('gnm')], writes=[yk])
            yield
            sl = slice(hh * 64, (hh + 1) * 64)
            p.op('pool', lambda e: e.tensor_tensor(out=yo, in0=yo, in1=LNXG[:, sl], op=ALU.mult), reads=[yk, 'LNXG'], writes=[yk])
            p.op('pool', lambda e: e.tensor_tensor(out=yo, in0=yo, in1=LNXB[:, sl], op=ALU.add), reads=[yk, 'LNXB'], writes=[yk])
            p.op('dve', lambda e: e.scalar_tensor_tensor(out=yo, in0=V_h, scalar=SBON[:, hh:hh + 1], in1=yo, op0=ALU.mult, op1=ALU.add),
                 reads=[yk, tmk, ('SBON', m, q % 2)], writes=[yk])

        for blk in range(nblk):
            r0 = blk * 512
            xv = xp_d[r0 + 1:r0 + 513, :].rearrange("(j p) d -> p j d", p=128)
            xpv = xp_d[r0:r0 + 512, :].rearrange("(j p) d -> p j d", p=128)
            for j in range(4):
                p.dma('sp', X[:, j, :], xv[:, j, :], writes=[('X', j)])
                p.dma('sp', XP[:, j, :], xpv[:, j, :], writes=[('XP', j)])
            for j in range(4):
                p.op('dve', lambda e, j=j: e.tensor_tensor(out=X[:, j, :], in0=X[:, j, :], in1=SC1[:], op=ALU.mult), reads=[('X', j), 'SC1'], writes=[('X', j)])
                p.op('pool', lambda e, j=j: e.tensor_tensor(out=XP[:, j, :], in0=XP[:, j, :], in1=SC1[:], op=ALU.mult), reads=[('XP', j), 'SC1'], writes=[('XP', j)])
                p.op('dve', lambda e, j=j: e.tensor_tensor(out=HB[:, j, :], in0=X[:, j, :], in1=SH[:], op=ALU.add), reads=[('X', j), 'SH'], writes=[('HB', j)])
                p.op('pool', lambda e, j=j: e.tensor_tensor(out=XXB[:, j, :], in0=XP[:, j, :], in1=X[:, j, :], op=ALU.subtract), reads=[('XP', j), ('X', j)], writes=[('XXB', j)])
            if blk == 0:
                p.op('dve', lambda e: e.tensor_scalar(out=XXB[0:1, 0, :], in0=HB[0:1, 0, :], scalar1=-1.0, scalar2=None, op0=ALU.mult),
                     reads=[('HB', 0), ('XXB', 0)], writes=[('XXB', 0)])
            cx.to_featmajor(HB, [('HB', j) for j in range(4)], hT, 'hT', 4, 8)
            cx.to_featmajor(XXB, [('XXB', j) for j in range(4)], xxT, 'xxT', 4, 8)
            ps, pk = cx.psum()
            proj_fm(ps, pk, 768, 128)
            p.op('act', lambda e, ps=ps: e.activation(out=LA[0:64, :], in_=ps[0:64, :], func=AF.Tanh), reads=[pk], writes=['LA'])
            p.op('act', lambda e, ps=ps: e.copy(out=LA[64:128, :], in_=ps[64:128, :]), reads=[pk], writes=['LA'])
            ps, pk = cx.psum()
            proj_fm(ps, pk, 896, 128)
            p.op('act', lambda e, ps=ps: e.activation(out=GS[:, 0, :], in_=ps[:], func=AF.Sigmoid), reads=[pk], writes=['GS'])
            ps, pk = cx.psum()
            proj_fm(ps, pk, 1024, 32)
            p.op('act', lambda e, ps=ps: e.activation(out=GS[0:32, 1, :], in_=ps[0:32, :], func=AF.Sigmoid), reads=[pk], writes=['GS'])
            for j in range(4):
                ps, pk = cx.psum()
                p.op('pe', lambda e, j=j, ps=ps: e.matmul(ps[:, 0:256], lhsT=GS[:, 0, j * 128:(j + 1) * 128], rhs=G2a[:], start=True, stop=False),
                     reads=['GS', 'G2a'], writes=[pk])
                p.op('pe', lambda e, j=j, ps=ps: e.matmul(ps[:, 0:256], lhsT=GS[0:32, 1, j * 128:(j + 1) * 128], rhs=G2b[:], start=False, stop=True),
                     reads=['GS', 'G2b'], writes=[pk])
                p.op('act', lambda e, j=j, ps=ps: e.copy(out=Gtok[:, j, :], in_=ps[:, 0:256]), reads=[pk], writes=[('Gtok', j)])
            for m in range(2):
                B_ = Bm[m]
                bk = lambda n, m=m: ('B', n, m)
                ps, pk = cx.psum()
                proj_fm(ps, pk, m * 128, 128)
                p.op('act', lambda e, ps=ps: e.copy(out=Fm["Rf"][:], in_=ps[:]), reads=[pk], writes=['Rf'])
                ps, pk = cx.psum()
                proj_fm(ps, pk, 256 + m * 128, 128)
                p.op('act', lambda e, ps=ps: e.copy(out=Fm["Kf"][:], in_=ps[:]), reads=[pk], writes=['Kf'])
                ps, pk = cx.psum()
                proj_fm(ps, pk, 512 + m * 128, 128)
                p.op('act', lambda e, ps=ps, B_=B_: e.copy(out=B_["VT"][:], in_=ps[:]), reads=[pk], writes=[bk("VT")])
                ps, pk = cx.psum()
                p.op('pe', lambda e, ps=ps, m=m: e.matmul(ps[:], lhsT=W2A[0:64, m * 128:(m + 1) * 128], rhs=LA[0:64, :], start=True, stop=True),
                     reads=['W2A', 'LA'], writes=[pk])
                p.op('act', lambda e, ps=ps, m=m: e.activation(out=Fm["LW"][:], in_=ps[:], func=AF.Sigmoid, bias=chv[:, m, 0:1], scale=1.0),
                     reads=[pk, 'chv'], writes=['LW'])
                ps, pk = cx.psum()
                p.op('pe', lambda e, ps=ps, m=m: e.matmul(ps[:], lhsT=W2A[64:128, m * 128:(m + 1) * 128], rhs=LA[64:128, :], start=True, stop=True),
                     reads=['W2A', 'LA'], writes=[pk])
                p.op('act', lambda e, ps=ps, m=m: e.activation(out=Fm["Af"][:], in_=ps[:], func=AF.Sigmoid, bias=chv[:, m, 1:2], scale=1.0),
                     reads=[pk, 'chv'], writes=['Af'])
                F_ = Fm
                p.op('dve', lambda e: e.tensor_scalar(out=F_["LW"][:], in0=F_["LW"][:], scalar1=DECAY_SCALE, scalar2=None, op0=ALU.mult), reads=['LW'], writes=['LW'])
                p.op('dve', lambda e: e.tensor_tensor_scan(out=F_["CS"][:], data0=RMASK[:], data1=F_["LW"][:], initial=0.0, op0=ALU.mult, op1=ALU.add),
                     reads=['LW', 'RMASK'], writes=['CS'])
                p.op('act', lambda e: e.activation(out=F_["E1"][:], in_=F_["CS"][:], func=AF.Exp), reads=['CS'], writes=['E1'])
                p.op('act', lambda e: e.activation(out=F_["E2"][:], in_=F_["CS"][:], func=AF.Exp, scale=-1.0), reads=['CS'], writes=['E2'])
                p.op('pool', lambda e: e.tensor_tensor(out=F_["E3"][:], in0=F_["CS"][:], in1=F_["LW"][:], op=ALU.subtract), reads=['CS', 'LW'], writes=['E3'])
                p.op('act', lambda e: e.activation(out=F_["E3"][:], in_=F_["E3"][:], func=AF.Exp), reads=['E3'], writes=['E3'])
                for q in range(4):
                    p.op('dve', lambda e, q=q: e.tensor_scalar(out=F_["E4"][:, q * 128:(q + 1) * 128], in0=F_["E2"][:, q * 128:(q + 1) * 128],
                                                               scalar1=F_["E1"][:, q * 128 + 127:q * 128 + 128], scalar2=None, op0=ALU.mult),
                         reads=['E1', 'E2'], writes=['E4'])
                p.op('dve', lambda e, m=m: e.tensor_scalar(out=F_["KK"][:], in0=F_["Kf"][:], scalar1=chv[:, m, 2:3], scalar2=None, op0=ALU.mult), reads=['Kf', 'chv'], writes=['KK'])
                p.op('pool', lambda e: e.tensor_tensor(out=F_["T1"][:], in0=F_["KK"][:], in1=F_["KK"][:], op=ALU.mult), reads=['KK'], writes=['T1'])
                ps, pk = cx.psum()
                p.op('pe', lambda e, ps=ps: e.matmul(ps[:], lhsT=BONES[:], rhs=F_["T1"][:], start=True, stop=True), reads=['BONES', 'T1'], writes=[pk])
                p.op('act', lambda e, ps=ps: e.activation(out=F_["T1"][:], in_=ps[:], func=AF.Sqrt), reads=[pk], writes=['T1'])
                p.op('dve', lambda e: e.tensor_scalar(out=F_["T1"][:], in0=F_["T1"][:], scalar1=1e-12, scalar2=None, op0=ALU.max), reads=['T1'], writes=['T1'])
                p.op('dve', lambda e: e.reciprocal(out=F_["T1"][:], in_=F_["T1"][:]), reads=['T1'], writes=['T1'])
                p.op('dve', lambda e: e.tensor_tensor(out=F_["KK"][:], in0=F_["KK"][:], in1=F_["T1"][:], op=ALU.mult), reads=['KK', 'T1'], writes=['KK'])
                p.op('dve', lambda e, m=m: e.tensor_scalar(out=F_["T1"][:], in0=F_["Af"][:], scalar1=chv[:, m, 3:4], scalar2=chv[:, m, 5:6], op0=ALU.mult, op1=ALU.add),
                     reads=['Af', 'chv'], writes=['T1'])
                p.op('pool', lambda e: e.tensor_tensor(out=F_["KM"][:], in0=F_["Kf"][:], in1=F_["T1"][:], op=ALU.mult), reads=['Kf', 'T1'], writes=['KM'])
                p.op('pool', lambda e: e.tensor_tensor(out=F_["Bf"][:], in0=F_["KK"][:], in1=F_["Af"][:], op=ALU.mult), reads=['KK', 'Af'], writes=['Bf'])
                p.op('dve', lambda e, B_=B_: e.tensor_tensor(out=B_["RT"][:], in0=F_["Rf"][:], in1=F_["E1"][:], op=ALU.mult), reads=['Rf', 'E1'], writes=[bk("RT")])
                p.op('pool', lambda e, B_=B_: e.tensor_tensor(out=B_["KT"][:], in0=F_["KM"][:], in1=F_["E2"][:], op=ALU.mult), reads=['KM', 'E2'], writes=[bk("KT")])
                p.op('dve', lambda e, B_=B_: e.tensor_tensor(out=B_["BT"][:], in0=F_["Bf"][:], in1=F_["E2"][:], op=ALU.mult), reads=['Bf', 'E2'], writes=[bk("BT")])
                p.op('dve', lambda e, B_=B_: e.scalar_tensor_tensor(out=B_["AT"][:], in0=F_["KK"][:], scalar=-1.0, in1=F_["E3"][:], op0=ALU.mult, op1=ALU.mult),
                     reads=['KK', 'E3'], writes=[bk("AT")])
                p.op('pool', lambda e, B_=B_: e.tensor_tensor(out=B_["KH"][:], in0=F_["KM"][:], in1=F_["E4"][:], op=ALU.mult), reads=['KM', 'E4'], writes=[bk("KH")])
                p.op('dve', lambda e, B_=B_: e.tensor_tensor(out=B_["BH"][:], in0=F_["Bf"][:], in1=F_["E4"][:], op=ALU.mult), reads=['Bf', 'E4'], writes=[bk("BH")])
                p.op('dve', lambda e, m=m: e.scalar_tensor_tensor(out=PROD[m][:], in0=F_["Rf"][:], scalar=chv[:, m, 4:5], in1=F_["KM"][:], op0=ALU.mult, op1=ALU.mult),
                     reads=['Rf', 'KM', 'chv'], writes=[('PROD', m)])
                for q in range(4):
                    pass
                p.op('act', lambda e, m=m: e.copy(out=PCs[m][:], in_=F_["E1"][:].rearrange("p (q t) -> p q t", t=128)[:, :, 127]), reads=['E1'], writes=[('PC', m)])

            for q0 in (0, 2):
                for q in (q0, q0 + 1):
                    cs_ = slice(q * 128, (q + 1) * 128)
                    for m in range(2):
                        B_ = Bm[m]
                        bk = lambda n, m=m: ('B', n, m)
                        bank, bkk = cx.ptbank()
                        bv = bank[:].rearrange("p a b -> p (a b)")
                        for i, n in enumerate(["AT", "BH", "KH", "VT"]):
                            p.op('pe', lambda e, cs_=cs_, q=q, i=i, n=n, B_=B_, bv=bv: e.transpose(out=bv[:, i * 128:(i + 1) * 128], in_=B_[n][:, cs_], identity=ident[:]),
                                 reads=[bk(n), 'ident'], writes=[bkk])
                        cx.copy('act', TMs2[m][q % 2][:], bv[:, 0:512], [bkk], [('TMs', m, q % 2)])
                        ps, pk = cx.psum()
                        p.op('pe', lambda e, cs_=cs_, q=q, ps=ps, m=m: e.matmul(ps[:, 0:2], lhsT=PROD[m][:, cs_], rhs=HSEL[:], start=True, stop=True), reads=[('PROD', m), 'HSEL'], writes=[pk])
                        p.op('dve', lambda e, cs_=cs_, q=q, ps=ps, m=m: e.tensor_copy(out=SBON2[q % 2][:, 2 * m:2 * m + 2], in_=ps[:, 0:2]), reads=[pk], writes=[('SBON', m, q % 2)])

                alive = [head_chain(m, hl, q, slice(q * 128, (q + 1) * 128)) for q in (q0, q0 + 1) for m in range(2) for hl in range(2)]
                while alive:
                    nxt_alive = []
                    for g_ in alive:
                        try:
                            next(g_)
                            nxt_alive.append(g_)
                        except StopIteration:
                            pass
                    alive = nxt_alive
                for q in (q0, q0 + 1):
                    cs_ = slice(q * 128, (q + 1) * 128)
                    yks = [('YO', h, q % 2) for h in range(4)]
                    p.op('dve', lambda e, cs_=cs_, q=q: e.tensor_tensor(out=YO2[q % 2][:], in0=YO2[q % 2][:], in1=Gtok[:, q, :], op=ALU.mult), reads=yks + [('Gtok', q)], writes=yks)
                    p.dma('sp', yg_d[r0 + q * 128:r0 + (q + 1) * 128, :], YO2[q % 2][:], reads=yks)
        p.finish()
        p.emit()
    return nc


def maps_A(inp):
    aw, ab = _ada_blocks(inp['ada_w'], inp['ada_b'], [0, 1])
    muT = np.ascontiguousarray(inp['rw_mu'][0].reshape(6, 8, 128).transpose(2, 0, 1))
    wl1 = np.ascontiguousarray(np.concatenate([inp['rw_w1'][0], inp['rw_a1'][0], inp['rw_g1'][0]], axis=1))
    maps = []
    for c in range(NCORE):
        b, hg = c // 4, c % 4
        cols = slice(256 * hg, 256 * hg + 256)
        wr = inp['rw_w_rkv'][0]
        wrkv = np.ascontiguousarray(np.concatenate([wr[:, 0 * D:1 * D][:, cols], wr[:, 1 * D:2 * D][:, cols], wr[:, 2 * D:3 * D][:, cols]], axis=1))
        w2a = np.ascontiguousarray(np.concatenate([inp['rw_w2'][0][:, cols], inp['rw_a2'][0][:, cols]], axis=0))
        vec = lambda v: v.reshape(-1)[cols].reshape(2, 128).T
        chv = np.zeros((128, 2, 6), np.float32)
        for i, v in enumerate([inp['rw_w0'][0], inp['rw_a0'][0], inp['rw_k_k'][0], inp['rw_k_a'][0], inp['rw_r_k'][0]]):
            chv[:, :, i] = vec(v)
        xpad = np.concatenate([np.zeros((1, D), np.float32), inp['x'][b]], axis=0)
        maps.append({
            "xpad": xpad, "cT": _cT(inp['c'][b]), "adaw": aw, "adab": ab, "muT": muT,
            "w_rkv": wrkv, "w_l1": wl1, "w2a": w2a, "g2": np.ascontiguousarray(inp['rw_g2'][0][:, cols]),
            "chv": chv, "lnx": np.ascontiguousarray(np.stack([inp['rw_lnx_g'][0][cols], inp['rw_lnx_b'][0][cols]])),
        })
    return maps


HALO = 2048
NEG = -30000.0
REC = 66
PASS_T = 1024


def build_C(do_attn=True, npass=TPC // PASS_T, nexp=NEXP, ntile_attn=32):
    nc = bass.Bass("TRN2", target_bir_lowering=False)
    dt = lambda name, shape, dtype, kind: nc.dram_tensor(name, shape, dtype, kind=kind).ap()
    x2_d = dt("x2", [TPC, D], F32, "ExternalInput")
    q_d = dt("q", [TPC, 1536], BF16, "ExternalInput")
    kh_d = dt("kh", [TPC + HALO, 1536], BF16, "ExternalInput")
    vh_d = dt("vh", [TPC + HALO, 1536], BF16, "ExternalInput")
    hb_d = dt("halo_bias", [128, 1], F32, "ExternalInput")
    cT_d = dt("cT", [128, 8], F32, "ExternalInput")
    adaw_d = dt("adaw", [4, D, D], F32, "ExternalInput")
    adab_d = dt("adab", [4, D], F32, "ExternalInput")
    lng_d = dt("lng", [2, D], F32, "ExternalInput")
    lnb_d = dt("lnb", [2, D], F32, "ExternalInput")
    wo_d = dt("w_o", [512, D], F32, "ExternalInput")
    rt_d = dt("router", [8, D], F32, "ExternalInput")
    wgu_d = dt("moe_gu", [NEXP, D, 2 * D_FFE], F32, "ExternalInput")
    wdn_d = dt("moe_dn", [NEXP, D_FFE, D], F32, "ExternalInput")
    out_d = dt("out", [TPC, D], F32, "ExternalOutput")
    scr_d = dt("scr_att", [3, TPC, 8 * REC], F32, "Internal")
    x3_d = dt("scr_x3", [TPC, D], F32, "Internal")

    with ExitStack() as st:
        cx = Ctx(nc, st)
        p = cx.p
        sb = cx.sb
        ident = cx.ident
        names = ["G1", "Gb0", "Bb0", "G2", "B2", "G3g", "Gb1", "Bb1"]
        T = {n: sb(n, [128, D], F32) for n in names}
        RB = sb("RB", [128, 8, D], F32)
        XA = sb("XA", [128, D], F32)
        YB = sb("YB", [128, D], F32)
        TMP = sb("TMP", [128, D], F32)
        W8 = [sb("W8_%d" % i, [128, 8, 512], BF16) for i in range(2)]
        scB = _silu_c_bcast(cx, cT_d)

        def mod(dst, dkey, blk, plus_one):
            _mod_tile(cx, scB, dst, dkey, adaw_d[blk], adab_d[blk:blk + 1, :], W8[0], ('W8', 0), TMP[:], 'TMP', plus_one)

        mod(T["G1"][:], "G1", 0, True)
        mod(XA[:], 'XA', 1, False)
        mod(YB[:], 'YB', 2, True)
        mod(T["G3g"][:], "G3g", 3, True)
        cx.bcast_row(T["Gb0"][:], "Gb0", lng_d[0:1, :])
        cx.bcast_row(T["Bb0"][:], "Bb0", lnb_d[0:1, :])
        cx.bcast_row(T["Gb1"][:], "Gb1", lng_d[1:2, :])
        cx.bcast_row(T["Bb1"][:], "Bb1", lnb_d[1:2, :])
        p.op('dve', lambda e: e.tensor_tensor(out=T["G2"][:], in0=T["Gb0"][:], in1=YB[:], op=ALU.mult), reads=["Gb0", 'YB'], writes=["G2"])
        p.op('dve', lambda e: e.tensor_tensor(out=T["B2"][:], in0=T["Bb0"][:], in1=YB[:], op=ALU.mult), reads=["Bb0", 'YB'], writes=["B2"])
        p.op('dve', lambda e: e.tensor_tensor(out=T["B2"][:], in0=T["B2"][:], in1=XA[:], op=ALU.add), reads=["B2", 'XA'], writes=["B2"])
        for e_ in range(8):
            cx.bcast_row(RB[:, e_, :], ('RB', e_), rt_d[e_:e_ + 1, :])
        WO = sb("WO", [128, 4, D], BF16)
        p.dma('pool', WO[:, :, 0:512], wo_d.rearrange("(c p) n -> p c n", p=128)[:, :, 0:512], writes=['WO'])
        p.dma('pool', WO[:, :, 512:1024], wo_d.rearrange("(c p) n -> p c n", p=128)[:, :, 512:1024], writes=['WO'])

        if do_attn:
          with ExitStack() as st2:
            sb2 = lambda name, shape, dt_: st2.enter_context(nc.sbuf_tensor(name, shape, dt_))
            MASK = sb2("MASK", [128, 256], F32)
            MASKF = sb2("MASKF", [128, 256], F32)
            hbias = sb2("hbias", [128, 1], F32)
            p.dma('sp', hbias[:], hb_d, writes=['hbias'])
            p.op('pool', lambda e: e.memset(MASK[:], 0.0), writes=['MASK'])
            p.op('pool', lambda e: e.affine_select(out=MASK[:], in_=MASK[:], pattern=[[1, 256]], compare_op=ALU.is_ge, fill=NEG, base=0, channel_multiplier=-1),
                 reads=['MASK'], writes=['MASK'])
            p.op('pool', lambda e: e.affine_select(out=MASK[:], in_=MASK[:], pattern=[[-1, 256]], compare_op=ALU.is_ge, fill=NEG, base=128, channel_multiplier=1),
                 reads=['MASK'], writes=['MASK'])
            p.op('dve', lambda e: e.tensor_copy(out=MASKF[:], in_=MASK[:]), reads=['MASK'], writes=['MASKF'])
            p.op('dve', lambda e: e.tensor_scalar(out=MASKF[:, 0:128], in0=MASKF[:, 0:128], scalar1=hbias[:], scalar2=None, op0=ALU.add),
                 reads=['MASKF', 'hbias'], writes=['MASKF'])
            QB = [sb2("QB%d" % i, [128, 512], BF16) for i in range(2)]
            KB = [sb2("KB%d" % i, [128, 2, 512], BF16) for i in range(2)]
            VB = [sb2("VB%d" % i, [128, 2, 512], BF16) for i in range(2)]
            QTs = [sb2("QT%d" % i, [128, 4, 128], BF16) for i in range(2)]
            KTs = [sb2("KT%d" % i, [128, 4, 256], BF16) for i in range(2)]
            SMs = [sb2("SM%d" % h, [128, 256], F32) for h in range(8)]
            PBs = [sb2("PB%d" % h, [128, 256], BF16) for h in range(8)]
            PTs = [sb2("PT%d" % h, [128, 2, 128], BF16) for h in range(8)]
            OUTA = [sb2("OUTA%d" % i, [128, 8, REC], F32) for i in range(2)]
            mxs = [sb2("mx%d" % h, [128, 1], F32) for h in range(8)]
            nmxs = [sb2("nmx%d" % h, [128, 1], F32) for h in range(8)]

            def attn_head(h, bi, mk, mkk, oa):
                hp, hl = h // 2, h % 2
                P = slice(64 * hl, 64 * hl + 64)
                QT, KT, SM, PB, PT, mx, nmx = QTs[bi], KTs[bi], SMs[h], PBs[h], PTs[h], mxs[h], nmxs[h]
                qk, kk_, oak = ('QT', bi), ('KT', bi), ('OUTA', bi, h)
                K_ = lambda n: (n, h)
                ps, pk = cx.psum()
                p.op('pe', lambda e, ps=ps: e.matmul(ps[:, 0:256], lhsT=QT[P, hp, :], rhs=KT[P, hp, :], start=True, stop=True), reads=[qk, kk_], writes=[pk])
                p.op('dve', lambda e, ps=ps: e.tensor_tensor(out=SM[:], in0=ps[:, 0:256], in1=mk[:], op=ALU.add), reads=[pk, mkk], writes=[K_('SM')])
                yield
                p.op('dve', lambda e: e.reduce_max(out=mx[:], in_=SM[:], axis=AX.X), reads=[K_('SM')], writes=[K_('mx')])
                p.op('dve', lambda e: e.tensor_scalar(out=nmx[:], in0=mx[:], scalar1=-0.125, scalar2=None, op0=ALU.mult), reads=[K_('mx')], writes=[K_('nmx')])
                yield
                p.op('act', lambda e: e.activation(out=PB[:], in_=SM[:], func=AF.Exp, bias=nmx[:], scale=0.125, accum_out=oa[:, h, 64:65]),
                     reads=[K_('SM'), K_('nmx')], writes=[K_('PB'), oak])
                p.op('dve', lambda e: e.tensor_scalar(out=oa[:, h, 65:66], in0=mx[:], scalar1=0.125, scalar2=None, op0=ALU.mult), reads=[K_('mx'), oak], writes=[oak])
                yield
                bank, bkk = cx.ptbank()
                for a in range(2):
                    p.op('pe', lambda e, a=a, bank=bank: e.transpose(out=bank[:, 0, a * 128:(a + 1) * 128], in_=PB[:, a * 128:(a + 1) * 128], identity=ident[:]),
                         reads=[K_('PB'), 'ident'], writes=[bkk])
                cx.copy('act', PT[:].rearrange("p a b -> p (a b)"), bank[:, 0, 0:256], [bkk], [K_('PT')])
                yield
                ps2, pk2 = cx.psum()
                for a in range(2):
                    p.op('pe', lambda e, a=a, ps2=ps2: e.matmul(ps2[:, 0:64], lhsT=PT[:, a, :], rhs=VB[bi][:, a, h * 64:(h + 1) * 64], start=(a == 0), stop=(a == 1)),
                         reads=[K_('PT'), ('VB', bi)], writes=[pk2])
                p.op('dve', lambda e, ps2=ps2: e.tensor_copy(out=oa[:, h, 0:64], in_=ps2[:, 0:64]), reads=[pk2, oak], writes=[oak])

            nb_i = 0
            for g, d in enumerate((1, 4, 16)):
                nblocks = TPC // (128 * d)
                for n in range(nblocks):
                    if n * d * 128 >= ntile_attn * 128:
                        continue
                    for ph in range(d):
                        bi = nb_i % 2
                        nb_i += 1
                        t0 = 128 * d * n + ph
                        qv = q_d[t0:t0 + 127 * d + 1:d, g * 512:(g + 1) * 512]
                        r0 = HALO + 128 * d * (n - 1) + ph
                        kv_ = kh_d[r0:r0 + 255 * d + 1:d, g * 512:(g + 1) * 512].rearrange("(a p) c -> p a c", a=2)
                        vv_ = vh_d[r0:r0 + 255 * d + 1:d, g * 512:(g + 1) * 512].rearrange("(a p) c -> p a c", a=2)
                        p.dma('sp', QB[bi][:], qv, writes=[('QB', bi)])
                        p.dma('sp', KB[bi][:], kv_, writes=[('KB', bi)])
                        p.dma('sp', VB[bi][:], vv_, writes=[('VB', bi)])
                        bank, bkk = cx.ptbank()
                        bv = bank[:].rearrange("p a b -> p (a b)")
                        for hp in range(4):
                            p.op('pe', lambda e, hp=hp, bv=bv, bi=bi: e.transpose(out=bv[:, hp * 128:(hp + 1) * 128], in_=QB[bi][:, hp * 128:(hp + 1) * 128], identity=ident[:]),
                                 reads=[('QB', bi), 'ident'], writes=[bkk])
                        cx.copy('act', QTs[bi][:].rearrange("p a b -> p (a b)"), bv[:, 0:512], [bkk], [('QT', bi)])
                        bank, bkk = cx.ptbank()
                        bv = bank[:].rearrange("p a b -> p (a b)")
                        for hp in range(4):
                            for a in range(2):
                                p.op('pe', lambda e, hp=hp, a=a, bv=bv, bi=bi: e.transpose(out=bv[:, hp * 256 + a * 128:hp * 256 + (a + 1) * 128],
                                                                                         in_=KB[bi][:, a, hp * 128:(hp + 1) * 128], identity=ident[:]),
                                     reads=[('KB', bi), 'ident'], writes=[bkk])
                        cx.copy('dve', KTs[bi][:].rearrange("p a b -> p (a b)"), bv[:, 0:1024], [bkk], [('KT', bi)])
                        first = (n == 0)
                        mk, mkk = (MASKF, 'MASKF') if first else (MASK, 'MASK')
                        oa = OUTA[bi]
                        alive = [attn_head(h, bi, mk, mkk, oa) for h in range(8)]
                        while alive:
                            nxt_alive = []
                            for g_ in alive:
                                try:
                                    next(g_)
                                    nxt_alive.append(g_)
                                except StopIteration:
                                    pass
                            alive = nxt_alive
                        sv = scr_d[g, t0:t0 + 127 * d + 1:d, :]
                        p.dma('sp', sv, oa[:].rearrange("p a b -> p (a b)"), reads=[('OUTA', bi, h) for h in range(8)],
                              writes=[('scr', g, (t0 + i * d) // 128) for i in range(0, 128, max(1, 128 // d))] if d > 1 else [('scr', g, t0 // 128)])
            p.barrier()

        NT = PASS_T // 128
        HTm2 = [sb("HTm%d" % i, [128, 8, PASS_T], BF16) for i in range(2)]
        ACC = sb("ACC", [128, NT, D], F32)
        GT2 = [sb("GT%d" % i, [128, NT, 8], F32) for i in range(2)]
        RECS = sb("RECS", [128, 3, 8 * REC], F32)
        HB = sb("HBc", [128, 1, D], BF16)
        OB = sb("OB", [128, 1, 512], BF16)
        OT = sb("OT", [128, 4, 128], BF16)
        HIDm = [sb("HIDm%d" % i, [128, 2, PASS_T], BF16) for i in range(2)]
        WDs = [sb("WDs%d" % i, [128, 2, D], BF16) for i in range(2)]
        SG = [sb("SG%d" % i, [128, 512], F32) for i in range(2)]
        mM = sb("mM", [128, 8], F32)
        cg = sb("cg", [128, 3, 8], F32)
        den = sb("den", [128, 8], F32)
        numt = sb("numt", [128, 8, 64], F32)
        tmp3 = sb("tmp3", [128, 8, 64], F32)
        lg = sb("lg", [128, 8], F32)
        m8 = sb("m8", [128, 8], F32)
        nm0 = sb("nm0", [128, 1], F32)
        gden = sb("gden", [128, 1], F32)
        ge = sb("ge", [128, 8], F32)
        w8_i = 0
        wd_i = 0
        def merge_tile(ps_i, jt):
            HTm = HTm2[ps_i % 2]
            GT = GT2[ps_i % 2]
            hb_ = ps_i % 2
            tile = ps_i * NT + jt
            r0 = tile * 128
            for g in range(3):
                p.dma('sp', RECS[:, g, :], scr_d[g, r0:r0 + 128, :], reads=[('scr', g, tile)], writes=[('RECS', g)])
            rk = [('RECS', g) for g in range(3)]
            R = lambda g: RECS[:, g, :].rearrange("p (h r) -> p h r", r=REC)
            p.op('dve', lambda e: e.tensor_tensor(out=mM[:], in0=R(0)[:, :, 65], in1=R(1)[:, :, 65], op=ALU.max), reads=rk, writes=['mM'])
            p.op('dve', lambda e: e.tensor_tensor(out=mM[:], in0=mM[:], in1=R(2)[:, :, 65], op=ALU.max), reads=rk + ['mM'], writes=['mM'])
            for g in range(3):
                p.op('dve', lambda e, g=g: e.tensor_tensor(out=cg[:, g, :], in0=R(g)[:, :, 65], in1=mM[:], op=ALU.subtract), reads=rk + ['mM'], writes=[('cg', g)])
                p.op('act', lambda e, g=g: e.activation(out=cg[:, g, :], in_=cg[:, g, :], func=AF.Exp), reads=[('cg', g)], writes=[('cg', g)])
            p.op('dve', lambda e: e.tensor_tensor(out=den[:], in0=cg[:, 0, :], in1=R(0)[:, :, 64], op=ALU.mult), reads=rk + [('cg', 0)], writes=['den'])
            p.op('dve', lambda e: e.tensor_tensor(out=numt[:], in0=R(0)[:, :, 0:64], in1=cg[:, 0, :].unsqueeze(2).to_broadcast([128, 8, 64]), op=ALU.mult), reads=rk + [('cg', 0)], writes=['numt'])
            for g in (1, 2):
                p.op('dve', lambda e, g=g: e.tensor_tensor(out=lg[:], in0=cg[:, g, :], in1=R(g)[:, :, 64], op=ALU.mult), reads=rk + [('cg', g)], writes=['lg'])
                p.op('dve', lambda e: e.tensor_tensor(out=den[:], in0=den[:], in1=lg[:], op=ALU.add), reads=['den', 'lg'], writes=['den'])
                p.op('dve', lambda e, g=g: e.tensor_tensor(out=tmp3[:], in0=R(g)[:, :, 0:64], in1=cg[:, g, :].unsqueeze(2).to_broadcast([128, 8, 64]), op=ALU.mult), reads=rk + [('cg', g)], writes=['tmp3'])
                p.op('dve', lambda e: e.tensor_tensor(out=numt[:], in0=numt[:], in1=tmp3[:], op=ALU.add), reads=['numt', 'tmp3'], writes=['numt'])
            p.op('dve', lambda e: e.reciprocal(out=den[:], in_=den[:]), reads=['den'], writes=['den'])
            p.op('dve', lambda e: e.tensor_tensor(out=OB[:, 0, :].rearrange("p (h d) -> p h d", d=64), in0=numt[:], in1=den[:].unsqueeze(2).to_broadcast([128, 8, 64]), op=ALU.mult),
                 reads=['numt', 'den'], writes=['OB'])
            bank, bkk = cx.ptbank()
            for c in range(4):
                p.op('pe', lambda e, c=c, bank=bank: e.transpose(out=bank[:, 0, c * 128:(c + 1) * 128], in_=OB[:, 0, c * 128:(c + 1) * 128], identity=ident[:]), reads=['OB', 'ident'], writes=[bkk])
            cx.copy('act', OT[:].rearrange("p a b -> p (a b)"), bank[:, 0, :], [bkk], ['OT'])
            p.dma('sp', XA[:], x2_d[r0:r0 + 128, :], writes=['XA'])
            for half in range(2):
                ps, pk = cx.psum()
                for c in range(4):
                    p.op('pe', lambda e, c=c, ps=ps, half=half: e.matmul(ps[:], lhsT=OT[:, c, :], rhs=WO[:, c, half * 512:(half + 1) * 512], start=(c == 0), stop=(c == 3)), reads=['OT', 'WO'], writes=[pk])
                sl = slice(half * 512, (half + 1) * 512)
                p.op('dve', lambda e, ps=ps, sl=sl: e.tensor_tensor(out=YB[:, sl], in0=ps[:], in1=T["G1"][:, sl], op=ALU.mult), reads=[pk, "G1"], writes=['YB'])
            p.op('dve', lambda e: e.scalar_tensor_tensor(out=XA[:], in0=XA[:], scalar=ALPHA, in1=YB[:], op0=ALU.mult, op1=ALU.add), reads=['XA', 'YB'], writes=['XA'])
            cx.layer_norm_tile(XA[:], 'XA', 'c')
            p.op('dve', lambda e: e.tensor_tensor(out=TMP[:], in0=XA[:], in1=T["G2"][:], op=ALU.mult), reads=['XA', "G2"], writes=['TMP'])
            p.op('dve', lambda e: e.tensor_tensor(out=TMP[:], in0=TMP[:], in1=T["B2"][:], op=ALU.add), reads=['TMP', "B2"], writes=['TMP'])
            p.op('act', lambda e: e.copy(out=HB[:, 0, :], in_=TMP[:]), reads=['TMP'], writes=[('HBc', 0)])
            p.op('dve', lambda e: e.tensor_tensor(out=XA[:], in0=XA[:], in1=T["Gb0"][:], op=ALU.mult), reads=['XA', "Gb0"], writes=['XA'])
            p.op('dve', lambda e: e.tensor_tensor(out=XA[:], in0=XA[:], in1=T["Bb0"][:], op=ALU.add), reads=['XA', "Bb0"], writes=['XA'])
            p.dma('sp', x3_d[r0:r0 + 128, :], XA[:], reads=['XA'], writes=[('x3', tile)])
            for e_ in range(8):
                p.op('dve', lambda e, e_=e_: e.tensor_tensor(out=YB[:], in0=TMP[:], in1=RB[:, e_, :], op=ALU.mult), reads=['TMP', ('RB', e_)], writes=['YB'])
                p.op('act', lambda e, e_=e_: e.activation(out=YB[:], in_=YB[:], func=AF.Identity, accum_out=lg[:, e_:e_ + 1]), reads=['YB'], writes=['YB', 'lg'])
            p.op('dve', lambda e: e.max(out=m8[:], in_=lg[:]), reads=['lg'], writes=['m8'])
            p.op('dve', lambda e: e.tensor_scalar(out=nm0[:], in0=m8[:, 0:1], scalar1=-1.0, scalar2=None, op0=ALU.mult), reads=['m8'], writes=['nm0'])
            p.op('act', lambda e: e.activation(out=ge[:], in_=lg[:], func=AF.Exp, bias=nm0[:], scale=1.0), reads=['lg', 'nm0'], writes=['ge'])
            p.op('dve', lambda e: e.scalar_tensor_tensor(out=ge[:], in0=lg[:], scalar=m8[:, 1:2], in1=ge[:], op0=ALU.is_ge, op1=ALU.mult), reads=['lg', 'm8', 'ge'], writes=['ge'])
            p.op('dve', lambda e: e.reduce_sum(out=gden[:], in_=ge[:], axis=AX.X), reads=['ge'], writes=['gden'])
            p.op('dve', lambda e: e.reciprocal(out=gden[:], in_=gden[:]), reads=['gden'], writes=['gden'])
            p.op('dve', lambda e, jt=jt: e.tensor_scalar(out=GT[:, jt, :], in0=ge[:], scalar1=gden[:], scalar2=None, op0=ALU.mult), reads=['ge', 'gden'], writes=[('GT', hb_, jt)])
            bank, bkk = None, None
            for c2 in range(0, 8, 4):
                bank, bkk = cx.ptbank()
                bv = bank[:].rearrange("p a b -> p (a b)")
                for cc in range(4):
                    p.op('pe', lambda e, c2=c2, cc=cc, bv=bv: e.transpose(out=bv[:, cc * 128:(cc + 1) * 128], in_=HB[:, 0, (c2 + cc) * 128:(c2 + cc + 1) * 128], identity=ident[:]),
                         reads=[('HBc', 0), 'ident'], writes=[bkk])
                cx.copy(cx.evac_eng(), HTm[:, c2:c2 + 4, jt * 128:(jt + 1) * 128], bv[:, 0:512].rearrange("p (a b) -> p a b", b=128), [bkk], [('HTm', hb_, jt)])

        for jt in range(NT):
            merge_tile(0, jt)
        for ps_i in range(npass):
            HTm = HTm2[ps_i % 2]
            GT = GT2[ps_i % 2]
            hb_ = ps_i % 2
            htk = [('HTm', hb_, jt) for jt in range(NT)]
            for jt in range(NT):
                p.op('pool', lambda e, jt=jt: e.memset(ACC[:, jt, :], 0.0), writes=[('ACC', jt)])
            for ex in range(nexp):
                gv = wgu_d[ex].rearrange("(c p) n -> p c n", p=128)
                dv = wdn_d[ex].rearrange("(c p) n -> p c n", p=128)
                for s in range(D_FFE // 256):
                    wb, wk = W8[w8_i % 2], ('W8', w8_i % 2)
                    w8_i += 1
                    p.dma('pool', wb[:, :, 0:256], gv[:, :, s * 256:(s + 1) * 256], writes=[wk])
                    p.dma('pool', wb[:, :, 256:512], gv[:, :, D_FFE + s * 256:D_FFE + (s + 1) * 256], writes=[wk])
                    wdb, wdk = WDs[wd_i % 2], ('WDs', wd_i % 2)
                    hb, hk = HIDm[wd_i % 2], ('HIDm', wd_i % 2)
                    wd_i += 1
                    p.dma('pool', wdb[:, :, 0:512], dv[:, 2 * s:2 * s + 2, 0:512], writes=[wdk])
                    p.dma('pool', wdb[:, :, 512:1024], dv[:, 2 * s:2 * s + 2, 512:1024], writes=[wdk])
                    for fc in range(2):
                        for tg in range(PASS_T // 512):
                            tsl = slice(tg * 512, (tg + 1) * 512)
                            psg, pkg = cx.psum()
                            for c in range(8):
                                p.op('pe', lambda e, c=c, fc=fc, psg=psg, wb=wb, tsl=tsl, hb_=hb_: e.matmul(psg[:], lhsT=wb[:, c, fc * 128:(fc + 1) * 128], rhs=HTm2[hb_][:, c, tsl], start=(c == 0), stop=(c == 7)),
                                     reads=htk + [wk], writes=[pkg])
                            psu, pku = cx.psum()
                            for c in range(8):
                                p.op('pe', lambda e, c=c, fc=fc, psu=psu, wb=wb, tsl=tsl, hb_=hb_: e.matmul(psu[:], lhsT=wb[:, c, 256 + fc * 128:256 + (fc + 1) * 128], rhs=HTm2[hb_][:, c, tsl], start=(c == 0), stop=(c == 7)),
                                     reads=htk + [wk], writes=[pku])
                            sgi = (fc * 2 + tg) % 2
                            sg, sgk = SG[sgi], ('SG', sgi)
                            p.op('act', lambda e, psg=psg, sg=sg: e.activation(out=sg[:], in_=psg[:], func=AF.Silu), reads=[pkg], writes=[sgk])
                            p.op('dve', lambda e, psu=psu, sg=sg, hb=hb, fc=fc, tsl=tsl: e.tensor_tensor(out=hb[:, fc, tsl], in0=psu[:], in1=sg[:], op=ALU.mult), reads=[pku, sgk], writes=[hk])
                    for jt in range(NT):
                        for half in range(2):
                            ps, pk = cx.psum()
                            for fc in range(2):
                                p.op('pe', lambda e, fc=fc, ps=ps, hb=hb, wdb=wdb, jt=jt, half=half: e.matmul(ps[:], lhsT=hb[:, fc, jt * 128:(jt + 1) * 128], rhs=wdb[:, fc, half * 512:(half + 1) * 512],
                                                                                                         start=(fc == 0), stop=(fc == 1)), reads=[hk, wdk], writes=[pk])
                            sl = slice(half * 512, (half + 1) * 512)
                            p.op('dve', lambda e, ps=ps, jt=jt, sl=sl, ex=ex, hb_=hb_: e.scalar_tensor_tensor(out=ACC[:, jt, sl], in0=ps[:], scalar=GT2[hb_][:, jt, ex:ex + 1], in1=ACC[:, jt, sl], op0=ALU.mult, op1=ALU.add),
                                 reads=[pk, ('GT', hb_, jt), ('ACC', jt)], writes=[('ACC', jt)])
                if ps_i + 1 < npass and ex < NT:
                    merge_tile(ps_i + 1, ex)
            if ps_i + 1 < npass:
                for jt in range(min(nexp, NT), NT):
                    merge_tile(ps_i + 1, jt)
            for jt in range(NT):
                tile = ps_i * NT + jt
                r0 = tile * 128
                p.dma('sp', XA[:], x3_d[r0:r0 + 128, :], reads=[('x3', tile)], writes=['XA'])
                p.op('dve', lambda e, jt=jt: e.tensor_tensor(out=ACC[:, jt, :], in0=ACC[:, jt, :], in1=T["G3g"][:], op=ALU.mult), reads=[('ACC', jt), "G3g"], writes=[('ACC', jt)])
                p.op('dve', lambda e, jt=jt: e.scalar_tensor_tensor(out=XA[:], in0=XA[:], scalar=ALPHA, in1=ACC[:, jt, :], op0=ALU.mult, op1=ALU.add), reads=['XA', ('ACC', jt)], writes=['XA'])
                cx.layer_norm_tile(XA[:], 'XA', 'd')
                p.op('dve', lambda e: e.tensor_tensor(out=XA[:], in0=XA[:], in1=T["Gb1"][:], op=ALU.mult), reads=['XA', "Gb1"], writes=['XA'])
                p.op('dve', lambda e: e.tensor_tensor(out=XA[:], in0=XA[:], in1=T["Bb1"][:], op=ALU.add), reads=['XA', "Bb1"], writes=['XA'])
                p.dma('sp', out_d[r0:r0 + 128, :], XA[:], reads=['XA'])
        p.finish()
        p.emit()
    return nc


def maps_C(inp, x2, q, k, v):
    idxs = [8, 9, 10, 11]
    aw, ab = _ada_blocks(inp['ada_w'], inp['ada_b'], idxs)
    maps = []
    for c in range(NCORE):
        b, s0 = c // 4, (c % 4) * TPC
        if s0 == 0:
            kh = np.concatenate([np.zeros((HALO, 1536), k.dtype), k[b, 0:TPC]], axis=0)
            vh = np.concatenate([np.zeros((HALO, 1536), v.dtype), v[b, 0:TPC]], axis=0)
        else:
            kh = np.ascontiguousarray(k[b, s0 - HALO:s0 + TPC])
            vh = np.ascontiguousarray(v[b, s0 - HALO:s0 + TPC])
        maps.append({
            "x2": np.ascontiguousarray(x2[b, s0:s0 + TPC]), "q": np.ascontiguousarray(q[b, s0:s0 + TPC]),
            "kh": kh, "vh": vh,
            "halo_bias": np.full((128, 1), NEG if s0 == 0 else 0.0, np.float32),
            "cT": _cT(inp['c'][b]), "adaw": aw, "adab": ab,
            "lng": np.ascontiguousarray(inp['ln_g'][1]), "lnb": np.ascontiguousarray(inp['ln_b'][1]),
            "w_o": inp['attn_w_o'][0], "router": np.ascontiguousarray(inp['moe_router'][0].T),
            "moe_gu": inp['moe_w_gu'][0], "moe_dn": inp['moe_w_down'][0],
        })
    return maps


def _run(nc, maps):
    return run_bass_kernel_spmd(nc, maps, core_ids=list(range(NCORE))).results


def kernel(**inputs):
    inp = {k: np.asarray(v) for k, v in inputs.items()}
    resA = _run(build_A(), maps_A(inp))
    yg = np.empty((2, SEQ, D), np.float32)
    for c in range(NCORE):
        b, hg = c // 4, c % 4
        yg[b, :, 256 * hg:256 * hg + 256] = np.asarray(resA[c]["yg"])
    del resA
    resB = _run(build_B(), maps_B(inp, yg))
    x2 = np.empty((2, SEQ, D), np.float32)
    q = np.empty((2, SEQ, 1536), ml_dtypes.bfloat16)
    k = np.empty((2, SEQ, 1536), ml_dtypes.bfloat16)
    v = np.empty((2, SEQ, 1536), ml_dtypes.bfloat16)
    for c in range(NCORE):
        b, s0 = c // 4, (c % 4) * TPC
        x2[b, s0:s0 + TPC] = np.asarray(resB[c]["x2"])
        q[b, s0:s0 + TPC] = np.asarray(resB[c]["q"])
        k[b, s0:s0 + TPC] = np.asarray(resB[c]["k"])
        v[b, s0:s0 + TPC] = np.asarray(resB[c]["v"])
    del resB
    resC = _run(build_C(), maps_C(inp, x2, q, k, v))
    out = np.empty((2, SEQ, D), np.float32)
    for c in range(NCORE):
        b, s0 = c // 4, (c % 4) * TPC
        out[b, s0:s0 + TPC] = np.asarray(resC[c]["out"])
    return out
```
